# Optimizing a Trainium2 kernel written in Bass

```python
import jax, jax.numpy as jnp
from jax import lax
import numpy as np

D_MODEL = 2048
BATCH = 4
SEQ = 2048
DEPTH = 1
DEC_BATCH = 128
DEC_SEQ = 1
PAST_LEN = 16384
PAGE_SIZE = 128

HEAD_DIM = 128
A_Q_HEADS = 8
A_KV_HEADS = 2
A_GQA = A_Q_HEADS // A_KV_HEADS
A_WINDOW = 128
B_GROUPS = ((128, 1), (512, 4), (2048, 16))
B_HEADS_PER_GROUP = 4
N_B_GROUPS = len(B_GROUPS)
B_HEADS = N_B_GROUPS * B_HEADS_PER_GROUP
BAND_BLOCK = 128
A_Q_W = A_Q_HEADS * HEAD_DIM
A_KV_W = A_KV_HEADS * HEAD_DIM
B_W = B_HEADS * HEAD_DIM
B_OUT_W = B_HEADS_PER_GROUP * HEAD_DIM
IN_SPLITS = (A_Q_W, A_KV_W, A_KV_W, B_W, B_W, B_W, D_MODEL, D_MODEL)
N_IN = sum(IN_SPLITS)
N_ALIBI_HEADS = A_Q_HEADS + B_HEADS
ATTN_SCALE = HEAD_DIM ** -0.5
PEER_HEADS = 8
PEER_NKEYS = 128
PEER_EXPERTS = PEER_NKEYS * PEER_NKEYS
PEER_DKEY = 128
PEER_TOPK = 16
PEER_CHUNK = 128
NORM_EPS = 1e-6
NEG_INF = -1e30

kernel_name = 'hybrid_swa_sink_dilated_peer_step'


def rmsnorm(x, w):
    xf = x.astype(jnp.float32)
    y = xf * lax.rsqrt(jnp.mean(xf * xf, axis=-1, keepdims=True) + NORM_EPS)
    return (y * w.astype(jnp.float32)).astype(x.dtype)


def alibi_slopes():
    return 2.0 ** (-8.0 * jnp.arange(1, N_ALIBI_HEADS + 1, dtype=jnp.float32) / N_ALIBI_HEADS)


def project(xn, w_in, q_norm_a, k_norm_a, q_norm_b, k_norm_b):
    lead = xn.shape[:-1]
    z = xn @ w_in
    cuts = [int(c) for c in np.cumsum(IN_SPLITS)[:-1]]
    qa, ka, va, qb, kb, vb, ga, gb = jnp.split(z, cuts, axis=-1)
    qa = rmsnorm(qa.reshape(*lead, A_KV_HEADS, A_GQA, HEAD_DIM), q_norm_a)
    ka = rmsnorm(ka.reshape(*lead, A_KV_HEADS, HEAD_DIM), k_norm_a)
    va = va.reshape(*lead, A_KV_HEADS, HEAD_DIM)
    qb = rmsnorm(qb.reshape(*lead, N_B_GROUPS, B_HEADS_PER_GROUP, HEAD_DIM), q_norm_b)
    kb = rmsnorm(kb.reshape(*lead, N_B_GROUPS, B_HEADS_PER_GROUP, HEAD_DIM), k_norm_b)
    vb = vb.reshape(*lead, N_B_GROUPS, B_HEADS_PER_GROUP, HEAD_DIM)
    return qa, ka, va, qb, kb, vb, ga, gb


def masked_softmax(logits, mask, sink):
    logits = jnp.where(mask, logits, NEG_INF)
    m = jnp.max(logits, axis=-1)
    if sink is not None:
        m = jnp.maximum(m, sink)
    p = jnp.exp(logits - m[..., None])
    denom = jnp.sum(p, axis=-1)
    if sink is not None:
        denom = denom + jnp.exp(sink - m)
    return p / denom[..., None], m + jnp.log(denom)


def banded_window_attention(q, k, v, slopes, window_idx, dil, sink):
    N, n, Hk, G, hd = q.shape
    blk = BAND_BLOCK
    nb = -(-n // blk)
    n_pad = nb * blk
    pad = n_pad - n
    qb = jnp.pad(q, ((0, 0), (0, pad), (0, 0), (0, 0), (0, 0))).reshape(N, nb, blk, Hk, G, hd)

    def band(t):
        tp = jnp.pad(t, ((0, 0), (blk, pad), (0, 0), (0, 0)))
        prev = tp[:, :n_pad].reshape(N, nb, blk, Hk, hd)
        cur = tp[:, blk:].reshape(N, nb, blk, Hk, hd)
        return jnp.concatenate([prev, cur], axis=2)

    kw, vw = band(k), band(v)
    s = jnp.einsum('nbqhgd,nbshd->nbhgqs', qb, kw, preferred_element_type=jnp.float32) * ATTN_SCALE
    qi = jnp.arange(blk)[:, None]
    sj = jnp.arange(2 * blk)[None, :]
    dist = qi - sj + blk
    key_pos = (jnp.arange(nb) * blk - blk)[:, None, None] + sj[None]
    mask = (dist >= 0) & (dist <= window_idx) & (key_pos >= 0)
    logits = s - slopes[:, :, None, None] * (dil * dist).astype(jnp.float32)
    p, lse = masked_softmax(logits, mask[:, None, None], None if sink is None else sink[:, :, None])
    o = jnp.einsum('nbhgqs,nbshd->nbqhgd', p, vw.astype(jnp.float32)).astype(q.dtype)
    o = o.reshape(N, n_pad, Hk, G, hd)[:, :n]
    lse = lse.transpose(0, 1, 4, 2, 3).reshape(N, n_pad, Hk, G)[:, :n]
    return o, lse


def gathered_window_attention(q, k_new, v_new, cache_kv, slopes, window, dil, sink):
    L = cache_kv.shape[1]
    ds = q.shape[1]
    kcat = jnp.concatenate([cache_kv[:, :, 0], k_new], axis=1)
    vcat = jnp.concatenate([cache_kv[:, :, 1], v_new], axis=1)
    steps = jnp.arange(window // dil + 1)
    idx = L + jnp.arange(ds)[:, None] - steps[None, :] * dil
    valid = idx >= 0
    idx = jnp.maximum(idx, 0)
    kg, vg = kcat[:, idx], vcat[:, idx]
    s = jnp.einsum('bjhgd,bjshd->bjhgs', q, kg, preferred_element_type=jnp.float32) * ATTN_SCALE
    logits = s - slopes[:, :, None] * (steps * dil).astype(jnp.float32)
    p, lse = masked_softmax(logits, valid[None, :, None, None, :], sink)
    o = jnp.einsum('bjhgs,bjshd->bjhgd', p, vg.astype(jnp.float32)).astype(q.dtype)
    new_buf = jnp.stack([kcat, vcat], axis=2)[:, -L:]
    return o, lse, new_buf


def to_residue(t, d):
    b, s = t.shape[:2]
    rest = t.shape[2:]
    return t.reshape(b, s // d, d, *rest).swapaxes(1, 2).reshape(b * d, s // d, *rest)


def from_residue(t, d, b):
    n = t.shape[1]
    rest = t.shape[2:]
    return t.reshape(b, d, n, *rest).swapaxes(1, 2).reshape(b, n * d, *rest)


def window_rows(k, v, window):
    L = min(window, k.shape[1])
    return jnp.stack([k[:, -L:], v[:, -L:]], axis=2)


def combine_dilated(outs, lses):
    o = jnp.stack(outs, axis=-3)
    w = jax.nn.softmax(jnp.stack(lses, axis=-2), axis=-2)
    return jnp.sum(w[..., None] * o.astype(jnp.float32), axis=-3).astype(outs[0].dtype)


def dilated_prompt(qb, kb, vb, slopes_b):
    b = qb.shape[0]
    outs, lses = [], []
    for g, (win, dil) in enumerate(B_GROUPS):
        o, lse = banded_window_attention(
            to_residue(qb[:, :, g, :, None, :], dil), to_residue(kb[:, :, g], dil),
            to_residue(vb[:, :, g], dil), slopes_b[g], win // dil, dil, None)
        outs.append(from_residue(o[:, :, :, 0], dil, b))
        lses.append(from_residue(lse[:, :, :, 0], dil, b))
    return combine_dilated(outs, lses)


def dilated_sample(qb, kb, vb, caches, slopes_b):
    outs, lses, bufs = [], [], []
    for g, (win, dil) in enumerate(B_GROUPS):
        o, lse, buf = gathered_window_attention(
            qb[:, :, g, :, None, :], kb[:, :, g], vb[:, :, g], caches[g], slopes_b[g], win, dil, None)
        outs.append(o[:, :, :, 0])
        lses.append(lse[:, :, :, 0])
        bufs.append(buf)
    return combine_dilated(outs, lses), bufs


def peer_route(hn, wq, subkeys):
    t = hn.shape[0]
    q = (hn @ wq).reshape(t, PEER_HEADS, 2, PEER_DKEY // 2)
    s = jnp.einsum('thcd,cnd->thcn', q, subkeys, preferred_element_type=jnp.float32)
    half_s, half_i = lax.top_k(s, PEER_TOPK)
    cand = (half_s[:, :, 0, :, None] + half_s[:, :, 1, None, :]).reshape(t, PEER_HEADS, PEER_TOPK * PEER_TOPK)
    best_s, best_c = lax.top_k(cand, PEER_TOPK)
    i1 = jnp.take_along_axis(half_i[:, :, 0], best_c // PEER_TOPK, axis=-1)
    i2 = jnp.take_along_axis(half_i[:, :, 1], best_c % PEER_TOPK, axis=-1)
    experts = (i1 * PEER_NKEYS + i2).reshape(t, -1)
    return experts, jax.nn.softmax(best_s, axis=-1).reshape(t, -1)


def peer_experts(hn, experts, gates, u, v):
    t, d = hn.shape
    n_chunks = -(-t // PEER_CHUNK)
    pad = n_chunks * PEER_CHUNK - t

    def chunked(a):
        return jnp.pad(a, ((0, pad), (0, 0))).reshape(n_chunks, PEER_CHUNK, a.shape[-1])

    def one_chunk(args):
        xc, ec, gc = args
        act = jax.nn.gelu(jnp.einsum('cd,ced->ce', xc, u[ec], preferred_element_type=jnp.float32),
                          approximate=False)
        return jnp.einsum('ce,ced->cd', (gc * act).astype(xc.dtype), v[ec])

    out = lax.map(one_chunk, (chunked(hn), chunked(experts), chunked(gates)))
    return out.reshape(n_chunks * PEER_CHUNK, d)[:t]


def block_output(x, oa, ob, ga, gb, w_branch_a, w_branch_b, w_out,
                 norm2_w, peer_wq, peer_subkeys, peer_u, peer_v):
    lead = x.shape[:-1]
    ya = oa.reshape(*lead, A_Q_W) @ w_branch_a
    yb = ob.reshape(*lead, B_OUT_W) @ w_branch_b
    h = x + (jax.nn.sigmoid(ga) * ya + jax.nn.sigmoid(gb) * yb) @ w_out
    hn = rmsnorm(h, norm2_w).reshape(-1, D_MODEL)
    experts, gates = peer_route(hn, peer_wq, peer_subkeys)
    return h + peer_experts(hn, experts, gates, peer_u, peer_v).reshape(h.shape)


def setup_inputs(seed: int = 0) -> dict:
    key = jax.random.key(seed)
    ks = jax.random.split(key, 24)

    def nrm(k, shape, scale):
        return jax.random.normal(k, shape, jnp.float32) * scale

    def gain(k, shape):
        return 1.0 + 0.02 * jax.random.normal(k, shape, jnp.float32)

    la = min(A_WINDOW, PAST_LEN)
    l1, l2, l3 = (min(w, PAST_LEN) for w, _ in B_GROUPS)
    return {
        'x_prompt': nrm(ks[0], (BATCH, SEQ, D_MODEL), 1.0),
        'x_sample': nrm(ks[1], (DEC_BATCH, DEC_SEQ, D_MODEL), 1.0),
        'cache_a_kv': nrm(ks[2], (DEPTH, DEC_BATCH, la, 2, A_KV_HEADS, HEAD_DIM), 1.0),
        'cache_b1_kv': nrm(ks[3], (DEPTH, DEC_BATCH, l1, 2, B_HEADS_PER_GROUP, HEAD_DIM), 1.0),
        'cache_b2_kv': nrm(ks[4], (DEPTH, DEC_BATCH, l2, 2, B_HEADS_PER_GROUP, HEAD_DIM), 1.0),
        'cache_b3_kv': nrm(ks[5], (DEPTH, DEC_BATCH, l3, 2, B_HEADS_PER_GROUP, HEAD_DIM), 1.0),
        'norm1_w': gain(ks[6], (DEPTH, D_MODEL)),
        'w_in': nrm(ks[7], (DEPTH, D_MODEL, N_IN), D_MODEL ** -0.5),
        'q_norm_a': gain(ks[8], (DEPTH, HEAD_DIM)),
        'k_norm_a': gain(ks[9], (DEPTH, HEAD_DIM)),
        'sink_a': nrm(ks[10], (DEPTH, A_Q_HEADS), 0.5),
        'q_norm_b': gain(ks[11], (DEPTH, HEAD_DIM)),
        'k_norm_b': gain(ks[12], (DEPTH, HEAD_DIM)),
        'w_branch_a': nrm(ks[13], (DEPTH, A_Q_W, D_MODEL), A_Q_W ** -0.5),
        'w_branch_b': nrm(ks[14], (DEPTH, B_OUT_W, D_MODEL), B_OUT_W ** -0.5),
        'w_out': nrm(ks[15], (DEPTH, D_MODEL, D_MODEL), D_MODEL ** -0.5),
        'norm2_w': gain(ks[16], (DEPTH, D_MODEL)),
        'peer_wq': nrm(ks[17], (DEPTH, D_MODEL, PEER_HEADS * PEER_DKEY), D_MODEL ** -0.5),
        'peer_subkeys': nrm(ks[18], (DEPTH, 2, PEER_NKEYS, PEER_DKEY // 2), (PEER_DKEY // 2) ** -0.5),
        'peer_u': nrm(ks[19], (DEPTH, PEER_EXPERTS, D_MODEL), D_MODEL ** -0.5),
        'peer_v': nrm(ks[20], (DEPTH, PEER_EXPERTS, D_MODEL), PEER_HEADS ** -0.5),
    }


def reference(x_prompt, x_sample, cache_a_kv, cache_b1_kv, cache_b2_kv, cache_b3_kv,
              norm1_w, w_in, q_norm_a, k_norm_a, sink_a, q_norm_b, k_norm_b,
              w_branch_a, w_branch_b, w_out, norm2_w, peer_wq, peer_subkeys, peer_u, peer_v):
    slopes = alibi_slopes()
    slopes_a = slopes[:A_Q_HEADS].reshape(A_KV_HEADS, A_GQA)
    slopes_b = slopes[A_Q_HEADS:].reshape(N_B_GROUPS, B_HEADS_PER_GROUP, 1)
    hp, hs = x_prompt, x_sample
    sa_p, sa_s = [], []
    sb_p = [[] for _ in B_GROUPS]
    sb_s = [[] for _ in B_GROUPS]
    for l in range(DEPTH):
        sink = sink_a[l].astype(jnp.float32).reshape(A_KV_HEADS, A_GQA)
        ffn_w = (norm2_w[l], peer_wq[l], peer_subkeys[l], peer_u[l], peer_v[l])
        qa, ka, va, qb, kb, vb, ga, gb = project(rmsnorm(hp, norm1_w[l]), w_in[l],
                                                 q_norm_a[l], k_norm_a[l], q_norm_b[l], k_norm_b[l])
        oa, _ = banded_window_attention(qa, ka, va, slopes_a, A_WINDOW, 1, sink)
        ob = dilated_prompt(qb, kb, vb, slopes_b)
        sa_p.append(window_rows(ka, va, A_WINDOW))
        for g, (win, _) in enumerate(B_GROUPS):
            sb_p[g].append(window_rows(kb[:, :, g], vb[:, :, g], win))
        hp = block_output(hp, oa, ob, ga, gb, w_branch_a[l], w_branch_b[l], w_out[l], *ffn_w)
        qa, ka, va, qb, kb, vb, ga, gb = project(rmsnorm(hs, norm1_w[l]), w_in[l],
                                                 q_norm_a[l], k_norm_a[l], q_norm_b[l], k_norm_b[l])
        oa, _, buf_a = gathered_window_attention(qa, ka, va, cache_a_kv[l], slopes_a, A_WINDOW, 1, sink)
        ob, bufs_b = dilated_sample(qb, kb, vb, (cache_b1_kv[l], cache_b2_kv[l], cache_b3_kv[l]), slopes_b)
        sa_s.append(buf_a)
        for g in range(N_B_GROUPS):
            sb_s[g].append(bufs_b[g])
        hs = block_output(hs, oa, ob, ga, gb, w_branch_a[l], w_branch_b[l], w_out[l], *ffn_w)
    return (hp, hs,
            jnp.stack(sa_p), jnp.stack(sb_p[0]), jnp.stack(sb_p[1]), jnp.stack(sb_p[2]),
            jnp.stack(sa_s), jnp.stack(sb_s[0]), jnp.stack(sb_s[1]), jnp.stack(sb_s[2]))
```

```python
import numpy as np
from contextlib import ExitStack
import concourse.bass as bass
import concourse.mybir as mybir
from concourse.bass_utils import run_bass_kernel_spmd

F32 = mybir.dt.float32
BF16 = mybir.dt.bfloat16
I32 = mybir.dt.int32
U32 = mybir.dt.uint32
AF = mybir.ActivationFunctionType
ALU = mybir.AluOpType
AX = mybir.AxisListType

STREAMS = ("pe", "act", "dve", "pool", "sp")
NDMA_SEMS = 8


class Prog:
    def __init__(self, nc):
        self.nc = nc
        self.ops = {s: [] for s in STREAMS}
        self.semnames = list(STREAMS) + [f"d{s}{j}" for s in ("sp", "act", "pool") for j in range(NDMA_SEMS)]
        self.count = {n: 0 for n in self.semnames}
        self.dma_i = {"sp": 0, "act": 0, "pool": 0}
        self.last_w = {}
        self.readers = {}
        self.waited = {s: {} for s in STREAMS}
        self.n_ops = 0

    def _deps(self, reads, writes):
        deps = {}

        def add(d):
            if d is not None and d[1] > deps.get(d[0], 0):
                deps[d[0]] = d[1]

        for k in reads:
            add(self.last_w.get(k))
        for k in writes:
            add(self.last_w.get(k))
            for r in self.readers.get(k, ()):
                add(r)
        return deps

    def op(self, stream, fn, reads=(), writes=(), dma=False):
        deps = self._deps(reads, writes)
        if dma:
            dname = f"d{stream}{self.dma_i[stream] % NDMA_SEMS}"
            if self.count[dname] > 0:
                deps[dname] = max(deps.get(dname, 0), self.count[dname])
        w = self.waited[stream]
        waits = []
        for sname, v in deps.items():
            if v > w.get(sname, 0):
                waits.append((sname, v))
                w[sname] = v
        if dma:
            j = self.dma_i[stream] % NDMA_SEMS
            self.dma_i[stream] += 1
            sname = f"d{stream}{j}"
            inc = 16
        else:
            sname = stream
            inc = 1
        self.count[sname] += inc
        val = self.count[sname]
        self.ops[stream].append((waits, fn, sname, inc))
        for k in reads:
            self.readers.setdefault(k, []).append((sname, val))
        for k in writes:
            self.last_w[k] = (sname, val)
            self.readers[k] = []
        self.n_ops += 1
        return (sname, val)

    def barrier(self):
        waits = [(n, c) for n, c in self.count.items() if c > 0]
        for s in STREAMS:
            w = self.waited[s]
            ws = [(n, c) for n, c in waits if c > w.get(n, 0)]
            for n, c in ws:
                w[n] = c
            if ws:
                self.ops[s].append((ws, None, None, 0))
        self.last_w = {}
        self.readers = {}

    def emit(self):
        nc = self.nc
        with ExitStack() as es:
            sems = {n: es.enter_context(nc.semaphore(n)) for n in self.semnames}
            block = es.enter_context(nc.Block())

            def make(stream):
                def body(eng):
                    for waits, fn, sname, inc in self.ops[stream]:
                        for wn, wv in waits:
                            eng.wait_ge(sems[wn], wv)
                        if fn is not None:
                            fn(eng).then_inc(sems[sname], inc)
                return body

            block.tensor(make("pe"))
            block.scalar(make("act"))
            block.vector(make("dve"))
            block.gpsimd(make("pool"))
            block.sync(make("sp"))


D = 2048
NTOK = 1024
NT = 8
NS = 16
NIN = 10240
EPS = 1e-6
SCALE = 128.0 ** -0.5
SLOPES = [float(2.0 ** (-8.0 * i / 20.0)) for i in range(1, 21)]
XT_H, XT_O, XT_S = 0, 1024, 2048
NXT = 2064

GROUPS = {
    "A": dict(nh=1, nd=2, dil=1, win=128, H=2, L=128),
    "B1": dict(nh=1, nd=2, dil=1, win=128, H=4, L=128),
    "B2": dict(nh=4, nd=5, dil=4, win=512, H=4, L=512),
    "B3": dict(nh=8, nd=16, dil=16, win=2048, H=4, L=2048),
}


def make_units():
    units = []
    for kvh in range(2):
        units.append(dict(name=f"A{kvh}", grp="A", nq=4, nk=1,
                          qjobs=[(kvh * 512, 256, 0), (kvh * 512 + 256, 256, 2)],
                          kcol=1024 + kvh * 128, vcol=1280 + kvh * 128, kvhead0=kvh,
                          slopes=[SLOPES[kvh * 4 + g] for g in range(4)], ohead0=kvh * 4))
    for g, gname in enumerate(("B1", "B2", "B3")):
        for hp in range(2):
            units.append(dict(name=f"{gname}_{hp}", grp=gname, nq=2, nk=2,
                              qjobs=[(1536 + g * 512 + hp * 256, 256, 0)],
                              kcol=3072 + g * 512 + hp * 256, vcol=4608 + g * 512 + hp * 256,
                              kvhead0=hp * 2,
                              slopes=[SLOPES[8 + g * 4 + hp * 2 + j] for j in range(2)], ohead0=hp * 2))
    return units


UNITS = make_units()


def unit_variants(u):
    G = GROUPS[u["grp"]]
    nd, dil, win = G["nd"], G["dil"], G["win"]
    s = np.arange(128)[:, None].astype(np.float64)
    q = np.arange(128)[None, :].astype(np.float64)
    if u["grp"] == "B3":
        nvar = 2
        vmap = [(0, [0.0] * u["nq"])] + [(1, [-sl * 128.0 * (dl - 1) for sl in u["slopes"]]) for dl in range(1, nd)]
        var_delta = [0, 1]
    else:
        nvar = nd
        vmap = [(dl, [0.0] * u["nq"]) for dl in range(nd)]
        var_delta = list(range(nd))
    tab = np.zeros((128, u["nq"], nvar, 128), np.float32)
    for j, sl in enumerate(u["slopes"]):
        for v, dl in enumerate(var_delta):
            dist = 128.0 * dl + q - s
            ok = (dist >= 0) & (dist <= win) & (np.mod(dist, dil) == 0)
            tab[:, j, v, :] = np.where(ok, np.exp(-sl * np.where(ok, dist, 0.0)), 0.0)
    return nvar, vmap, tab


def build(stop_after=None, debug=False, bis=None):
    nc = bass.Bass("TRN2", target_bir_lowering=False)

    def din(name, shape, dt=F32):
        return nc.dram_tensor(name, list(shape), dt, kind="ExternalInput").ap()

    def dout(name, shape, dt=F32):
        return nc.dram_tensor(name, list(shape), dt, kind="ExternalOutput").ap()

    xo = din("xo", [NTOK, D]); xh = din("xh", [NTOK, D]); xs = din("xs", [NS, D])
    flag = din("flag", [128, 2])
    caches = {"A": din("ca", [NS, 128, 512]), "B1": din("cb1", [NS, 128, 1024]),
              "B2": din("cb2", [NS, 512, 1024]), "B3": din("cb3", [NS, 2048, 1024])}
    norm1_w = din("norm1_w", [D]); w_in = din("w_in", [D, NIN])
    qna = din("qna", [128]); kna = din("kna", [128]); sink = din("sink", [8])
    qnb = din("qnb", [128]); knb = din("knb", [128])
    wba = din("wba", [1024, D]); wbb = din("wbb", [512, D]); wout = din("wout", [D, D])
    norm2_w = din("norm2_w", [D]); wq = din("wq", [D, 1024]); subk = din("subk", [2, 128, 64])
    with_peer = stop_after is None and not debug
    if with_peer:
        pu = din("pu", [16384, D]); pv = din("pv", [16384, D])
    sbias_d = din("sbias", [128, 20]); dmask_d = din("dmask", [128, NS * NS]); iota16_d = din("iota16", [128, 16])
    subkT = din("subkT", [2, 64, 128])
    tabs_d = {}
    for u in UNITS:
        nvar, _, _ = unit_variants(u)
        tabs_d[u["name"]] = din("tab_" + u["name"], [128, u["nq"] * nvar * 128])

    yp = dout("yp", [NTOK, D]); ys = dout("ys", [NS, D])
    kvp = {"A": dout("kva_p", [128, 512]), "B1": dout("kvb1_p", [128, 1024]),
           "B2": dout("kvb2_p", [512, 1024]), "B3": dout("kvb3_p", [1024, 1024])}
    kvs = {"A": dout("kva_s", [NS, 128, 512]), "B1": dout("kvb1_s", [NS, 128, 1024]),
           "B2": dout("kvb2_s", [NS, 512, 1024]), "B3": dout("kvb3_s", [NS, 2048, 1024])}
    dbg = {}
    if debug:
        dbg["oaT"] = dout("dbg_oaT", [128, 8, 1040], BF16)
        dbg["obT"] = dout("dbg_obT", [128, 4, 1040], BF16)

    P = Prog(nc)
    with ExitStack() as es:
        def sb(name, shape, dt=F32):
            return es.enter_context(nc.sbuf_tensor(name, list(shape), dt))

        def ps(name, shape, dt=F32):
            return es.enter_context(nc.psum_tensor(name, list(shape), dt))

        ident = sb("ident", [128, 128])
        flag_t = sb("flag_t", [128, 2])
        gains = sb("gains", [128, 4, 128])
        esink = sb("esink", [128, 8])
        esA = ExitStack()

        def sbA(name, shape, dt=F32):
            return esA.enter_context(nc.sbuf_tensor(name, list(shape), dt))
        xnT = sbA("xnT", [128, 16, NXT], BF16)
        wring = [sbA(f"wring{i}", [128, 16, 256], BF16) for i in range(2)]
        oaT = sbA("oaT", [128, 8, 1040], BF16)
        obT = sbA("obT", [128, 4, 1040], BF16)
        pz = ps("pz", [128, 2, 512])
        ptr = ps("ptr", [128, 2, 512])
        pst = ps("pst", [128, 2, 512])
        po = ps("po", [128, 4, 256])

        P.op("pool", lambda e: e.memset(ident[:], 0.0), writes=["ident"])
        P.op("pool", lambda e: e.affine_select(out=ident[:], in_=ident[:], pattern=[[-1, 128]],
                                               compare_op=ALU.not_equal, fill=1.0, base=0, channel_multiplier=1),
             reads=["ident"], writes=["ident"])
        P.op("sp", lambda e: e.dma_start(out=flag_t[:], in_=flag), writes=["flag"], dma=True)
        for i, g in enumerate((qna, kna, qnb, knb)):
            P.op("sp", lambda e, i=i, g=g: e.dma_start(out=gains[:, i, :], in_=g.partition_broadcast(128)),
                 writes=[("gains", i)], dma=True)
        P.op("sp", lambda e: e.dma_start(out=esink[:], in_=sink.partition_broadcast(128)), writes=["esink"], dma=True)
        P.op("act", lambda e: e.activation(out=esink[:], in_=esink[:], func=AF.Exp), reads=["esink"], writes=["esink"])
        for gname, G in GROUPS.items():
            L = G["L"]
            for t0 in range(0, NS, 4):
                P.op("act", lambda e, gname=gname, L=L, t0=t0: e.dma_start(
                    out=kvs[gname][t0:t0 + 4, 0:L - 1, :], in_=caches[gname][t0:t0 + 4, 1:L, :]),
                    writes=[("kvs_copy", gname, t0)], dma=True)

        with ExitStack() as es1:
            def sb1(name, shape, dt=F32):
                return es1.enter_context(nc.sbuf_tensor(name, list(shape), dt))
            w1rep = sb1("w1rep", [128, D])
            xbuf = [sb1(f"xbuf{i}", [128, D]) for i in range(2)]
            xnb = [sb1(f"xnb{i}", [128, D]) for i in range(2)]
            junk = sb1("junk1", [128, D], BF16)
            st1 = [sb1(f"st1_{i}", [128, 4]) for i in range(2)]
            P.op("sp", lambda e: e.dma_start(out=w1rep[:], in_=norm1_w.partition_broadcast(128)), writes=["w1rep"], dma=True)
            tiles = [(xh, i, XT_H + i * 128, 128) for i in range(8)] + [(xo, i, XT_O + i * 128, 128) for i in range(8)] + [(xs, 0, XT_S, NS)]
            for ti, (src, i, toff, n) in enumerate(tiles):
                s = ti % 2
                xb, xn_, st = xbuf[s], xnb[s], st1[s]
                P.op("sp", lambda e, xb=xb, src=src, i=i, n=n: e.dma_start(out=xb[:n, :], in_=src[i * 128:i * 128 + n, :]),
                     writes=[("xbuf", s)], dma=True)
                P.op("act", lambda e, xb=xb, st=st, n=n: e.activation(out=junk[:n, :], in_=xb[:n, :], func=AF.Square, accum_out=st[:n, 0:1]),
                     reads=[("xbuf", s)], writes=["junk1", ("st1", s)])
                P.op("act", lambda e, st=st, n=n: e.activation(out=st[:n, 1:2], in_=st[:n, 0:1], func=AF.Ln, scale=1.0 / D, bias=EPS),
                     reads=[("st1", s)], writes=[("st1", s)])
                P.op("act", lambda e, st=st, n=n: e.activation(out=st[:n, 2:3], in_=st[:n, 1:2], func=AF.Exp, scale=-0.5),
                     reads=[("st1", s)], writes=[("st1", s)])
                P.op("dve", lambda e, xb=xb, xn_=xn_, st=st, n=n: e.scalar_tensor_tensor(
                    out=xn_[:n, :], in0=xb[:n, :], scalar=st[:n, 2:3], in1=w1rep[:n, :], op0=ALU.mult, op1=ALU.mult),
                    reads=[("xbuf", s), ("st1", s), "w1rep"], writes=[("xnb", s)])
                for k4 in range(4):
                    bank = (ti * 4 + k4) % 2
                    for kk in range(4):
                        k = k4 * 4 + kk
                        P.op("pe", lambda e, xn_=xn_, k=k, kk=kk, bank=bank, n=n: e.transpose(
                            out=ptr[:, bank, kk * 128:kk * 128 + n], in_=xn_[:n, k * 128:(k + 1) * 128], identity=ident[:n, :n]),
                            reads=[("xnb", s), "ident"], writes=[("ptr", bank)])
                    eng = "act" if k4 % 2 == 0 else "dve"
                    if eng == "act":
                        P.op("act", lambda e, k4=k4, bank=bank, toff=toff, n=n: e.activation(
                            out=xnT[:, k4 * 4:(k4 + 1) * 4, toff:toff + n],
                            in_=ptr[:, bank, :].rearrange("p (a b) -> p a b", b=128)[:, :, :n], func=AF.Copy),
                            reads=[("ptr", bank)], writes=[("xnT", toff)])
                    else:
                        P.op("dve", lambda e, k4=k4, bank=bank, toff=toff, n=n: e.tensor_copy(
                            out=xnT[:, k4 * 4:(k4 + 1) * 4, toff:toff + n],
                            in_=ptr[:, bank, :].rearrange("p (a b) -> p a b", b=128)[:, :, :n]),
                            reads=[("ptr", bank)], writes=[("xnT", toff)])
            P.barrier()

        wcount = [0]
        ring = list(wring)

        def load_w(src, col0, ncols, nk=16):
            s = wcount[0] % len(ring)
            wcount[0] += 1
            wt = ring[s]
            P.op("pool", lambda e: e.dma_start(out=wt[:, :nk, :ncols],
                                               in_=src[0:nk * 128, col0:col0 + ncols].rearrange("(k p) n -> p k n", p=128)),
                 writes=[("wring", s)], dma=True)
            return wt, ("wring", s)

        if stop_after == 1:
            P.barrier(); P.emit(); esA.close(); return nc

        with ExitStack() as es2:
            def sb2(name, shape, dt=F32):
                return es2.enter_context(nc.sbuf_tensor(name, list(shape), dt))
            QT = sb2("QT", [128, 4, NTOK], BF16)
            accBv = sb2("accBv", [128, NT, 4, 128])
            accBd = sb2("accBd", [128, NT, 4])
            onesc = sb2("onesc", [128, 2], BF16)
            po2 = po[:].rearrange("p a b -> p (a b)")
            KT = sb2("KT", [128, 2, 16 * 128], BF16)
            VA = sb2("VA", [128, 16, 2, 128], BF16)
            tabt = sb2("tabt", [128, 2 * 5 * 128], BF16)
            sq = sb2("sq", [128, 256])
            zn = [sb2(f"zn{i}", [128, 256]) for i in range(2)]
            vf = [sb2(f"vf{i}", [128, 256]) for i in range(2)]
            st2 = [sb2(f"st2_{i}", [128, 8]) for i in range(2)]
            Eb = [sb2(f"Eb{i}", [128, 512]) for i in range(2)]
            Pm = [sb2(f"Pm{i}", [128, 4096], BF16) for i in range(2)]
            ot = [sb2(f"ot{i}", [128, 4, 128]) for i in range(2)]
            rc = [sb2(f"rc{i}", [128, 4]) for i in range(2)]
            cnt = dict(z=0, tr=0, zn=0, vf=0, st=0, E=0, pm=0, ot=0)

            def proj_tile(wt, wkey, ncols, toff, n):
                b = cnt["z"] % 2
                cnt["z"] += 1
                for k in range(16):
                    P.op("pe", lambda e, k=k, b=b: e.matmul(pz[:n, b, :ncols], lhsT=xnT[:, k, toff:toff + n], rhs=wt[:, k, :ncols],
                                                         start=(k == 0), stop=(k == 15)),
                         reads=[("xnT", toff), wkey], writes=[("pz", b)])
                return b

            def qk_norm(b, nheads, gain_idx, n):
                ncols = nheads * 128
                s = cnt["zn"] % 2
                cnt["zn"] += 1
                st = st2[s]
                P.op("act", lambda e: e.activation(out=sq[:n, :ncols], in_=pz[:n, b, :ncols], func=AF.Square),
                     reads=[("pz", b)], writes=["sq"])
                P.op("dve", lambda e: e.tensor_reduce(out=st[:n, 0:nheads], in_=sq[:n, :ncols].rearrange("p (h d) -> p h d", d=128),
                                                      axis=AX.X, op=ALU.add),
                     reads=["sq"], writes=[("st2", s)])
                P.op("act", lambda e: e.activation(out=st[:n, 2:2 + nheads], in_=st[:n, 0:nheads], func=AF.Ln, scale=1.0 / 128, bias=EPS),
                     reads=[("st2", s)], writes=[("st2", s)])
                P.op("act", lambda e: e.activation(out=st[:n, 4:4 + nheads], in_=st[:n, 2:2 + nheads], func=AF.Exp, scale=-0.5),
                     reads=[("st2", s)], writes=[("st2", s)])
                for h in range(nheads):
                    P.op("dve", lambda e, h=h: e.scalar_tensor_tensor(
                        out=zn[s][:n, h * 128:(h + 1) * 128], in0=pz[:n, b, h * 128:(h + 1) * 128], scalar=st[:n, 4 + h:5 + h],
                        in1=gains[:n, gain_idx, :], op0=ALU.mult, op1=ALU.mult),
                        reads=[("pz", b), ("st2", s), ("gains", gain_idx)], writes=[("zn", s)])
                return s

            def transpose_to(src_ap_fn, nheads, n, dst_ap, dkey, rkeys):
                bank = cnt["tr"] % 2
                cnt["tr"] += 1
                for h in range(nheads):
                    P.op("pe", lambda e, h=h: e.transpose(out=ptr[:, bank, h * 128:h * 128 + n], in_=src_ap_fn(h), identity=ident[:n, :n]),
                         reads=list(rkeys) + ["ident"], writes=[("ptr", bank)])
                P.op("act", lambda e: e.activation(out=dst_ap, in_=ptr[:, bank, :nheads * 128].rearrange("p (a b) -> p a b", b=128)[:, :, :n],
                                                   func=AF.Copy),
                     reads=[("ptr", bank)], writes=[dkey])

            for ui, u in enumerate(UNITS):
                if bis is not None and ui >= bis[0]:
                    break
                G = GROUPS[u["grp"]]
                nh, nd = G["nh"], G["nd"]
                nq, nk = u["nq"], u["nk"]
                gq = nq // nk
                isA = u["grp"] == "A"
                nvar, vmap, _ = unit_variants(u)
                W = G["H"] * 128
                P.op("pool", lambda e, u=u, nvar=nvar, nq=nq: e.dma_start(out=tabt[:, :nq * nvar * 128], in_=tabs_d[u["name"]]),
                     writes=["tabt"], dma=True)
                for (c0, nc_, h0) in u["qjobs"]:
                    wt, wkey = load_w(w_in, c0, nc_)
                    for i in range(NT):
                        if bis is not None and len(bis) > 2 and bis[2] < 2:
                            break
                        b = proj_tile(wt, wkey, nc_, XT_O + i * 128, 128)
                        s = qk_norm(b, nc_ // 128, 0 if isA else 2, 128)
                        transpose_to(lambda h, s=s: zn[s][:, h * 128:(h + 1) * 128], nc_ // 128, 128,
                                     QT[:, h0:h0 + nc_ // 128, i * 128:(i + 1) * 128], ("QT", i), [("zn", s)])
                if bis is not None and len(bis) > 2 and bis[2] < 3:
                    continue
                nkc = nk * 128
                wt, wkey = load_w(w_in, u["kcol"], nkc)
                ktiles = [(XT_H + (8 - nh + j) * 128, j, None) for j in range(nh)] + [(XT_O + i * 128, nh + i, i) for i in range(NT)]
                out_tiles = {"A": [7], "B1": [7], "B2": [4, 5, 6, 7], "B3": list(range(8))}[u["grp"]]
                for (toff, kti, own_i) in ktiles:
                    b = proj_tile(wt, wkey, nkc, toff, 128)
                    s = qk_norm(b, nk, 1 if isA else 3, 128)
                    if own_i is not None and own_i in out_tiles:
                        r0 = (own_i - out_tiles[0]) * 128
                        cc = u["kvhead0"] * 128
                        P.op("sp", lambda e, s=s, r0=r0, cc=cc, nkc=nkc, grp=u["grp"]: e.dma_start(
                            out=kvp[grp][r0:r0 + 128, cc:cc + nkc], in_=zn[s][:, :nkc]),
                            reads=[("zn", s)], writes=[("kvp", u["name"], "k", own_i)], dma=True)
                    transpose_to(lambda h, s=s: zn[s][:, h * 128:(h + 1) * 128], nk, 128,
                                 KT[:, 0:nk, kti * 128:(kti + 1) * 128], ("KT", kti), [("zn", s)])
                if bis is not None and len(bis) > 2 and bis[2] == 3.5:
                    load_w(w_in, u["vcol"], nkc)
                    load_w(w_in, u["vcol"], nkc)
                    continue
                if bis is not None and len(bis) > 2 and bis[2] < 4:
                    continue
                vm = bis[3] if (bis is not None and len(bis) > 3) else 15
                if vm & 1:
                    P.op("dve", lambda e: e.tensor_copy(out=onesc[:, 0:2], in_=flag_t[:, 0:2]), reads=["flag"], writes=["onesc"])
                wt, wkey = load_w(w_in, u["vcol"], nkc)
                for (toff, kti, own_i) in ktiles:
                    b = proj_tile(wt, wkey, nkc, toff, 128)
                    is_out = own_i is not None and own_i in out_tiles
                    if not is_out:
                        P.op("dve", lambda e, b=b, kti=kti, nk=nk, nkc=nkc: e.tensor_copy(
                            out=VA[:, kti, 0:nk, :], in_=pz[:, b, :nkc].rearrange("p (h d) -> p h d", d=128)),
                            reads=[("pz", b)], writes=[("VA", kti)])
                    else:
                        s = cnt["vf"] % 2
                        cnt["vf"] += 1
                        r0 = (own_i - out_tiles[0]) * 128
                        cc = W + u["kvhead0"] * 128
                        P.op("dve", lambda e, b=b, s=s, nkc=nkc: e.tensor_copy(out=vf[s][:, :nkc], in_=pz[:, b, :nkc]),
                             reads=[("pz", b)], writes=[("vf", s)])
                        P.op("pool", lambda e, s=s, kti=kti, nk=nk, nkc=nkc: e.tensor_copy(
                            out=VA[:, kti, 0:nk, :], in_=vf[s][:, :nkc].rearrange("p (h d) -> p h d", d=128)),
                            reads=[("vf", s)], writes=[("VA", kti)])
                        P.op("sp", lambda e, s=s, r0=r0, cc=cc, nkc=nkc, grp=u["grp"]: e.dma_start(
                            out=kvp[grp][r0:r0 + 128, cc:cc + nkc], in_=vf[s][:, :nkc]),
                            reads=[("vf", s)], writes=[("kvp", u["name"], "v", own_i)], dma=True)
                for qt in range(NT):
                    if bis is not None and bis[1] == 0:
                        break
                    deltas = [dl for dl in range(nd) if qt - dl >= -nh]
                    ps_ = cnt["pm"] % 2
                    cnt["pm"] += 1
                    for di, dl in enumerate(deltas):
                        kti = qt - dl + nh
                        var, biases = vmap[dl]
                        sb_ = cnt["E"] % 2
                        cnt["E"] += 1
                        for k in range(nk):
                            P.op("pe", lambda e, k=k, kti=kti, sb_=sb_, qt=qt, gq=gq: e.matmul(
                                pst[:, sb_, k * gq * 128:(k + 1) * gq * 128], lhsT=KT[:, k, kti * 128:(kti + 1) * 128],
                                rhs=QT[:, k * gq:(k + 1) * gq, qt * 128:(qt + 1) * 128], start=True, stop=True),
                                reads=[("KT", kti), ("QT", qt)], writes=[("pst", sb_)])
                        if all(bv == 0.0 for bv in biases):
                            P.op("act", lambda e, sb_=sb_, nq=nq: e.activation(out=Eb[sb_][:, :nq * 128], in_=pst[:, sb_, :nq * 128],
                                                                             func=AF.Exp, scale=SCALE),
                                 reads=[("pst", sb_)], writes=[("Eb", sb_)])
                        else:
                            for j in range(nq):
                                P.op("act", lambda e, sb_=sb_, j=j, bv=biases[j]: e.activation(
                                    out=Eb[sb_][:, j * 128:(j + 1) * 128], in_=pst[:, sb_, j * 128:(j + 1) * 128],
                                    func=AF.Exp, scale=SCALE, bias=bv),
                                    reads=[("pst", sb_)], writes=[("Eb", sb_)])
                        P.op("dve", lambda e, sb_=sb_, ps_=ps_, di=di, var=var, nq=nq, nvar=nvar: e.tensor_tensor(
                            out=Pm[ps_][:, di * nq * 128:(di + 1) * nq * 128].rearrange("p (j q) -> p j q", q=128),
                            in0=Eb[sb_][:, :nq * 128].rearrange("p (j q) -> p j q", q=128),
                            in1=tabt[:, :nq * nvar * 128].rearrange("p (j v q) -> p j v q", v=nvar, q=128)[:, :, var, :],
                            op=ALU.mult),
                            reads=[("Eb", sb_), "tabt"], writes=[("Pm", ps_, di)])
                    for j in range(nq):
                        for di, dl in enumerate(deltas):
                            kti = qt - dl + nh
                            P.op("pe", lambda e, j=j, di=di, kti=kti, ps_=ps_, gq=gq, nq=nq, first=(di == 0), last=(di == len(deltas) - 1): e.matmul(
                                po2[:, j * 128:(j + 1) * 128], lhsT=Pm[ps_][:, (di * nq + j) * 128:(di * nq + j + 1) * 128], rhs=VA[:, kti, j // gq, :],
                                start=first, stop=last),
                                reads=[("Pm", ps_, di), ("VA", kti)], writes=["po_num"])
                    for j in range(nq):
                        for di, dl in enumerate(deltas):
                            kti = qt - dl + nh
                            oc = 1 if kti < nh else 0
                            P.op("pe", lambda e, j=j, di=di, oc=oc, ps_=ps_, nq=nq, first=(di == 0), last=(di == len(deltas) - 1): e.matmul(
                                po2[:, 512 + j:513 + j], lhsT=Pm[ps_][:, (di * nq + j) * 128:(di * nq + j + 1) * 128], rhs=onesc[:, oc:oc + 1],
                                start=first, stop=last),
                                reads=[("Pm", ps_, di), "onesc"], writes=["po_den"])
                    if isA:
                        so = cnt["ot"] % 2
                        cnt["ot"] += 1
                        h0 = u["ohead0"]
                        P.op("dve", lambda e, so=so, h0=h0: e.tensor_tensor(out=rc[so][:, 0:4], in0=po2[:, 512:516], in1=esink[:, h0:h0 + 4], op=ALU.add),
                             reads=["po_den", "esink"], writes=[("rc", so)])
                        P.op("dve", lambda e, so=so: e.reciprocal(out=rc[so][:, 0:4], in_=rc[so][:, 0:4]), reads=[("rc", so)], writes=[("rc", so)])
                        P.op("dve", lambda e, so=so: e.tensor_tensor(out=ot[so][:], in0=po2[:, 0:512].rearrange("p (h d) -> p h d", d=128),
                                                                    in1=rc[so][:, 0:4].unsqueeze(2).to_broadcast([128, 4, 128]), op=ALU.mult),
                             reads=["po_num", ("rc", so)], writes=[("ot", so)])
                        transpose_to(lambda h, so=so: ot[so][:, h, :], 4, 128, oaT[:, h0:h0 + 4, qt * 128:(qt + 1) * 128],
                                     ("oaT", h0, qt), [("ot", so)])
                    else:
                        h0 = u["ohead0"]
                        if u["grp"] == "B1":
                            P.op("dve", lambda e, h0=h0, qt=qt: e.tensor_copy(out=accBv[:, qt, h0:h0 + 2, :], in_=po2[:, 0:256].rearrange("p (h d) -> p h d", d=128)),
                                 reads=["po_num"], writes=[("accBv", qt, h0)])
                            P.op("dve", lambda e, h0=h0, qt=qt: e.tensor_copy(out=accBd[:, qt, h0:h0 + 2], in_=po2[:, 512:514]),
                                 reads=["po_den"], writes=[("accBd", qt, h0)])
                        else:
                            P.op("dve", lambda e, h0=h0, qt=qt: e.tensor_tensor(out=accBv[:, qt, h0:h0 + 2, :], in0=po2[:, 0:256].rearrange("p (h d) -> p h d", d=128),
                                                                               in1=accBv[:, qt, h0:h0 + 2, :], op=ALU.add),
                                 reads=["po_num", ("accBv", qt, h0)], writes=[("accBv", qt, h0)])
                            P.op("dve", lambda e, h0=h0, qt=qt: e.tensor_tensor(out=accBd[:, qt, h0:h0 + 2], in0=po2[:, 512:514],
                                                                               in1=accBd[:, qt, h0:h0 + 2], op=ALU.add),
                                 reads=["po_den", ("accBd", qt, h0)], writes=[("accBd", qt, h0)])
            for qt in range(NT):
                if bis is not None:
                    break
                so = cnt["ot"] % 2
                cnt["ot"] += 1
                P.op("dve", lambda e, so=so, qt=qt: e.reciprocal(out=rc[so][:, 0:4], in_=accBd[:, qt, :]),
                     reads=[("accBd", qt, 0), ("accBd", qt, 2)], writes=[("rc", so)])
                P.op("dve", lambda e, so=so, qt=qt: e.tensor_tensor(out=ot[so][:], in0=accBv[:, qt, :, :],
                                                                   in1=rc[so][:, 0:4].unsqueeze(2).to_broadcast([128, 4, 128]), op=ALU.mult),
                     reads=[("accBv", qt, 0), ("accBv", qt, 2), ("rc", so)], writes=[("ot", so)])
                transpose_to(lambda h, so=so: ot[so][:, h, :], 4, 128, obT[:, 0:4, qt * 128:(qt + 1) * 128], ("obT", qt), [("ot", so)])
            P.barrier()

        if stop_after == 2 or bis is not None:
            if debug and bis is None:
                P.op("sp", lambda e: e.dma_start(out=dbg["oaT"][:, :, 0:1024], in_=oaT[:, :, 0:1024]), reads=[], writes=["dbg1"], dma=True)
                P.op("sp", lambda e: e.dma_start(out=dbg["obT"][:, :, 0:1024], in_=obT[:, :, 0:1024]), reads=[], writes=["dbg2"], dma=True)
            P.barrier(); P.emit(); esA.close(); return nc

        with ExitStack() as es3:
            def sb3(name, shape, dt=F32):
                return es3.enter_context(nc.sbuf_tensor(name, list(shape), dt))
            qs = sb3("qs", [NS, 20 * 128]); ks = sb3("ks", [NS, 14 * 128]); vs = sb3("vs", [NS, 14 * 128])
            qsb = sb3("qsb", [NS, 20 * 128], BF16)
            sel = sb3("sel", [NS, NS, 128], BF16)
            kvc = sb3("kvc", [128, NS, 1024], BF16)
            qrep = sb3("qrep", [128, 1024], BF16)
            prod = sb3("prod", [128, 1024])
            Ssb = sb3("Ssb", [128, NS * 8]); S2 = sb3("S2", [128, NS * 8])
            Pb = sb3("Pb", [128, 8, NS], BF16)
            Pexp = sb3("Pexp", [128, 8, NS, NS], BF16)
            sbias_t = sb3("sbias_t", [128, 20]); dmask_t = sb3("dmask_t", [128, NS, NS])
            onesb = sb3("onesb", [128, 1], BF16)
            sq3 = sb3("sq3", [NS, 256]); st3 = [sb3(f"st3_{i}", [NS, 8]) for i in range(2)]
            lg = sb3("lg", [NS, 20]); enew = sb3("enew", [NS, 20])
            tmpv = sb3("tmpv", [NS, 8, 128])
            numA = sb3("numA", [NS, 8, 128]); denA = sb3("denA", [NS, 8])
            numB = sb3("numB", [NS, 4, 128]); denB = sb3("denB", [NS, 4])
            pst_f = pst[:].rearrange("p a b -> p (a b)")
            po_f = po[:].rearrange("p a b -> p (a b)")
            P.op("sp", lambda e: e.dma_start(out=sbias_t[:], in_=sbias_d), writes=["sbias"], dma=True)
            P.op("sp", lambda e: e.dma_start(out=dmask_t[:].rearrange("p a b -> p (a b)"), in_=dmask_d), writes=["dmask"], dma=True)
            P.op("pool", lambda e: e.memset(onesb[:], 1.0), writes=["onesb"])
            P.op("dve", lambda e: e.tensor_copy(out=sel[:], in_=ident[:NS, :NS].unsqueeze(2).to_broadcast([NS, NS, 128])),
                 reads=["ident"], writes=["sel"])
            for c in range(24):
                wt, wkey = load_w(w_in, c * 256, 256)
                b = c % 2
                for k in range(16):
                    P.op("pe", lambda e, k=k, b=b, wt=wt: e.matmul(pz[:NS, b, :256], lhsT=xnT[:, k, XT_S:XT_S + NS], rhs=wt[:, k, :256],
                                                                  start=(k == 0), stop=(k == 15)),
                         reads=[("xnT", XT_S), wkey], writes=[("pz", b)])
                if c < 4:
                    kind, dst, gi = "n", qs[:, c * 256:(c + 1) * 256], 0
                elif c == 4:
                    kind, dst, gi = "n", ks[:, 0:256], 1
                elif c == 5:
                    kind, dst, gi = "v", vs[:, 0:256], None
                elif c < 12:
                    kind, dst, gi = "n", qs[:, 1024 + (c - 6) * 256:1024 + (c - 5) * 256], 2
                elif c < 18:
                    kind, dst, gi = "n", ks[:, 256 + (c - 12) * 256:256 + (c - 11) * 256], 3
                else:
                    kind, dst, gi = "v", vs[:, 256 + (c - 18) * 256:256 + (c - 17) * 256], None
                dkey = ("sdst", c)
                if kind == "v":
                    P.op("act", lambda e, b=b, dst=dst: e.activation(out=dst, in_=pz[:NS, b, :256], func=AF.Copy),
                         reads=[("pz", b)], writes=[dkey, "svs"])
                else:
                    st = st3[c % 2]
                    skey = ("st3", c % 2)
                    P.op("act", lambda e, b=b: e.activation(out=sq3[:, :], in_=pz[:NS, b, :256], func=AF.Square), reads=[("pz", b)], writes=["sq3"])
                    P.op("dve", lambda e, st=st: e.tensor_reduce(out=st[:, 0:2], in_=sq3[:, :].rearrange("p (h d) -> p h d", d=128), axis=AX.X, op=ALU.add),
                         reads=["sq3"], writes=[skey])
                    P.op("act", lambda e, st=st: e.activation(out=st[:, 2:4], in_=st[:, 0:2], func=AF.Ln, scale=1.0 / 128, bias=EPS), reads=[skey], writes=[skey])
                    P.op("act", lambda e, st=st: e.activation(out=st[:, 4:6], in_=st[:, 2:4], func=AF.Exp, scale=-0.5), reads=[skey], writes=[skey])
                    for h in range(2):
                        P.op("dve", lambda e, h=h, b=b, st=st, dst=dst, gi=gi: e.scalar_tensor_tensor(
                            out=dst[:, h * 128:(h + 1) * 128], in0=pz[:NS, b, h * 128:(h + 1) * 128], scalar=st[:, 4 + h:5 + h],
                            in1=gains[:NS, gi, :], op0=ALU.mult, op1=ALU.mult),
                            reads=[("pz", b), skey, ("gains", gi)], writes=[dkey, "sqk"])
            P.op("dve", lambda e: e.tensor_copy(out=qsb[:], in_=qs[:]), reads=["sqk"], writes=["qsb"])
            P.op("dve", lambda e: e.tensor_tensor(out=prod[:NS, 0:1024].rearrange("p (k g d) -> p k g d", k=2, g=4),
                                                  in0=qs[:, 0:1024].rearrange("p (k g d) -> p k g d", k=2, g=4),
                                                  in1=ks[:, 0:256].rearrange("p (k d) -> p k d", k=2).unsqueeze(2).to_broadcast([NS, 2, 4, 128]),
                                                  op=ALU.mult), reads=["sqk"], writes=["prod"])
            P.op("dve", lambda e: e.tensor_reduce(out=lg[:, 0:8], in_=prod[:NS, 0:1024].rearrange("p (h d) -> p h d", d=128), axis=AX.X, op=ALU.add),
                 reads=["prod"], writes=["lg"])
            for (c0, c1) in ((1024, 2048), (2048, 2560)):
                n_ = c1 - c0
                P.op("dve", lambda e, c0=c0, c1=c1, n_=n_: e.tensor_tensor(out=prod[:NS, 0:n_], in0=qs[:, c0:c1], in1=ks[:, c0 - 768:c1 - 768], op=ALU.mult),
                     reads=["sqk"], writes=["prod"])
                P.op("dve", lambda e, c0=c0, c1=c1, n_=n_: e.tensor_reduce(out=lg[:, c0 // 128:c1 // 128], in_=prod[:NS, 0:n_].rearrange("p (h d) -> p h d", d=128),
                                                                      axis=AX.X, op=ALU.add),
                     reads=["prod"], writes=["lg"])
            P.op("act", lambda e: e.activation(out=enew[:], in_=lg[:], func=AF.Exp, scale=SCALE), reads=["lg"], writes=["enew"])
            for gi_, gname in enumerate(("A", "B1", "B2", "B3")):
                G = GROUPS[gname]
                H, L, dil = G["H"], G["L"], G["dil"]
                W = 2 * H * 128
                isA = gname == "A"
                hq = 8 if isA else 4
                hb = 0 if isA else 8 + 4 * (gi_ - 1)
                kb = 0 if isA else 2 + 4 * (gi_ - 1)
                qoff = hb * 128
                csrc = bass.AP(caches[gname].tensor, 0, [[dil * W, 128], [L * W, NS], [1, W]])
                P.op("pool", lambda e, csrc=csrc, W=W: e.dma_start(out=kvc[:, :, :W], in_=csrc), writes=["kvc"], dma=True)
                P.op("sp", lambda e, gname=gname, L=L, H=H, kb=kb: e.dma_start(out=kvs[gname][:, L - 1, 0:H * 128], in_=ks[:, kb * 128:(kb + H) * 128]),
                     reads=["sqk"], writes=[("kvs_newk", gname)], dma=True)
                P.op("sp", lambda e, gname=gname, L=L, H=H, kb=kb: e.dma_start(out=kvs[gname][:, L - 1, H * 128:2 * H * 128], in_=vs[:, kb * 128:(kb + H) * 128]),
                     reads=["svs"], writes=[("kvs_newv", gname)], dma=True)
                tc_ = 1 if isA else 2
                for t0 in range(0, NS, tc_):
                    for j in range(tc_):
                        for half in range(hq * 128 // 512):
                            P.op("pe", lambda e, t0=t0, j=j, half=half, hq=hq, qoff=qoff: e.matmul(
                                pst_f[:, j * hq * 128 + half * 512:j * hq * 128 + (half + 1) * 512], lhsT=sel[:, t0 + j, :],
                                rhs=qsb[:, qoff + half * 512:qoff + (half + 1) * 512], start=True, stop=True),
                                reads=["sel", "qsb"], writes=["pstf"])
                    P.op("act", lambda e: e.activation(out=qrep[:], in_=pst_f, func=AF.Copy), reads=["pstf"], writes=["qrep"])
                    if isA:
                        P.op("dve", lambda e, t0=t0: e.tensor_tensor(
                            out=prod[:].rearrange("p (k g d) -> p k g d", k=2, g=4),
                            in0=kvc[:, t0, 0:256].rearrange("p (k d) -> p k d", k=2).unsqueeze(2).to_broadcast([128, 2, 4, 128]),
                            in1=qrep[:].rearrange("p (k g d) -> p k g d", k=2, g=4), op=ALU.mult),
                            reads=["kvc", "qrep"], writes=["prod"])
                    else:
                        P.op("dve", lambda e, t0=t0: e.tensor_tensor(
                            out=prod[:].rearrange("p (t c) -> p t c", t=2), in0=kvc[:, t0:t0 + 2, 0:512],
                            in1=qrep[:].rearrange("p (t c) -> p t c", t=2), op=ALU.mult),
                            reads=["kvc", "qrep"], writes=["prod"])
                    P.op("dve", lambda e, t0=t0, hq=hq, tc_=tc_: e.tensor_reduce(
                        out=Ssb[:, t0 * hq:(t0 + tc_) * hq], in_=prod[:].rearrange("p (h d) -> p h d", d=128), axis=AX.X, op=ALU.add),
                        reads=["prod"], writes=["Ssb"])
                P.op("dve", lambda e, hq=hq, hb=hb: e.scalar_tensor_tensor(
                    out=S2[:, :NS * hq].rearrange("p (t h) -> p t h", h=hq), in0=Ssb[:, :NS * hq].rearrange("p (t h) -> p t h", h=hq),
                    scalar=SCALE, in1=sbias_t[:, hb:hb + hq].unsqueeze(1).to_broadcast([128, NS, hq]), op0=ALU.mult, op1=ALU.add),
                    reads=["Ssb", "sbias"], writes=["S2"])
                P.op("act", lambda e, hq=hq: e.activation(out=Pb[:, 0:hq, :], in_=S2[:, :NS * hq].rearrange("p (t h) -> p h t", h=hq), func=AF.Exp),
                     reads=["S2"], writes=["Pb"])
                P.op("dve", lambda e, hq=hq: e.tensor_tensor(
                    out=Pexp[:, 0:hq, :, :], in0=Pb[:, 0:hq, :].unsqueeze(2).to_broadcast([128, hq, NS, NS]),
                    in1=dmask_t[:].unsqueeze(1).to_broadcast([128, hq, NS, NS]), op=ALU.mult),
                    reads=["Pb", "dmask"], writes=["Pexp"])
                for h in range(hq):
                    kvh = h // 4 if isA else h
                    for tp in range(NS):
                        P.op("pe", lambda e, h=h, tp=tp, kvh=kvh, H=H: e.matmul(
                            po_f[:NS, h * 128:(h + 1) * 128], lhsT=Pexp[:, h, tp, :], rhs=kvc[:, tp, (H + kvh) * 128:(H + kvh + 1) * 128],
                            start=(tp == 0), stop=(tp == NS - 1)),
                            reads=["Pexp", "kvc"], writes=["pof"])
                    P.op("pe", lambda e, h=h: e.matmul(pz[:NS, 1, h:h + 1], lhsT=Pb[:, h, :], rhs=onesb[:, 0:1], start=True, stop=True),
                         reads=["Pb", "onesb"], writes=[("pz", 1)])
                if isA:
                    P.op("dve", lambda e: e.tensor_tensor(
                        out=tmpv[:].rearrange("p (k g) d -> p k g d", k=2),
                        in0=vs[:, 0:256].rearrange("p (k d) -> p k d", k=2).unsqueeze(2).to_broadcast([NS, 2, 4, 128]),
                        in1=enew[:, 0:8].rearrange("p (k g) -> p k g", k=2).unsqueeze(3).to_broadcast([NS, 2, 4, 128]), op=ALU.mult),
                        reads=["svs", "enew"], writes=["tmpv"])
                    P.op("dve", lambda e: e.tensor_tensor(out=numA[:], in0=po_f[:NS, 0:1024].rearrange("p (h d) -> p h d", d=128), in1=tmpv[:], op=ALU.add),
                         reads=["pof", "tmpv"], writes=["numA"])
                    P.op("dve", lambda e: e.tensor_tensor(out=denA[:], in0=pz[:NS, 1, 0:8], in1=enew[:, 0:8], op=ALU.add),
                         reads=[("pz", 1), "enew"], writes=["denA"])
                    P.op("dve", lambda e: e.tensor_tensor(out=denA[:], in0=denA[:], in1=esink[:NS, 0:8], op=ALU.add),
                         reads=["denA", "esink"], writes=["denA"])
                else:
                    P.op("dve", lambda e, kb=kb, hb=hb: e.tensor_tensor(
                        out=tmpv[:, 0:4, :], in0=vs[:, kb * 128:(kb + 4) * 128].rearrange("p (h d) -> p h d", d=128),
                        in1=enew[:, hb:hb + 4].unsqueeze(2).to_broadcast([NS, 4, 128]), op=ALU.mult),
                        reads=["svs", "enew"], writes=["tmpv"])
                    if gname == "B1":
                        P.op("dve", lambda e: e.tensor_tensor(out=numB[:], in0=po_f[:NS, 0:512].rearrange("p (h d) -> p h d", d=128), in1=tmpv[:, 0:4, :], op=ALU.add),
                             reads=["pof", "tmpv"], writes=["numB"])
                        P.op("dve", lambda e, hb=hb: e.tensor_tensor(out=denB[:], in0=pz[:NS, 1, 0:4], in1=enew[:, hb:hb + 4], op=ALU.add),
                             reads=[("pz", 1), "enew"], writes=["denB"])
                    else:
                        P.op("dve", lambda e: e.tensor_tensor(out=numB[:], in0=numB[:], in1=tmpv[:, 0:4, :], op=ALU.add), reads=["numB", "tmpv"], writes=["numB"])
                        P.op("dve", lambda e: e.tensor_tensor(out=numB[:], in0=po_f[:NS, 0:512].rearrange("p (h d) -> p h d", d=128), in1=numB[:], op=ALU.add),
                             reads=["pof", "numB"], writes=["numB"])
                        P.op("dve", lambda e, hb=hb: e.tensor_tensor(out=denB[:], in0=denB[:], in1=enew[:, hb:hb + 4], op=ALU.add), reads=["denB", "enew"], writes=["denB"])
                        P.op("dve", lambda e: e.tensor_tensor(out=denB[:], in0=pz[:NS, 1, 0:4], in1=denB[:], op=ALU.add), reads=[("pz", 1), "denB"], writes=["denB"])
            for (num, den, nh_, dstT, nm) in ((numA, denA, 8, oaT, "oaTs"), (numB, denB, 4, obT, "obTs")):
                P.op("dve", lambda e, den=den: e.reciprocal(out=den[:], in_=den[:]), reads=["denA", "denB"], writes=["denA", "denB"])
                P.op("dve", lambda e, num=num, den=den, nh_=nh_: e.tensor_tensor(out=num[:], in0=num[:], in1=den[:].unsqueeze(2).to_broadcast([NS, nh_, 128]), op=ALU.mult),
                     reads=["numA", "numB", "denA", "denB"], writes=["numA", "numB"])
                for h0 in range(0, nh_, 4):
                    for h in range(4):
                        P.op("pe", lambda e, num=num, h0=h0, h=h: e.transpose(out=ptr[:, 0, h * 128:h * 128 + NS], in_=num[:, h0 + h, :], identity=ident[:NS, :NS]),
                             reads=["numA", "numB", "ident"], writes=[("ptr", 0)])
                    P.op("act", lambda e, dstT=dstT, h0=h0: e.activation(
                        out=dstT[:, h0:h0 + 4, 1024:1024 + NS], in_=ptr[:, 0, :].rearrange("p (a b) -> p a b", b=128)[:, :, :NS], func=AF.Copy),
                        reads=[("ptr", 0)], writes=[(nm, h0)])
            P.barrier()

        if debug:
            P.op("sp", lambda e: e.dma_start(out=dbg["oaT"], in_=oaT[:]), reads=[], writes=["dbg1"], dma=True)
            P.op("sp", lambda e: e.dma_start(out=dbg["obT"], in_=obT[:]), reads=[], writes=["dbg2"], dma=True)
        if stop_after == 3:
            P.barrier(); P.emit(); esA.close(); return nc
        esR = ExitStack()
        mT = esR.enter_context(nc.sbuf_tensor("mT", [128, 16, 1040], BF16, side="right"))
        pst_f = pst[:].rearrange("p a b -> p (a b)")
        po_f = po[:].rearrange("p a b -> p (a b)")
        with ExitStack() as es4:
            def sb4(name, shape, dt=F32):
                return es4.enter_context(nc.sbuf_tensor(name, list(shape), dt))
            wr4 = [sb4(f"wr4_{i}", [128, 16, 256], BF16) for i in range(2)]
            ring[:] = list(wring) + wr4
            sga = [sb4(f"sga{i}", [128, 512]) for i in range(2)]
            sgb = [sb4(f"sgb{i}", [128, 512]) for i in range(2)]
            t1 = [sb4(f"t1_{i}", [128, 512]) for i in range(2)]
            t2 = [sb4(f"t2_{i}", [128, 512]) for i in range(2)]
            psets = [(pz[:, 0, :], pz[:, 1, :], pst[:, 0, :], pst[:, 1, :], ("pz", 0), ("pz", 1), ("pst", 0), ("pst", 1)),
                     (ptr[:, 0, :], ptr[:, 1, :], po_f[:, 0:512], po_f[:, 512:1024], ("ptr", 0), ("ptr", 1), ("pof", 0), ("pof", 1))]
            blocks = [(XT_O, 0, 512), (XT_O + 512, 512, 512), (XT_S, 1024, NS)]
            it = 0
            for c in range(8):
                wga, kga = load_w(w_in, 6144 + c * 256, 256)
                wgb, kgb = load_w(w_in, 8192 + c * 256, 256)
                wa, ka_ = load_w(wba, c * 256, 256, nk=8)
                wb_, kb_ = load_w(wbb, c * 256, 256, nk=4)
                for j in range(2):
                    dm = c * 2 + j
                    for (toff, ooff, n) in blocks:
                        s_ = it % 2
                        it += 1
                        GA, GB, YA, YB, kGA, kGB, kYA, kYB = psets[s_]
                        for (dst, dkey, wt_, wk_, nk_, src, skey_) in ((GA, kGA, wga, kga, 16, lambda k, toff=toff, n=n: xnT[:, k, toff:toff + n], "xn"),
                                                                       (GB, kGB, wgb, kgb, 16, lambda k, toff=toff, n=n: xnT[:, k, toff:toff + n], "xn"),
                                                                       (YA, kYA, wa, ka_, 8, lambda k, ooff=ooff, n=n: oaT[:, k, ooff:ooff + n], "oa"),
                                                                       (YB, kYB, wb_, kb_, 4, lambda k, ooff=ooff, n=n: obT[:, k, ooff:ooff + n], "ob")):
                            for k in range(nk_):
                                P.op("pe", lambda e, dst=dst, wt_=wt_, k=k, j=j, src=src, nk_=nk_, n=n: e.matmul(
                                    dst[:, :n], lhsT=wt_[:, k, j * 128:(j + 1) * 128], rhs=src(k), start=(k == 0), stop=(k == nk_ - 1)),
                                    reads=[wk_], writes=[dkey])
                        P.op("act", lambda e, GA=GA, s_=s_, n=n: e.activation(out=sga[s_][:, :n], in_=GA[:, :n], func=AF.Sigmoid), reads=[kGA], writes=[("sga", s_)])
                        P.op("act", lambda e, GB=GB, s_=s_, n=n: e.activation(out=sgb[s_][:, :n], in_=GB[:, :n], func=AF.Sigmoid), reads=[kGB], writes=[("sgb", s_)])
                        P.op("dve", lambda e, YA=YA, s_=s_, n=n: e.tensor_tensor(out=t1[s_][:, :n], in0=YA[:, :n], in1=sga[s_][:, :n], op=ALU.mult),
                             reads=[kYA, ("sga", s_)], writes=[("t1", s_)])
                        P.op("dve", lambda e, YB=YB, s_=s_, n=n: e.tensor_tensor(out=t2[s_][:, :n], in0=YB[:, :n], in1=sgb[s_][:, :n], op=ALU.mult),
                             reads=[kYB, ("sgb", s_)], writes=[("t2", s_)])
                        P.op("dve", lambda e, s_=s_, n=n, dm=dm, ooff=ooff: e.tensor_tensor(out=mT[:, dm, ooff:ooff + n], in0=t1[s_][:, :n], in1=t2[s_][:, :n], op=ALU.add),
                             reads=[("t1", s_), ("t2", s_)], writes=[("mT", dm, ooff)])
            P.barrier()
        esA.close()

        es5 = ExitStack()
        def sb5(name, shape, dt=F32):
            return es5.enter_context(nc.sbuf_tensor(name, list(shape), dt))
        h_t = sb5("h_t", [128, 9, D])
        with ExitStack() as es5b:
            def sb5b(name, shape, dt=F32):
                return es5b.enter_context(nc.sbuf_tensor(name, list(shape), dt))
            ring[:] = [sb5b(f"wr5_{i}", [128, 16, 256], BF16) for i in range(2)]
            xblk = [sb5b(f"xblk{i}", [128, 256]) for i in range(3)]
            it = 0
            for c in range(8):
                wt, wkey = load_w(wout, c * 256, 256)
                for i in range(9):
                    n = 128 if i < 8 else NS
                    ooff = i * 128
                    b = it % 2
                    xs_ = it % 3
                    it += 1
                    srcx = xo[i * 128:(i + 1) * 128, c * 256:(c + 1) * 256] if i < 8 else xs[:, c * 256:(c + 1) * 256]
                    P.op("sp", lambda e, xs_=xs_, srcx=srcx, n=n: e.dma_start(out=xblk[xs_][:n, :], in_=srcx), writes=[("xblk", xs_)], dma=True)
                    for k in range(16):
                        P.op("pe", lambda e, k=k, b=b, n=n, ooff=ooff, wt=wt: e.matmul(pz[:n, b, :256], lhsT=mT[:, k, ooff:ooff + n], rhs=wt[:, k, :256],
                                                                                  start=(k == 0), stop=(k == 15)),
                             reads=[wkey], writes=[("pz", b)])
                    P.op("dve", lambda e, b=b, n=n, i=i, c=c, xs_=xs_: e.tensor_tensor(out=h_t[:n, i, c * 256:(c + 1) * 256], in0=pz[:n, b, :256],
                                                                                    in1=xblk[xs_][:n, :], op=ALU.add),
                         reads=[("pz", b), ("xblk", xs_)], writes=[("h", i)])
            P.barrier()
        esR.close()
        if debug:
            P.op("sp", lambda e: e.dma_start(out=yp, in_=h_t[:, 0:8, :].rearrange("p i d -> p i d")) if False else e.dma_start(out=yp.rearrange("(i p) d -> p i d", p=128), in_=h_t[:, 0:8, :]),
                 reads=[], writes=["dbgh"], dma=True)
            P.op("sp", lambda e: e.dma_start(out=ys, in_=h_t[:NS, 8, :]), reads=[], writes=["dbghs"], dma=True)
            P.barrier(); P.emit(); es5.close(); return nc

        with ExitStack() as es6:
            def sb6(name, shape, dt=F32):
                return es6.enter_context(nc.sbuf_tensor(name, list(shape), dt))
            wqb = sb6("wqb", [128, 16, 1024], BF16)
            w2rep = sb6("w2rep", [128, D])
            skb = sb6("skb", [128, 256])
            iota16 = sb6("iota16t", [128, 16]); thr16 = sb6("thr16", [128, 16])
            hn = sb6("hn", [128, D])
            hnT = sb6("hnT", [128, 16, 128], BF16)
            qf = sb6("qf", [128, 1024])
            qT = sb6("qT", [128, 8, 128])
            sS = sb6("sS", [128, 2048])
            s2 = sb6("s2", [128, 256])
            vals = sb6("vals", [128, 16, 16]); idxu = sb6("idxu", [128, 16, 16], U32); idxf = sb6("idxf", [128, 16, 16])
            cand = sb6("cand", [128, 8, 256])
            best = sb6("best", [128, 8, 16]); bcu = sb6("bcu", [128, 8, 16], U32); bcf = sb6("bcf", [128, 128])
            akf = sb6("akf", [128, 128]); bkf = sb6("bkf", [128, 128])
            eq = sb6("eq", [128, 128, 16])
            i1f = sb6("i1f", [128, 128]); i2f = sb6("i2f", [128, 128]); ef = sb6("ef", [128, 128]); eidx = sb6("eidx", [128, 128], U32)
            gate = sb6("gate", [128, 8, 16]); gsum = sb6("gsum", [128, 8])
            act_ = sb6("act_", [128, 128]); wgt = sb6("wgt", [128, 128])
            ubuf = [sb6(f"ubuf{i}", [128, D]) for i in range(3)]
            junk6 = sb6("junk6", [128, D], BF16)
            st6 = sb6("st6", [128, 8])
            for c4 in range(4):
                P.op("pool", lambda e, c4=c4: e.dma_start(out=wqb[:, :, c4 * 256:(c4 + 1) * 256],
                                                         in_=wq[:, c4 * 256:(c4 + 1) * 256].rearrange("(k p) n -> p k n", p=128)),
                     writes=[("wqb", c4)], dma=True)
            P.op("sp", lambda e: e.dma_start(out=w2rep[:], in_=norm2_w.partition_broadcast(128)), writes=["w2rep"], dma=True)
            P.op("sp", lambda e: e.dma_start(out=iota16[:], in_=iota16_d), writes=["iota16"], dma=True)
            P.op("dve", lambda e: e.tensor_scalar(out=thr16[:], in0=iota16[:], scalar1=16.0, scalar2=None, op0=ALU.mult), reads=["iota16"], writes=["thr16"])
            P.op("dve", lambda e: e.memset(skb[:], 0.0), writes=["skb"])
            P.op("sp", lambda e: e.dma_start(out=skb[0:64, 0:128], in_=subkT[0]), reads=["skb"], writes=["skb"], dma=True)
            P.op("sp", lambda e: e.dma_start(out=skb[64:128, 128:256], in_=subkT[1]), reads=["skb"], writes=["skb"], dma=True)
            ucnt = [0]
            for i in range(9):
                n = 128 if i < 8 else NS
                hi = h_t[:n, i, :]
                hk = ("h", i)
                P.op("act", lambda e, hi=hi, n=n: e.activation(out=junk6[:n, :], in_=hi, func=AF.Square, accum_out=st6[:n, 0:1]), reads=[hk], writes=["junk6", "st6"])
                P.op("act", lambda e, n=n: e.activation(out=st6[:n, 1:2], in_=st6[:n, 0:1], func=AF.Ln, scale=1.0 / D, bias=EPS), reads=["st6"], writes=["st6"])
                P.op("act", lambda e, n=n: e.activation(out=st6[:n, 2:3], in_=st6[:n, 1:2], func=AF.Exp, scale=-0.5), reads=["st6"], writes=["st6"])
                P.op("dve", lambda e, hi=hi, n=n: e.scalar_tensor_tensor(out=hn[:n, :], in0=hi, scalar=st6[:n, 2:3], in1=w2rep[:n, :], op0=ALU.mult, op1=ALU.mult),
                     reads=[hk, "st6", "w2rep"], writes=["hn"])
                for k4 in range(4):
                    bank = k4 % 2
                    for kk in range(4):
                        k = k4 * 4 + kk
                        P.op("pe", lambda e, k=k, kk=kk, bank=bank, n=n: e.transpose(out=ptr[:, bank, kk * 128:kk * 128 + n], in_=hn[:n, k * 128:(k + 1) * 128],
                                                                                     identity=ident[:n, :n]), reads=["hn", "ident"], writes=[("ptr", bank)])
                    P.op("act", lambda e, k4=k4, bank=bank, n=n: e.activation(out=hnT[:, k4 * 4:(k4 + 1) * 4, :n],
                                                                            in_=ptr[:, bank, :].rearrange("p (a b) -> p a b", b=128)[:, :, :n], func=AF.Copy),
                         reads=[("ptr", bank)], writes=["hnT"])
                for half in range(2):
                    for k in range(16):
                        P.op("pe", lambda e, k=k, half=half, n=n: e.matmul(pz[:n, half, :], lhsT=hnT[:, k, :n], rhs=wqb[:, k, half * 512:(half + 1) * 512],
                                                                        start=(k == 0), stop=(k == 15)),
                             reads=["hnT"] + [("wqb", c4) for c4 in range(4)], writes=[("pz", half)])
                P.op("act", lambda e, n=n: e.activation(out=qf[:n, :].rearrange("p (a b) -> p a b", a=2), in_=pz[:n, :, :], func=AF.Copy),
                     reads=[("pz", 0), ("pz", 1)], writes=["qf"])
                for h4 in range(2):
                    for hh in range(4):
                        h = h4 * 4 + hh
                        P.op("pe", lambda e, h=h, hh=hh, h4=h4, n=n: e.transpose(out=ptr[:, h4, hh * 128:hh * 128 + n], in_=qf[:n, h * 128:(h + 1) * 128],
                                                                                identity=ident[:n, :n]), reads=["qf", "ident"], writes=[("ptr", h4)])
                    P.op("act", lambda e, h4=h4, n=n: e.activation(out=qT[:, h4 * 4:(h4 + 1) * 4, :n],
                                                                 in_=ptr[:, h4, :].rearrange("p (a b) -> p a b", b=128)[:, :, :n], func=AF.Copy),
                         reads=[("ptr", h4)], writes=["qT"])
                for h in range(8):
                    dst = pst_f[:n, h * 256:(h + 1) * 256] if h < 4 else po_f[:n, (h - 4) * 256:(h - 3) * 256]
                    P.op("pe", lambda e, h=h, dst=dst, n=n: e.matmul(dst, lhsT=qT[:, h, :n], rhs=skb[:, :], start=True, stop=True),
                         reads=["qT", "skb"], writes=["pstf" if h < 4 else "pof"])
                P.op("act", lambda e, n=n: e.activation(out=sS[:n, 0:1024], in_=pst_f[:n, :], func=AF.Copy), reads=["pstf"], writes=["sS"])
                P.op("dve", lambda e, n=n: e.tensor_copy(out=sS[:n, 1024:2048], in_=po_f[:n, :]), reads=["pof"], writes=["sS"])
                for hc in range(16):
                    src = sS[:n, hc * 128:(hc + 1) * 128]
                    P.op("dve", lambda e, src=src, hc=hc, n=n: e.max(out=vals[:n, hc, 0:8], in_=src), reads=["sS"], writes=["vals"])
                    P.op("dve", lambda e, src=src, hc=hc, n=n: e.max_index(out=idxu[:n, hc, 0:8], in_max=vals[:n, hc, 0:8], in_values=src),
                         reads=["sS", "vals"], writes=["idxu"])
                    P.op("dve", lambda e, src=src, hc=hc, n=n: e.match_replace(out=s2[:n, 0:128], in_to_replace=vals[:n, hc, 0:8], in_values=src, imm_value=-1e30),
                         reads=["sS", "vals"], writes=["s2"])
                    P.op("dve", lambda e, hc=hc, n=n: e.max(out=vals[:n, hc, 8:16], in_=s2[:n, 0:128]), reads=["s2"], writes=["vals"])
                    P.op("dve", lambda e, hc=hc, n=n: e.max_index(out=idxu[:n, hc, 8:16], in_max=vals[:n, hc, 8:16], in_values=s2[:n, 0:128]),
                         reads=["s2", "vals"], writes=["idxu"])
                P.op("dve", lambda e, n=n: e.tensor_copy(out=idxf[:n], in_=idxu[:n]), reads=["idxu"], writes=["idxf"])
                v4 = vals[:n].rearrange("p (h c) k -> p h c k", c=2)
                P.op("dve", lambda e, v4=v4, n=n: e.tensor_tensor(out=cand[:n].rearrange("p h (a b) -> p h a b", b=16),
                                                                 in0=v4[:, :, 0, :].unsqueeze(3).to_broadcast([n, 8, 16, 16]),
                                                                 in1=v4[:, :, 1, :].unsqueeze(2).to_broadcast([n, 8, 16, 16]), op=ALU.add),
                     reads=["vals"], writes=["cand"])
                for h in range(8):
                    src = cand[:n, h, :]
                    P.op("dve", lambda e, src=src, h=h, n=n: e.max(out=best[:n, h, 0:8], in_=src), reads=["cand"], writes=["best"])
                    P.op("dve", lambda e, src=src, h=h, n=n: e.max_index(out=bcu[:n, h, 0:8], in_max=best[:n, h, 0:8], in_values=src),
                         reads=["cand", "best"], writes=["bcu"])
                    P.op("dve", lambda e, src=src, h=h, n=n: e.match_replace(out=s2[:n, :], in_to_replace=best[:n, h, 0:8], in_values=src, imm_value=-1e30),
                         reads=["cand", "best"], writes=["s2"])
                    P.op("dve", lambda e, h=h, n=n: e.max(out=best[:n, h, 8:16], in_=s2[:n, :]), reads=["s2"], writes=["best"])
                    P.op("dve", lambda e, h=h, n=n: e.max_index(out=bcu[:n, h, 8:16], in_max=best[:n, h, 8:16], in_values=s2[:n, :]),
                         reads=["s2", "best"], writes=["bcu"])
                P.op("dve", lambda e, n=n: e.tensor_copy(out=bcf[:n, :], in_=bcu[:n].rearrange("p h k -> p (h k)")), reads=["bcu"], writes=["bcf"])
                P.op("dve", lambda e, n=n: e.tensor_tensor(out=eq[:n], in0=bcf[:n, :].unsqueeze(2).to_broadcast([n, 128, 16]),
                                                          in1=thr16[:n, :].unsqueeze(1).to_broadcast([n, 128, 16]), op=ALU.is_ge),
                     reads=["bcf", "thr16"], writes=["eq"])
                P.op("dve", lambda e, n=n: e.tensor_reduce(out=akf[:n, :], in_=eq[:n], axis=AX.X, op=ALU.add), reads=["eq"], writes=["akf"])
                P.op("dve", lambda e, n=n: e.tensor_scalar(out=akf[:n, :], in0=akf[:n, :], scalar1=-1.0, scalar2=None, op0=ALU.add), reads=["akf"], writes=["akf"])
                P.op("dve", lambda e, n=n: e.scalar_tensor_tensor(out=bkf[:n, :], in0=akf[:n, :], scalar=-16.0, in1=bcf[:n, :], op0=ALU.mult, op1=ALU.add),
                     reads=["akf", "bcf"], writes=["bkf"])
                i4 = idxf[:n].rearrange("p (h c) k -> p h c k", c=2)
                for (sel_f, cidx, dst_i, nm) in ((akf, 0, i1f, "i1f"), (bkf, 1, i2f, "i2f")):
                    P.op("dve", lambda e, sel_f=sel_f, n=n: e.tensor_tensor(out=eq[:n], in0=iota16[:n, :].unsqueeze(1).to_broadcast([n, 128, 16]),
                                                                           in1=sel_f[:n, :].unsqueeze(2).to_broadcast([n, 128, 16]), op=ALU.is_equal),
                         reads=["iota16", "akf", "bkf"], writes=["eq"])
                    P.op("dve", lambda e, cidx=cidx, i4=i4, n=n: e.tensor_tensor(out=eq[:n].rearrange("p (h k) a -> p h k a", k=16),
                                                                               in0=eq[:n].rearrange("p (h k) a -> p h k a", k=16),
                                                                               in1=i4[:, :, cidx, :].unsqueeze(2).to_broadcast([n, 8, 16, 16]), op=ALU.mult),
                         reads=["eq", "idxf"], writes=["eq"])
                    P.op("dve", lambda e, dst_i=dst_i, n=n: e.tensor_reduce(out=dst_i[:n, :], in_=eq[:n], axis=AX.X, op=ALU.add), reads=["eq"], writes=[nm])
                P.op("dve", lambda e, n=n: e.scalar_tensor_tensor(out=ef[:n, :], in0=i1f[:n, :], scalar=128.0, in1=i2f[:n, :], op0=ALU.mult, op1=ALU.add),
                     reads=["i1f", "i2f"], writes=["ef"])
                P.op("dve", lambda e, n=n: e.tensor_copy(out=eidx[:n, :], in_=ef[:n, :]), reads=["ef"], writes=["eidx"])
                P.op("dve", lambda e, n=n: e.tensor_tensor(out=gate[:n], in0=best[:n], in1=best[:n, :, 0:1].to_broadcast([n, 8, 16]), op=ALU.subtract),
                     reads=["best"], writes=["gate"])
                P.op("act", lambda e, n=n: e.activation(out=gate[:n], in_=gate[:n], func=AF.Exp), reads=["gate"], writes=["gate"])
                P.op("dve", lambda e, n=n: e.tensor_reduce(out=gsum[:n, :], in_=gate[:n], axis=AX.X, op=ALU.add), reads=["gate"], writes=["gsum"])
                P.op("dve", lambda e, n=n: e.reciprocal(out=gsum[:n, :], in_=gsum[:n, :]), reads=["gsum"], writes=["gsum"])
                P.op("dve", lambda e, n=n: e.tensor_tensor(out=gate[:n], in0=gate[:n], in1=gsum[:n, :].unsqueeze(2).to_broadcast([n, 8, 16]), op=ALU.mult),
                     reads=["gate", "gsum"], writes=["gate"])
                for slot in range(128):
                    ub = ucnt[0] % 3
                    ucnt[0] += 1
                    P.op("pool", lambda e, ub=ub, slot=slot, n=n: e.indirect_dma_start(
                        out=ubuf[ub][:n, :], out_offset=None, in_=pu, in_offset=bass.IndirectOffsetOnAxis(ap=eidx[:n, slot:slot + 1], axis=0)),
                        reads=["eidx"], writes=[("ubuf", ub)], dma=True)
                    P.op("dve", lambda e, ub=ub, slot=slot, n=n: e.scalar_tensor_tensor(
                        out=junk6[:n, :], in0=ubuf[ub][:n, :], scalar=1.0, in1=hn[:n, :], op0=ALU.mult, op1=ALU.mult,
                        accum_out=act_[:n, slot:slot + 1]),
                        reads=[("ubuf", ub), "hn"], writes=["junk6", "act_"])
                P.op("act", lambda e, n=n: e.activation(out=wgt[:n, :], in_=act_[:n, :], func=AF.Gelu), reads=["act_"], writes=["wgt"])
                P.op("dve", lambda e, n=n: e.tensor_tensor(out=wgt[:n, :], in0=wgt[:n, :], in1=gate[:n].rearrange("p h k -> p (h k)"), op=ALU.mult),
                     reads=["wgt", "gate"], writes=["wgt"])
                for slot in range(128):
                    ub = ucnt[0] % 3
                    ucnt[0] += 1
                    P.op("pool", lambda e, ub=ub, slot=slot, n=n: e.indirect_dma_start(
                        out=ubuf[ub][:n, :], out_offset=None, in_=pv, in_offset=bass.IndirectOffsetOnAxis(ap=eidx[:n, slot:slot + 1], axis=0)),
                        reads=["eidx"], writes=[("ubuf", ub)], dma=True)
                    P.op("dve", lambda e, ub=ub, slot=slot, n=n, hi=hi: e.scalar_tensor_tensor(
                        out=hi, in0=ubuf[ub][:n, :], scalar=wgt[:n, slot:slot + 1], in1=hi, op0=ALU.mult, op1=ALU.add),
                        reads=[("ubuf", ub), "wgt", hk], writes=[hk])
                dsty = yp[i * 128:(i + 1) * 128, :] if i < 8 else ys
                P.op("sp", lambda e, dsty=dsty, hi=hi: e.dma_start(out=dsty, in_=hi), reads=[hk], writes=[("y", i)], dma=True)
            P.barrier()
        es5.close()
        P.barrier()
        P.emit()
    return nc


def make_in_maps(inp, with_peer=True):
    xp = np.asarray(inp["x_prompt"], np.float32)
    xs = np.asarray(inp["x_sample"], np.float32)[:, 0, :]
    tabs = {"tab_" + u["name"]: np.ascontiguousarray(unit_variants(u)[2].reshape(128, -1)) for u in UNITS}
    shared = {
        "norm1_w": inp["norm1_w"][0], "w_in": inp["w_in"][0], "qna": inp["q_norm_a"][0], "kna": inp["k_norm_a"][0],
        "sink": inp["sink_a"][0], "qnb": inp["q_norm_b"][0], "knb": inp["k_norm_b"][0],
        "wba": inp["w_branch_a"][0], "wbb": inp["w_branch_b"][0], "wout": inp["w_out"][0],
        "norm2_w": inp["norm2_w"][0], "wq": inp["peer_wq"][0], "subk": inp["peer_subkeys"][0],
    }
    if with_peer:
        shared["pu"] = inp["peer_u"][0]; shared["pv"] = inp["peer_v"][0]
    shared = {k: np.ascontiguousarray(np.asarray(v, np.float32)) for k, v in shared.items()}
    shared.update(tabs)
    sb_ = np.zeros((128, 20), np.float32)
    ii = np.arange(128, dtype=np.float64)
    for h in range(20):
        dil = 1 if h < 12 else (4 if h < 16 else 16)
        sb_[:, h] = -SLOPES[h] * dil * (128.0 - ii)
    shared["sbias"] = sb_
    shared["dmask"] = np.ascontiguousarray(np.broadcast_to(np.eye(NS, dtype=np.float32).reshape(1, NS * NS), (128, NS * NS)))
    shared["iota16"] = np.ascontiguousarray(np.broadcast_to(np.arange(16, dtype=np.float32)[None, :], (128, 16)))
    shared["subkT"] = np.ascontiguousarray(np.asarray(inp["peer_subkeys"], np.float32)[0].transpose(0, 2, 1))
    maps = []
    for c in range(8):
        b, half = c // 2, c % 2
        m = dict(shared)
        m["xo"] = np.ascontiguousarray(xp[b, half * 1024:(half + 1) * 1024])
        m["xh"] = np.ascontiguousarray(xp[b, 0:1024]) if half == 1 else np.zeros((1024, D), np.float32)
        m["xs"] = np.ascontiguousarray(xs[c * NS:(c + 1) * NS])
        m["flag"] = np.ascontiguousarray(np.broadcast_to(np.array([[1.0, float(half)]], np.float32), (128, 2)))
        for nm, key in (("ca", "cache_a_kv"), ("cb1", "cache_b1_kv"), ("cb2", "cache_b2_kv"), ("cb3", "cache_b3_kv")):
            a = np.asarray(inp[key], np.float32)[0, c * NS:(c + 1) * NS]
            m[nm] = np.ascontiguousarray(a.reshape(NS, a.shape[1], -1))
        maps.append(m)
    return maps


_NC_CACHE = {}


def kernel(**inputs):
    if "nc" not in _NC_CACHE:
        _NC_CACHE["nc"] = build()
    nc = _NC_CACHE["nc"]
    maps = make_in_maps(inputs)
    res = run_bass_kernel_spmd(nc, maps, core_ids=list(range(8))).results
    y_p = np.zeros((4, 2048, D), np.float32)
    y_s = np.zeros((128, 1, D), np.float32)
    a_p = np.zeros((1, 4, 128, 2, 2, 128), np.float32)
    b1_p = np.zeros((1, 4, 128, 2, 4, 128), np.float32)
    b2_p = np.zeros((1, 4, 512, 2, 4, 128), np.float32)
    b3_p = np.zeros((1, 4, 2048, 2, 4, 128), np.float32)
    a_s = np.zeros((1, 128, 128, 2, 2, 128), np.float32)
    b1_s = np.zeros((1, 128, 128, 2, 4, 128), np.float32)
    b2_s = np.zeros((1, 128, 512, 2, 4, 128), np.float32)
    b3_s = np.zeros((1, 128, 2048, 2, 4, 128), np.float32)
    for c in range(8):
        b, half = c // 2, c % 2
        r = res[c]
        y_p[b, half * 1024:(half + 1) * 1024] = r["yp"]
        y_s[c * NS:(c + 1) * NS, 0] = r["ys"]
        b3_p[0, b, half * 1024:(half + 1) * 1024] = r["kvb3_p"].reshape(1024, 2, 4, 128)
        if half == 1:
            a_p[0, b] = r["kva_p"].reshape(128, 2, 2, 128)
            b1_p[0, b] = r["kvb1_p"].reshape(128, 2, 4, 128)
            b2_p[0, b] = r["kvb2_p"].reshape(512, 2, 4, 128)
        a_s[0, c * NS:(c + 1) * NS] = r["kva_s"].reshape(NS, 128, 2, 2, 128)
        b1_s[0, c * NS:(c + 1) * NS] = r["kvb1_s"].reshape(NS, 128, 2, 4, 128)
        b2_s[0, c * NS:(c + 1) * NS] = r["kvb2_s"].reshape(NS, 512, 2, 4, 128)
        b3_s[0, c * NS:(c + 1) * NS] = r["kvb3_s"].reshape(NS, 2048, 2, 4, 128)
    return (y_p, y_s, a_p, b1_p, b2_p, b3_p, a_s, b1_s, b2_s, b3_s)
```

```python
import numpy as np
from contextlib import ExitStack
import concourse.bass as bass
import concourse.mybir as mybir
from concourse.bass_utils import run_bass_kernel_spmd

F32 = mybir.dt.float32
BF16 = mybir.dt.bfloat16
I32 = mybir.dt.int32
U32 = mybir.dt.uint32
AF = mybir.ActivationFunctionType
ALU = mybir.AluOpType
AX = mybir.AxisListType

STREAMS = ("pe", "act", "dve", "pool", "sp")
NDMA_SEMS = 8


class Prog:
    def __init__(self, nc):
        self.nc = nc
        self.ops = {s: [] for s in STREAMS}
        self.semnames = list(STREAMS) + [f"d{s}{j}" for s in ("sp", "act", "pool") for j in range(NDMA_SEMS)]
        self.count = {n: 0 for n in self.semnames}
        self.dma_i = {"sp": 0, "act": 0, "pool": 0}
        self.last_w = {}
        self.readers = {}
        self.waited = {s: {} for s in STREAMS}
        self.n_ops = 0

    def _deps(self, reads, writes):
        deps = {}

        def add(d):
            if d is not None and d[1] > deps.get(d[0], 0):
                deps[d[0]] = d[1]

        for k in reads:
            add(self.last_w.get(k))
        for k in writes:
            add(self.last_w.get(k))
            for r in self.readers.get(k, ()):
                add(r)
        return deps

    def op(self, stream, fn, reads=(), writes=(), dma=False):
        deps = self._deps(reads, writes)
        if dma:
            dname = f"d{stream}{self.dma_i[stream] % NDMA_SEMS}"
            if self.count[dname] > 0:
                deps[dname] = max(deps.get(dname, 0), self.count[dname])
        w = self.waited[stream]
        waits = []
        for sname, v in deps.items():
            if v > w.get(sname, 0):
                waits.append((sname, v))
                w[sname] = v
        if dma:
            j = self.dma_i[stream] % NDMA_SEMS
            self.dma_i[stream] += 1
            sname = f"d{stream}{j}"
            inc = 16
        else:
            sname = stream
            inc = 1
        self.count[sname] += inc
        val = self.count[sname]
        self.ops[stream].append((waits, fn, sname, inc))
        for k in reads:
            self.readers.setdefault(k, []).append((sname, val))
        for k in writes:
            self.last_w[k] = (sname, val)
            self.readers[k] = []
        self.n_ops += 1
        return (sname, val)

    def barrier(self):
        waits = [(n, c) for n, c in self.count.items() if c > 0]
        for s in STREAMS:
            w = self.waited[s]
            ws = [(n, c) for n, c in waits if c > w.get(n, 0)]
            for n, c in ws:
                w[n] = c
            if ws:
                self.ops[s].append((ws, None, None, 0))
        self.last_w = {}
        self.readers = {}

    def emit(self):
        nc = self.nc
        with ExitStack() as es:
            sems = {n: es.enter_context(nc.semaphore(n)) for n in self.semnames}
            block = es.enter_context(nc.Block())

            def make(stream):
                def body(eng):
                    for waits, fn, sname, inc in self.ops[stream]:
                        for wn, wv in waits:
                            eng.wait_ge(sems[wn], wv)
                        if fn is not None:
                            fn(eng).then_inc(sems[sname], inc)
                return body

            block.tensor(make("pe"))
            block.scalar(make("act"))
            block.vector(make("dve"))
            block.gpsimd(make("pool"))
            block.sync(make("sp"))


D = 2048
NTOK = 1024
NT = 8
NS = 16
NIN = 10240
EPS = 1e-6
SCALE = 128.0 ** -0.5
SLOPES = [float(2.0 ** (-8.0 * i / 20.0)) for i in range(1, 21)]
XT_H, XT_O, XT_S = 0, 1024, 2048
NXT = 2064

GROUPS = {
    "A": dict(nh=1, nd=2, dil=1, win=128, H=2, L=128),
    "B1": dict(nh=1, nd=2, dil=1, win=128, H=4, L=128),
    "B2": dict(nh=4, nd=5, dil=4, win=512, H=4, L=512),
    "B3": dict(nh=8, nd=16, dil=16, win=2048, H=4, L=2048),
}


def make_units():
    units = []
    for kvh in range(2):
        units.append(dict(name=f"A{kvh}", grp="A", nq=4, nk=1,
                          qjobs=[(kvh * 512, 256, 0), (kvh * 512 + 256, 256, 2)],
                          kcol=1024 + kvh * 128, vcol=1280 + kvh * 128, kvhead0=kvh,
                          slopes=[SLOPES[kvh * 4 + g] for g in range(4)], ohead0=kvh * 4))
    for g, gname in enumerate(("B1", "B2", "B3")):
        for hp in range(2):
            units.append(dict(name=f"{gname}_{hp}", grp=gname, nq=2, nk=2,
                              qjobs=[(1536 + g * 512 + hp * 256, 256, 0)],
                              kcol=3072 + g * 512 + hp * 256, vcol=4608 + g * 512 + hp * 256,
                              kvhead0=hp * 2,
                              slopes=[SLOPES[8 + g * 4 + hp * 2 + j] for j in range(2)], ohead0=hp * 2))
    return units


UNITS = make_units()


def unit_variants(u):
    G = GROUPS[u["grp"]]
    nd, dil, win = G["nd"], G["dil"], G["win"]
    s = np.arange(128)[:, None].astype(np.float64)
    q = np.arange(128)[None, :].astype(np.float64)
    if u["grp"] == "B3":
        nvar = 2
        vmap = [(0, [0.0] * u["nq"])] + [(1, [-sl * 128.0 * (dl - 1) for sl in u["slopes"]]) for dl in range(1, nd)]
        var_delta = [0, 1]
    else:
        nvar = nd
        vmap = [(dl, [0.0] * u["nq"]) for dl in range(nd)]
        var_delta = list(range(nd))
    tab = np.zeros((128, u["nq"], nvar, 128), np.float32)
    for j, sl in enumerate(u["slopes"]):
        for v, dl in enumerate(var_delta):
            dist = 128.0 * dl + q - s
            ok = (dist >= 0) & (dist <= win) & (np.mod(dist, dil) == 0)
            tab[:, j, v, :] = np.where(ok, np.exp(-sl * np.where(ok, dist, 0.0)), 0.0)
    return nvar, vmap, tab


def build(stop_after=None, debug=False, bis=None):
    nc = bass.Bass("TRN2", target_bir_lowering=False)

    def din(name, shape, dt=F32):
        return nc.dram_tensor(name, list(shape), dt, kind="ExternalInput").ap()

    def dout(name, shape, dt=F32):
        return nc.dram_tensor(name, list(shape), dt, kind="ExternalOutput").ap()

    xo = din("xo", [NTOK, D]); xh = din("xh", [NTOK, D]); xs = din("xs", [NS, D])
    flag = din("flag", [128, 2])
    caches = {"A": din("ca", [NS, 128, 512]), "B1": din("cb1", [NS, 128, 1024]),
              "B2": din("cb2", [NS, 512, 1024]), "B3": din("cb3", [NS, 2048, 1024])}
    norm1_w = din("norm1_w", [D]); w_in = din("w_in", [D, NIN])
    qna = din("qna", [128]); kna = din("kna", [128]); sink = din("sink", [8])
    qnb = din("qnb", [128]); knb = din("knb", [128])
    wba = din("wba", [1024, D]); wbb = din("wbb", [512, D]); wout = din("wout", [D, D])
    norm2_w = din("norm2_w", [D]); wq = din("wq", [D, 1024]); subk = din("subk", [2, 128, 64])
    with_peer = stop_after is None and not debug
    if with_peer:
        pu = din("pu", [16384, D]); pv = din("pv", [16384, D])
    sbias_d = din("sbias", [128, 20]); dmask_d = din("dmask", [128, NS * NS]); iota16_d = din("iota16", [128, 16])
    subkT = din("subkT", [2, 64, 128])
    tabs_d = {}
    for u in UNITS:
        nvar, _, _ = unit_variants(u)
        tabs_d[u["name"]] = din("tab_" + u["name"], [128, u["nq"] * nvar * 128])

    yp = dout("yp", [NTOK, D]); ys = dout("ys", [NS, D])
    kvp = {"A": dout("kva_p", [128, 512]), "B1": dout("kvb1_p", [128, 1024]),
           "B2": dout("kvb2_p", [512, 1024]), "B3": dout("kvb3_p", [1024, 1024])}
    kvs = {"A": dout("kva_s", [NS, 128, 512]), "B1": dout("kvb1_s", [NS, 128, 1024]),
           "B2": dout("kvb2_s", [NS, 512, 1024]), "B3": dout("kvb3_s", [NS, 2048, 1024])}
    dbg = {}
    if debug:
        dbg["oaT"] = dout("dbg_oaT", [128, 8, 1040], BF16)
        dbg["obT"] = dout("dbg_obT", [128, 4, 1040], BF16)

    P = Prog(nc)
    with ExitStack() as es:
        def sb(name, shape, dt=F32):
            return es.enter_context(nc.sbuf_tensor(name, list(shape), dt))

        def ps(name, shape, dt=F32):
            return es.enter_context(nc.psum_tensor(name, list(shape), dt))

        ident = sb("ident", [128, 128])
        flag_t = sb("flag_t", [128, 2])
        gains = sb("gains", [128, 4, 128])
        esink = sb("esink", [128, 8])
        esA = ExitStack()

        def sbA(name, shape, dt=F32):
            return esA.enter_context(nc.sbuf_tensor(name, list(shape), dt))
        xnT = sbA("xnT", [128, 16, NXT], BF16)
        wring = [sbA(f"wring{i}", [128, 16, 256], BF16) for i in range(2)]
        oaT = sbA("oaT", [128, 8, 1040], BF16)
        obT = sbA("obT", [128, 4, 1040], BF16)
        pz = ps("pz", [128, 2, 512])
        ptr = ps("ptr", [128, 2, 512])
        pst = ps("pst", [128, 2, 512])
        po = ps("po", [128, 4, 256])

        P.op("pool", lambda e: e.memset(ident[:], 0.0), writes=["ident"])
        P.op("pool", lambda e: e.affine_select(out=ident[:], in_=ident[:], pattern=[[-1, 128]],
                                               compare_op=ALU.not_equal, fill=1.0, base=0, channel_multiplier=1),
             reads=["ident"], writes=["ident"])
        P.op("sp", lambda e: e.dma_start(out=flag_t[:], in_=flag), writes=["flag"], dma=True)
        for i, g in enumerate((qna, kna, qnb, knb)):
            P.op("sp", lambda e, i=i, g=g: e.dma_start(out=gains[:, i, :], in_=g.partition_broadcast(128)),
                 writes=[("gains", i)], dma=True)
        P.op("sp", lambda e: e.dma_start(out=esink[:], in_=sink.partition_broadcast(128)), writes=["esink"], dma=True)
        P.op("act", lambda e: e.activation(out=esink[:], in_=esink[:], func=AF.Exp), reads=["esink"], writes=["esink"])
        for gname, G in GROUPS.items():
            L = G["L"]
            for t0 in range(0, NS, 4):
                P.op("act", lambda e, gname=gname, L=L, t0=t0: e.dma_start(
                    out=kvs[gname][t0:t0 + 4, 0:L - 1, :], in_=caches[gname][t0:t0 + 4, 1:L, :]),
                    writes=[("kvs_copy", gname, t0)], dma=True)

        with ExitStack() as es1:
            def sb1(name, shape, dt=F32):
                return es1.enter_context(nc.sbuf_tensor(name, list(shape), dt))
            w1rep = sb1("w1rep", [128, D])
            xbuf = [sb1(f"xbuf{i}", [128, D]) for i in range(2)]
            xnb = [sb1(f"xnb{i}", [128, D]) for i in range(2)]
            junk = sb1("junk1", [128, D], BF16)
            st1 = [sb1(f"st1_{i}", [128, 4]) for i in range(2)]
            P.op("sp", lambda e: e.dma_start(out=w1rep[:], in_=norm1_w.partition_broadcast(128)), writes=["w1rep"], dma=True)
            tiles = [(xh, i, XT_H + i * 128, 128) for i in range(8)] + [(xo, i, XT_O + i * 128, 128) for i in range(8)] + [(xs, 0, XT_S, NS)]
            for ti, (src, i, toff, n) in enumerate(tiles):
                s = ti % 2
                xb, xn_, st = xbuf[s], xnb[s], st1[s]
                P.op("sp", lambda e, xb=xb, src=src, i=i, n=n: e.dma_start(out=xb[:n, :], in_=src[i * 128:i * 128 + n, :]),
                     writes=[("xbuf", s)], dma=True)
                P.op("act", lambda e, xb=xb, st=st, n=n: e.activation(out=junk[:n, :], in_=xb[:n, :], func=AF.Square, accum_out=st[:n, 0:1]),
                     reads=[("xbuf", s)], writes=["junk1", ("st1", s)])
                P.op("act", lambda e, st=st, n=n: e.activation(out=st[:n, 1:2], in_=st[:n, 0:1], func=AF.Ln, scale=1.0 / D, bias=EPS),
                     reads=[("st1", s)], writes=[("st1", s)])
                P.op("act", lambda e, st=st, n=n: e.activation(out=st[:n, 2:3], in_=st[:n, 1:2], func=AF.Exp, scale=-0.5),
                     reads=[("st1", s)], writes=[("st1", s)])
                P.op("dve", lambda e, xb=xb, xn_=xn_, st=st, n=n: e.scalar_tensor_tensor(
                    out=xn_[:n, :], in0=xb[:n, :], scalar=st[:n, 2:3], in1=w1rep[:n, :], op0=ALU.mult, op1=ALU.mult),
                    reads=[("xbuf", s), ("st1", s), "w1rep"], writes=[("xnb", s)])
                for k4 in range(4):
                    bank = (ti * 4 + k4) % 2
                    for kk in range(4):
                        k = k4 * 4 + kk
                        P.op("pe", lambda e, xn_=xn_, k=k, kk=kk, bank=bank, n=n: e.transpose(
                            out=ptr[:, bank, kk * 128:kk * 128 + n], in_=xn_[:n, k * 128:(k + 1) * 128], identity=ident[:n, :n]),
                            reads=[("xnb", s), "ident"], writes=[("ptr", bank)])
                    eng = "act" if k4 % 2 == 0 else "dve"
                    if eng == "act":
                        P.op("act", lambda e, k4=k4, bank=bank, toff=toff, n=n: e.activation(
                            out=xnT[:, k4 * 4:(k4 + 1) * 4, toff:toff + n],
                            in_=ptr[:, bank, :].rearrange("p (a b) -> p a b", b=128)[:, :, :n], func=AF.Copy),
                            reads=[("ptr", bank)], writes=[("xnT", toff)])
                    else:
                        P.op("dve", lambda e, k4=k4, bank=bank, toff=toff, n=n: e.tensor_copy(
                            out=xnT[:, k4 * 4:(k4 + 1) * 4, toff:toff + n],
                            in_=ptr[:, bank, :].rearrange("p (a b) -> p a b", b=128)[:, :, :n]),
                            reads=[("ptr", bank)], writes=[("xnT", toff)])
            P.barrier()

        wcount = [0]
        ring = list(wring)

        def load_w(src, col0, ncols, nk=16):
            s = wcount[0] % len(ring)
            wcount[0] += 1
            wt = ring[s]
            P.op("pool", lambda e: e.dma_start(out=wt[:, :nk, :ncols],
                                               in_=src[0:nk * 128, col0:col0 + ncols].rearrange("(k p) n -> p k n", p=128)),
                 writes=[("wring", s)], dma=True)
            return wt, ("wring", s)

        if stop_after == 1:
            P.barrier(); P.emit(); esA.close(); return nc

        with ExitStack() as es2:
            def sb2(name, shape, dt=F32):
                return es2.enter_context(nc.sbuf_tensor(name, list(shape), dt))
            QT = sb2("QT", [128, 4, NTOK], BF16)
            accBv = sb2("accBv", [128, NT, 4, 128])
            accBd = sb2("accBd", [128, NT, 4])
            onesc = sb2("onesc", [128, 2], BF16)
            po2 = po[:].rearrange("p a b -> p (a b)")
            KT = sb2("KT", [128, 2, 16 * 128], BF16)
            VA = sb2("VA", [128, 16, 2, 128], BF16)
            tabt = sb2("tabt", [128, 2 * 5 * 128], BF16)
            sq = sb2("sq", [128, 256])
            zn = [sb2(f"zn{i}", [128, 256]) for i in range(2)]
            vf = [sb2(f"vf{i}", [128, 256]) for i in range(2)]
            st2 = [sb2(f"st2_{i}", [128, 8]) for i in range(2)]
            Eb = [sb2(f"Eb{i}", [128, 512]) for i in range(2)]
            Pm = [sb2(f"Pm{i}", [128, 4096], BF16) for i in range(2)]
            ot = [sb2(f"ot{i}", [128, 4, 128]) for i in range(2)]
            rc = [sb2(f"rc{i}", [128, 4]) for i in range(2)]
            cnt = dict(z=0, tr=0, zn=0, vf=0, st=0, E=0, pm=0, ot=0)

            def proj_tile(wt, wkey, ncols, toff, n):
                b = cnt["z"] % 2
                cnt["z"] += 1
                for k in range(16):
                    P.op("pe", lambda e, k=k, b=b: e.matmul(pz[:n, b, :ncols], lhsT=xnT[:, k, toff:toff + n], rhs=wt[:, k, :ncols],
                                                         start=(k == 0), stop=(k == 15)),
                         reads=[("xnT", toff), wkey], writes=[("pz", b)])
                return b

            def qk_norm(b, nheads, gain_idx, n):
                ncols = nheads * 128
                s = cnt["zn"] % 2
                cnt["zn"] += 1
                st = st2[s]
                P.op("act", lambda e: e.activation(out=sq[:n, :ncols], in_=pz[:n, b, :ncols], func=AF.Square),
                     reads=[("pz", b)], writes=["sq"])
                P.op("dve", lambda e: e.tensor_reduce(out=st[:n, 0:nheads], in_=sq[:n, :ncols].rearrange("p (h d) -> p h d", d=128),
                                                      axis=AX.X, op=ALU.add),
                     reads=["sq"], writes=[("st2", s)])
                P.op("act", lambda e: e.activation(out=st[:n, 2:2 + nheads], in_=st[:n, 0:nheads], func=AF.Ln, scale=1.0 / 128, bias=EPS),
                     reads=[("st2", s)], writes=[("st2", s)])
                P.op("act", lambda e: e.activation(out=st[:n, 4:4 + nheads], in_=st[:n, 2:2 + nheads], func=AF.Exp, scale=-0.5),
                     reads=[("st2", s)], writes=[("st2", s)])
                for h in range(nheads):
                    P.op("dve", lambda e, h=h: e.scalar_tensor_tensor(
                        out=zn[s][:n, h * 128:(h + 1) * 128], in0=pz[:n, b, h * 128:(h + 1) * 128], scalar=st[:n, 4 + h:5 + h],
                        in1=gains[:n, gain_idx, :], op0=ALU.mult, op1=ALU.mult),
                        reads=[("pz", b), ("st2", s), ("gains", gain_idx)], writes=[("zn", s)])
                return s

            def transpose_to(src_ap_fn, nheads, n, dst_ap, dkey, rkeys):
                bank = cnt["tr"] % 2
                cnt["tr"] += 1
                for h in range(nheads):
                    P.op("pe", lambda e, h=h: e.transpose(out=ptr[:, bank, h * 128:h * 128 + n], in_=src_ap_fn(h), identity=ident[:n, :n]),
                         reads=list(rkeys) + ["ident"], writes=[("ptr", bank)])
                P.op("act", lambda e: e.activation(out=dst_ap, in_=ptr[:, bank, :nheads * 128].rearrange("p (a b) -> p a b", b=128)[:, :, :n],
                                                   func=AF.Copy),
                     reads=[("ptr", bank)], writes=[dkey])

            for ui, u in enumerate(UNITS):
                if bis is not None and ui >= bis[0]:
                    break
                G = GROUPS[u["grp"]]
                nh, nd = G["nh"], G["nd"]
                nq, nk = u["nq"], u["nk"]
                gq = nq // nk
                isA = u["grp"] == "A"
                nvar, vmap, _ = unit_variants(u)
                W = G["H"] * 128
                P.op("pool", lambda e, u=u, nvar=nvar, nq=nq: e.dma_start(out=tabt[:, :nq * nvar * 128], in_=tabs_d[u["name"]]),
                     writes=["tabt"], dma=True)
                for (c0, nc_, h0) in u["qjobs"]:
                    wt, wkey = load_w(w_in, c0, nc_)
                    for i in range(NT):
                        if bis is not None and len(bis) > 2 and bis[2] < 2:
                            break
                        b = proj_tile(wt, wkey, nc_, XT_O + i * 128, 128)
                        s = qk_norm(b, nc_ // 128, 0 if isA else 2, 128)
                        transpose_to(lambda h, s=s: zn[s][:, h * 128:(h + 1) * 128], nc_ // 128, 128,
                                     QT[:, h0:h0 + nc_ // 128, i * 128:(i + 1) * 128], ("QT", i), [("zn", s)])
                if bis is not None and len(bis) > 2 and bis[2] < 3:
                    continue
                nkc = nk * 128
                wt, wkey = load_w(w_in, u["kcol"], nkc)
                ktiles = [(XT_H + (8 - nh + j) * 128, j, None) for j in range(nh)] + [(XT_O + i * 128, nh + i, i) for i in range(NT)]
                out_tiles = {"A": [7], "B1": [7], "B2": [4, 5, 6, 7], "B3": list(range(8))}[u["grp"]]
                for (toff, kti, own_i) in ktiles:
                    b = proj_tile(wt, wkey, nkc, toff, 128)
                    s = qk_norm(b, nk, 1 if isA else 3, 128)
                    if own_i is not None and own_i in out_tiles:
                        r0 = (own_i - out_tiles[0]) * 128
                        cc = u["kvhead0"] * 128
                        P.op("sp", lambda e, s=s, r0=r0, cc=cc, nkc=nkc, grp=u["grp"]: e.dma_start(
                            out=kvp[grp][r0:r0 + 128, cc:cc + nkc], in_=zn[s][:, :nkc]),
                            reads=[("zn", s)], writes=[("kvp", u["name"], "k", own_i)], dma=True)
                    transpose_to(lambda h, s=s: zn[s][:, h * 128:(h + 1) * 128], nk, 128,
                                 KT[:, 0:nk, kti * 128:(kti + 1) * 128], ("KT", kti), [("zn", s)])
                if bis is not None and len(bis) > 2 and bis[2] == 3.5:
                    load_w(w_in, u["vcol"], nkc)
                    load_w(w_in, u["vcol"], nkc)
                    continue
                if bis is not None and len(bis) > 2 and bis[2] < 4:
                    continue
                vm = bis[3] if (bis is not None and len(bis) > 3) else 15
                if vm & 1:
                    P.op("dve", lambda e: e.tensor_copy(out=onesc[:, 0:2], in_=flag_t[:, 0:2]), reads=["flag"], writes=["onesc"])
                wt, wkey = load_w(w_in, u["vcol"], nkc)
                for (toff, kti, own_i) in ktiles:
                    b = proj_tile(wt, wkey, nkc, toff, 128)
                    is_out = own_i is not None and own_i in out_tiles
                    if not is_out:
                        P.op("dve", lambda e, b=b, kti=kti, nk=nk, nkc=nkc: e.tensor_copy(
                            out=VA[:, kti, 0:nk, :], in_=pz[:, b, :nkc].rearrange("p (h d) -> p h d", d=128)),
                            reads=[("pz", b)], writes=[("VA", kti)])
                    else:
                        s = cnt["vf"] % 2
                        cnt["vf"] += 1
                        r0 = (own_i - out_tiles[0]) * 128
                        cc = W + u["kvhead0"] * 128
                        P.op("dve", lambda e, b=b, s=s, nkc=nkc: e.tensor_copy(out=vf[s][:, :nkc], in_=pz[:, b, :nkc]),
                             reads=[("pz", b)], writes=[("vf", s)])
                        P.op("pool", lambda e, s=s, kti=kti, nk=nk, nkc=nkc: e.tensor_copy(
                            out=VA[:, kti, 0:nk, :], in_=vf[s][:, :nkc].rearrange("p (h d) -> p h d", d=128)),
                            reads=[("vf", s)], writes=[("VA", kti)])
                        P.op("sp", lambda e, s=s, r0=r0, cc=cc, nkc=nkc, grp=u["grp"]: e.dma_start(
                            out=kvp[grp][r0:r0 + 128, cc:cc + nkc], in_=vf[s][:, :nkc]),
                            reads=[("vf", s)], writes=[("kvp", u["name"], "v", own_i)], dma=True)
                for qt in range(NT):
                    if bis is not None and bis[1] == 0:
                        break
                    deltas = [dl for dl in range(nd) if qt - dl >= -nh]
                    ps_ = cnt["pm"] % 2
                    cnt["pm"] += 1
                    for di, dl in enumerate(deltas):
                        kti = qt - dl + nh
                        var, biases = vmap[dl]
                        sb_ = cnt["E"] % 2
                        cnt["E"] += 1
                        for k in range(nk):
                            P.op("pe", lambda e, k=k, kti=kti, sb_=sb_, qt=qt, gq=gq: e.matmul(
                                pst[:, sb_, k * gq * 128:(k + 1) * gq * 128], lhsT=KT[:, k, kti * 128:(kti + 1) * 128],
                                rhs=QT[:, k * gq:(k + 1) * gq, qt * 128:(qt + 1) * 128], start=True, stop=True),
                                reads=[("KT", kti), ("QT", qt)], writes=[("pst", sb_)])
                        if all(bv == 0.0 for bv in biases):
                            P.op("act", lambda e, sb_=sb_, nq=nq: e.activation(out=Eb[sb_][:, :nq * 128], in_=pst[:, sb_, :nq * 128],
                                                                             func=AF.Exp, scale=SCALE),
                                 reads=[("pst", sb_)], writes=[("Eb", sb_)])
                        else:
                            for j in range(nq):
                                P.op("act", lambda e, sb_=sb_, j=j, bv=biases[j]: e.activation(
                                    out=Eb[sb_][:, j * 128:(j + 1) * 128], in_=pst[:, sb_, j * 128:(j + 1) * 128],
                                    func=AF.Exp, scale=SCALE, bias=bv),
                                    reads=[("pst", sb_)], writes=[("Eb", sb_)])
                        P.op("dve", lambda e, sb_=sb_, ps_=ps_, di=di, var=var, nq=nq, nvar=nvar: e.tensor_tensor(
                            out=Pm[ps_][:, di * nq * 128:(di + 1) * nq * 128].rearrange("p (j q) -> p j q", q=128),
                            in0=Eb[sb_][:, :nq * 128].rearrange("p (j q) -> p j q", q=128),
                            in1=tabt[:, :nq * nvar * 128].rearrange("p (j v q) -> p j v q", v=nvar, q=128)[:, :, var, :],
                            op=ALU.mult),
                            reads=[("Eb", sb_), "tabt"], writes=[("Pm", ps_, di)])
                    for j in range(nq):
                        for di, dl in enumerate(deltas):
                            kti = qt - dl + nh
                            P.op("pe", lambda e, j=j, di=di, kti=kti, ps_=ps_, gq=gq, nq=nq, first=(di == 0), last=(di == len(deltas) - 1): e.matmul(
                                po2[:, j * 128:(j + 1) * 128], lhsT=Pm[ps_][:, (di * nq + j) * 128:(di * nq + j + 1) * 128], rhs=VA[:, kti, j // gq, :],
                                start=first, stop=last),
                                reads=[("Pm", ps_, di), ("VA", kti)], writes=["po_num"])
                    for j in range(nq):
                        for di, dl in enumerate(deltas):
                            kti = qt - dl + nh
                            oc = 1 if kti < nh else 0
                            P.op("pe", lambda e, j=j, di=di, oc=oc, ps_=ps_, nq=nq, first=(di == 0), last=(di == len(deltas) - 1): e.matmul(
                                po2[:, 512 + j:513 + j], lhsT=Pm[ps_][:, (di * nq + j) * 128:(di * nq + j + 1) * 128], rhs=onesc[:, oc:oc + 1],
                                start=first, stop=last),
                                reads=[("Pm", ps_, di), "onesc"], writes=["po_den"])
                    if isA:
                        so = cnt["ot"] % 2
                        cnt["ot"] += 1
                        h0 = u["ohead0"]
                        P.op("dve", lambda e, so=so, h0=h0: e.tensor_tensor(out=rc[so][:, 0:4], in0=po2[:, 512:516], in1=esink[:, h0:h0 + 4], op=ALU.add),
                             reads=["po_den", "esink"], writes=[("rc", so)])
                        P.op("dve", lambda e, so=so: e.reciprocal(out=rc[so][:, 0:4], in_=rc[so][:, 0:4]), reads=[("rc", so)], writes=[("rc", so)])
                        P.op("dve", lambda e, so=so: e.tensor_tensor(out=ot[so][:], in0=po2[:, 0:512].rearrange("p (h d) -> p h d", d=128),
                                                                    in1=rc[so][:, 0:4].unsqueeze(2).to_broadcast([128, 4, 128]), op=ALU.mult),
                             reads=["po_num", ("rc", so)], writes=[("ot", so)])
                        transpose_to(lambda h, so=so: ot[so][:, h, :], 4, 128, oaT[:, h0:h0 + 4, qt * 128:(qt + 1) * 128],
                                     ("oaT", h0, qt), [("ot", so)])
                    else:
                        h0 = u["ohead0"]
                        if u["grp"] == "B1":
                            P.op("dve", lambda e, h0=h0, qt=qt: e.tensor_copy(out=accBv[:, qt, h0:h0 + 2, :], in_=po2[:, 0:256].rearrange("p (h d) -> p h d", d=128)),
                                 reads=["po_num"], writes=[("accBv", qt, h0)])
                            P.op("dve", lambda e, h0=h0, qt=qt: e.tensor_copy(out=accBd[:, qt, h0:h0 + 2], in_=po2[:, 512:514]),
                                 reads=["po_den"], writes=[("accBd", qt, h0)])
                        else:
                            P.op("dve", lambda e, h0=h0, qt=qt: e.tensor_tensor(out=accBv[:, qt, h0:h0 + 2, :], in0=po2[:, 0:256].rearrange("p (h d) -> p h d", d=128),
                                                                               in1=accBv[:, qt, h0:h0 + 2, :], op=ALU.add),
                                 reads=["po_num", ("accBv", qt, h0)], writes=[("accBv", qt, h0)])
                            P.op("dve", lambda e, h0=h0, qt=qt: e.tensor_tensor(out=accBd[:, qt, h0:h0 + 2], in0=po2[:, 512:514],
                                                                               in1=accBd[:, qt, h0:h0 + 2], op=ALU.add),
                                 reads=["po_den", ("accBd", qt, h0)], writes=[("accBd", qt, h0)])
            for qt in range(NT):
                if bis is not None:
                    break
                so = cnt["ot"] % 2
                cnt["ot"] += 1
                P.op("dve", lambda e, so=so, qt=qt: e.reciprocal(out=rc[so][:, 0:4], in_=accBd[:, qt, :]),
                     reads=[("accBd", qt, 0), ("accBd", qt, 2)], writes=[("rc", so)])
                P.op("dve", lambda e, so=so, qt=qt: e.tensor_tensor(out=ot[so][:], in0=accBv[:, qt, :, :],
                                                                   in1=rc[so][:, 0:4].unsqueeze(2).to_broadcast([128, 4, 128]), op=ALU.mult),
                     reads=[("accBv", qt, 0), ("accBv", qt, 2), ("rc", so)], writes=[("ot", so)])
                transpose_to(lambda h, so=so: ot[so][:, h, :], 4, 128, obT[:, 0:4, qt * 128:(qt + 1) * 128], ("obT", qt), [("ot", so)])
            P.barrier()

        if stop_after == 2 or bis is not None:
            if debug and bis is None:
                P.op("sp", lambda e: e.dma_start(out=dbg["oaT"][:, :, 0:1024], in_=oaT[:, :, 0:1024]), reads=[], writes=["dbg1"], dma=True)
                P.op("sp", lambda e: e.dma_start(out=dbg["obT"][:, :, 0:1024], in_=obT[:, :, 0:1024]), reads=[], writes=["dbg2"], dma=True)
            P.barrier(); P.emit(); esA.close(); return nc

        with ExitStack() as es3:
            def sb3(name, shape, dt=F32):
                return es3.enter_context(nc.sbuf_tensor(name, list(shape), dt))
            qs = sb3("qs", [NS, 20 * 128]); ks = sb3("ks", [NS, 14 * 128]); vs = sb3("vs", [NS, 14 * 128])
            qsb = sb3("qsb", [NS, 20 * 128], BF16)
            sel = sb3("sel", [NS, NS, 128], BF16)
            kvc = sb3("kvc", [128, NS, 1024], BF16)
            qrep = sb3("qrep", [128, 1024], BF16)
            prod = sb3("prod", [128, 1024])
            Ssb = sb3("Ssb", [128, NS * 8]); S2 = sb3("S2", [128, NS * 8])
            Pb = sb3("Pb", [128, 8, NS], BF16)
            Pexp = sb3("Pexp", [128, 8, NS, NS], BF16)
            sbias_t = sb3("sbias_t", [128, 20]); dmask_t = sb3("dmask_t", [128, NS, NS])
            onesb = sb3("onesb", [128, 1], BF16)
            sq3 = sb3("sq3", [NS, 256]); st3 = [sb3(f"st3_{i}", [NS, 8]) for i in range(2)]
            lg = sb3("lg", [NS, 20]); enew = sb3("enew", [NS, 20])
            tmpv = sb3("tmpv", [NS, 8, 128])
            numA = sb3("numA", [NS, 8, 128]); denA = sb3("denA", [NS, 8])
            numB = sb3("numB", [NS, 4, 128]); denB = sb3("denB", [NS, 4])
            pst_f = pst[:].rearrange("p a b -> p (a b)")
            po_f = po[:].rearrange("p a b -> p (a b)")
            P.op("sp", lambda e: e.dma_start(out=sbias_t[:], in_=sbias_d), writes=["sbias"], dma=True)
            P.op("sp", lambda e: e.dma_start(out=dmask_t[:].rearrange("p a b -> p (a b)"), in_=dmask_d), writes=["dmask"], dma=True)
            P.op("pool", lambda e: e.memset(onesb[:], 1.0), writes=["onesb"])
            P.op("dve", lambda e: e.tensor_copy(out=sel[:], in_=ident[:NS, :NS].unsqueeze(2).to_broadcast([NS, NS, 128])),
                 reads=["ident"], writes=["sel"])
            for c in range(24):
                wt, wkey = load_w(w_in, c * 256, 256)
                b = c % 2
                for k in range(16):
                    P.op("pe", lambda e, k=k, b=b, wt=wt: e.matmul(pz[:NS, b, :256], lhsT=xnT[:, k, XT_S:XT_S + NS], rhs=wt[:, k, :256],
                                                                  start=(k == 0), stop=(k == 15)),
                         reads=[("xnT", XT_S), wkey], writes=[("pz", b)])
                if c < 4:
                    kind, dst, gi = "n", qs[:, c * 256:(c + 1) * 256], 0
                elif c == 4:
                    kind, dst, gi = "n", ks[:, 0:256], 1
                elif c == 5:
                    kind, dst, gi = "v", vs[:, 0:256], None
                elif c < 12:
                    kind, dst, gi = "n", qs[:, 1024 + (c - 6) * 256:1024 + (c - 5) * 256], 2
                elif c < 18:
                    kind, dst, gi = "n", ks[:, 256 + (c - 12) * 256:256 + (c - 11) * 256], 3
                else:
                    kind, dst, gi = "v", vs[:, 256 + (c - 18) * 256:256 + (c - 17) * 256], None
                dkey = ("sdst", c)
                if kind == "v":
                    P.op("act", lambda e, b=b, dst=dst: e.activation(out=dst, in_=pz[:NS, b, :256], func=AF.Copy),
                         reads=[("pz", b)], writes=[dkey, "svs"])
                else:
                    st = st3[c % 2]
                    skey = ("st3", c % 2)
                    P.op("act", lambda e, b=b: e.activation(out=sq3[:, :], in_=pz[:NS, b, :256], func=AF.Square), reads=[("pz", b)], writes=["sq3"])
                    P.op("dve", lambda e, st=st: e.tensor_reduce(out=st[:, 0:2], in_=sq3[:, :].rearrange("p (h d) -> p h d", d=128), axis=AX.X, op=ALU.add),
                         reads=["sq3"], writes=[skey])
                    P.op("act", lambda e, st=st: e.activation(out=st[:, 2:4], in_=st[:, 0:2], func=AF.Ln, scale=1.0 / 128, bias=EPS), reads=[skey], writes=[skey])
                    P.op("act", lambda e, st=st: e.activation(out=st[:, 4:6], in_=st[:, 2:4], func=AF.Exp, scale=-0.5), reads=[skey], writes=[skey])
                    for h in range(2):
                        P.op("dve", lambda e, h=h, b=b, st=st, dst=dst, gi=gi: e.scalar_tensor_tensor(
                            out=dst[:, h * 128:(h + 1) * 128], in0=pz[:NS, b, h * 128:(h + 1) * 128], scalar=st[:, 4 + h:5 + h],
                            in1=gains[:NS, gi, :], op0=ALU.mult, op1=ALU.mult),
                            reads=[("pz", b), skey, ("gains", gi)], writes=[dkey, "sqk"])
            P.op("dve", lambda e: e.tensor_copy(out=qsb[:], in_=qs[:]), reads=["sqk"], writes=["qsb"])
            P.op("dve", lambda e: e.tensor_tensor(out=prod[:NS, 0:1024].rearrange("p (k g d) -> p k g d", k=2, g=4),
                                                  in0=qs[:, 0:1024].rearrange("p (k g d) -> p k g d", k=2, g=4),
                                                  in1=ks[:, 0:256].rearrange("p (k d) -> p k d", k=2).unsqueeze(2).to_broadcast([NS, 2, 4, 128]),
                                                  op=ALU.mult), reads=["sqk"], writes=["prod"])
            P.op("dve", lambda e: e.tensor_reduce(out=lg[:, 0:8], in_=prod[:NS, 0:1024].rearrange("p (h d) -> p h d", d=128), axis=AX.X, op=ALU.add),
                 reads=["prod"], writes=["lg"])
            for (c0, c1) in ((1024, 2048), (2048, 2560)):
                n_ = c1 - c0
                P.op("dve", lambda e, c0=c0, c1=c1, n_=n_: e.tensor_tensor(out=prod[:NS, 0:n_], in0=qs[:, c0:c1], in1=ks[:, c0 - 768:c1 - 768], op=ALU.mult),
                     reads=["sqk"], writes=["prod"])
                P.op("dve", lambda e, c0=c0, c1=c1, n_=n_: e.tensor_reduce(out=lg[:, c0 // 128:c1 // 128], in_=prod[:NS, 0:n_].rearrange("p (h d) -> p h d", d=128),
                                                                      axis=AX.X, op=ALU.add),
                     reads=["prod"], writes=["lg"])
            P.op("act", lambda e: e.activation(out=enew[:], in_=lg[:], func=AF.Exp, scale=SCALE), reads=["lg"], writes=["enew"])
            for gi_, gname in enumerate(("A", "B1", "B2", "B3")):
                G = GROUPS[gname]
                H, L, dil = G["H"], G["L"], G["dil"]
                W = 2 * H * 128
                isA = gname == "A"
                hq = 8 if isA else 4
                hb = 0 if isA else 8 + 4 * (gi_ - 1)
                kb = 0 if isA else 2 + 4 * (gi_ - 1)
                qoff = hb * 128
                csrc = bass.AP(caches[gname].tensor, 0, [[dil * W, 128], [L * W, NS], [1, W]])
                P.op("pool", lambda e, csrc=csrc, W=W: e.dma_start(out=kvc[:, :, :W], in_=csrc), writes=["kvc"], dma=True)
                P.op("sp", lambda e, gname=gname, L=L, H=H, kb=kb: e.dma_start(out=kvs[gname][:, L - 1, 0:H * 128], in_=ks[:, kb * 128:(kb + H) * 128]),
                     reads=["sqk"], writes=[("kvs_newk", gname)], dma=True)
                P.op("sp", lambda e, gname=gname, L=L, H=H, kb=kb: e.dma_start(out=kvs[gname][:, L - 1, H * 128:2 * H * 128], in_=vs[:, kb * 128:(kb + H) * 128]),
                     reads=["svs"], writes=[("kvs_newv", gname)], dma=True)
                tc_ = 1 if isA else 2
                for t0 in range(0, NS, tc_):
                    for j in range(tc_):
                        for half in range(hq * 128 // 512):
                            P.op("pe", lambda e, t0=t0, j=j, half=half, hq=hq, qoff=qoff: e.matmul(
                                pst_f[:, j * hq * 128 + half * 512:j * hq * 128 + (half + 1) * 512], lhsT=sel[:, t0 + j, :],
                                rhs=qsb[:, qoff + half * 512:qoff + (half + 1) * 512], start=True, stop=True),
                                reads=["sel", "qsb"], writes=["pstf"])
                    P.op("act", lambda e: e.activation(out=qrep[:], in_=pst_f, func=AF.Copy), reads=["pstf"], writes=["qrep"])
                    if isA:
                        P.op("dve", lambda e, t0=t0: e.tensor_tensor(
                            out=prod[:].rearrange("p (k g d) -> p k g d", k=2, g=4),
                            in0=kvc[:, t0, 0:256].rearrange("p (k d) -> p k d", k=2).unsqueeze(2).to_broadcast([128, 2, 4, 128]),
                            in1=qrep[:].rearrange("p (k g d) -> p k g d", k=2, g=4), op=ALU.mult),
                            reads=["kvc", "qrep"], writes=["prod"])
                    else:
                        P.op("dve", lambda e, t0=t0: e.tensor_tensor(
                            out=prod[:].rearrange("p (t c) -> p t c", t=2), in0=kvc[:, t0:t0 + 2, 0:512],
                            in1=qrep[:].rearrange("p (t c) -> p t c", t=2), op=ALU.mult),
                            reads=["kvc", "qrep"], writes=["prod"])
                    P.op("dve", lambda e, t0=t0, hq=hq, tc_=tc_: e.tensor_reduce(
                        out=Ssb[:, t0 * hq:(t0 + tc_) * hq], in_=prod[:].rearrange("p (h d) -> p h d", d=128), axis=AX.X, op=ALU.add),
                        reads=["prod"], writes=["Ssb"])
                P.op("dve", lambda e, hq=hq, hb=hb: e.scalar_tensor_tensor(
                    out=S2[:, :NS * hq].rearrange("p (t h) -> p t h", h=hq), in0=Ssb[:, :NS * hq].rearrange("p (t h) -> p t h", h=hq),
                    scalar=SCALE, in1=sbias_t[:, hb:hb + hq].unsqueeze(1).to_broadcast([128, NS, hq]), op0=ALU.mult, op1=ALU.add),
                    reads=["Ssb", "sbias"], writes=["S2"])
                P.op("act", lambda e, hq=hq: e.activation(out=Pb[:, 0:hq, :], in_=S2[:, :NS * hq].rearrange("p (t h) -> p h t", h=hq), func=AF.Exp),
                     reads=["S2"], writes=["Pb"])
                P.op("dve", lambda e, hq=hq: e.tensor_tensor(
                    out=Pexp[:, 0:hq, :, :], in0=Pb[:, 0:hq, :].unsqueeze(2).to_broadcast([128, hq, NS, NS]),
                    in1=dmask_t[:].unsqueeze(1).to_broadcast([128, hq, NS, NS]), op=ALU.mult),
                    reads=["Pb", "dmask"], writes=["Pexp"])
                for h in range(hq):
                    kvh = h // 4 if isA else h
                    for tp in range(NS):
                        P.op("pe", lambda e, h=h, tp=tp, kvh=kvh, H=H: e.matmul(
                            po_f[:NS, h * 128:(h + 1) * 128], lhsT=Pexp[:, h, tp, :], rhs=kvc[:, tp, (H + kvh) * 128:(H + kvh + 1) * 128],
                            start=(tp == 0), stop=(tp == NS - 1)),
                            reads=["Pexp", "kvc"], writes=["pof"])
                    P.op("pe", lambda e, h=h: e.matmul(pz[:NS, 1, h:h + 1], lhsT=Pb[:, h, :], rhs=onesb[:, 0:1], start=True, stop=True),
                         reads=["Pb", "onesb"], writes=[("pz", 1)])
                if isA:
                    P.op("dve", lambda e: e.tensor_tensor(
                        out=tmpv[:].rearrange("p (k g) d -> p k g d", k=2),
                        in0=vs[:, 0:256].rearrange("p (k d) -> p k d", k=2).unsqueeze(2).to_broadcast([NS, 2, 4, 128]),
                        in1=enew[:, 0:8].rearrange("p (k g) -> p k g", k=2).unsqueeze(3).to_broadcast([NS, 2, 4, 128]), op=ALU.mult),
                        reads=["svs", "enew"], writes=["tmpv"])
                    P.op("dve", lambda e: e.tensor_tensor(out=numA[:], in0=po_f[:NS, 0:1024].rearrange("p (h d) -> p h d", d=128), in1=tmpv[:], op=ALU.add),
                         reads=["pof", "tmpv"], writes=["numA"])
                    P.op("dve", lambda e: e.tensor_tensor(out=denA[:], in0=pz[:NS, 1, 0:8], in1=enew[:, 0:8], op=ALU.add),
                         reads=[("pz", 1), "enew"], writes=["denA"])
                    P.op("dve", lambda e: e.tensor_tensor(out=denA[:], in0=denA[:], in1=esink[:NS, 0:8], op=ALU.add),
                         reads=["denA", "esink"], writes=["denA"])
                else:
                    P.op("dve", lambda e, kb=kb, hb=hb: e.tensor_tensor(
                        out=tmpv[:, 0:4, :], in0=vs[:, kb * 128:(kb + 4) * 128].rearrange("p (h d) -> p h d", d=128),
                        in1=enew[:, hb:hb + 4].unsqueeze(2).to_broadcast([NS, 4, 128]), op=ALU.mult),
                        reads=["svs", "enew"], writes=["tmpv"])
                    if gname == "B1":
                        P.op("dve", lambda e: e.tensor_tensor(out=numB[:], in0=po_f[:NS, 0:512].rearrange("p (h d) -> p h d", d=128), in1=tmpv[:, 0:4, :], op=ALU.add),
                             reads=["pof", "tmpv"], writes=["numB"])
                        P.op("dve", lambda e, hb=hb: e.tensor_tensor(out=denB[:], in0=pz[:NS, 1, 0:4], in1=enew[:, hb:hb + 4], op=ALU.add),
                             reads=[("pz", 1), "enew"], writes=["denB"])
                    else:
                        P.op("dve", lambda e: e.tensor_tensor(out=numB[:], in0=numB[:], in1=tmpv[:, 0:4, :], op=ALU.add), reads=["numB", "tmpv"], writes=["numB"])
                        P.op("dve", lambda e: e.tensor_tensor(out=numB[:], in0=po_f[:NS, 0:512].rearrange("p (h d) -> p h d", d=128), in1=numB[:], op=ALU.add),
                             reads=["pof", "numB"], writes=["numB"])
                        P.op("dve", lambda e, hb=hb: e.tensor_tensor(out=denB[:], in0=denB[:], in1=enew[:, hb:hb + 4], op=ALU.add), reads=["denB", "enew"], writes=["denB"])
                        P.op("dve", lambda e: e.tensor_tensor(out=denB[:], in0=pz[:NS, 1, 0:4], in1=denB[:], op=ALU.add), reads=[("pz", 1), "denB"], writes=["denB"])
            for (num, den, nh_, dstT, nm) in ((numA, denA, 8, oaT, "oaTs"), (numB, denB, 4, obT, "obTs")):
                P.op("dve", lambda e, den=den: e.reciprocal(out=den[:], in_=den[:]), reads=["denA", "denB"], writes=["denA", "denB"])
                P.op("dve", lambda e, num=num, den=den, nh_=nh_: e.tensor_tensor(out=num[:], in0=num[:], in1=den[:].unsqueeze(2).to_broadcast([NS, nh_, 128]), op=ALU.mult),
                     reads=["numA", "numB", "denA", "denB"], writes=["numA", "numB"])
                for h0 in range(0, nh_, 4):
                    for h in range(4):
                        P.op("pe", lambda e, num=num, h0=h0, h=h: e.transpose(out=ptr[:, 0, h * 128:h * 128 + NS], in_=num[:, h0 + h, :], identity=ident[:NS, :NS]),
                             reads=["numA", "numB", "ident"], writes=[("ptr", 0)])
                    P.op("act", lambda e, dstT=dstT, h0=h0: e.activation(
                        out=dstT[:, h0:h0 + 4, 1024:1024 + NS], in_=ptr[:, 0, :].rearrange("p (a b) -> p a b", b=128)[:, :, :NS], func=AF.Copy),
                        reads=[("ptr", 0)], writes=[(nm, h0)])
            P.barrier()

        if debug:
            P.op("sp", lambda e: e.dma_start(out=dbg["oaT"], in_=oaT[:]), reads=[], writes=["dbg1"], dma=True)
            P.op("sp", lambda e: e.dma_start(out=dbg["obT"], in_=obT[:]), reads=[], writes=["dbg2"], dma=True)
        if stop_after == 3:
            P.barrier(); P.emit(); esA.close(); return nc
        esR = ExitStack()
        mT = esR.enter_context(nc.sbuf_tensor("mT", [128, 16, 1040], BF16, side="right"))
        pst_f = pst[:].rearrange("p a b -> p (a b)")
        po_f = po[:].rearrange("p a b -> p (a b)")
        with ExitStack() as es4:
            def sb4(name, shape, dt=F32):
                return es4.enter_context(nc.sbuf_tensor(name, list(shape), dt))
            wr4 = [sb4(f"wr4_{i}", [128, 16, 256], BF16) for i in range(2)]
            ring[:] = list(wring) + wr4
            sga = [sb4(f"sga{i}", [128, 512]) for i in range(2)]
            sgb = [sb4(f"sgb{i}", [128, 512]) for i in range(2)]
            t1 = [sb4(f"t1_{i}", [128, 512]) for i in range(2)]
            t2 = [sb4(f"t2_{i}", [128, 512]) for i in range(2)]
            psets = [(pz[:, 0, :], pz[:, 1, :], pst[:, 0, :], pst[:, 1, :], ("pz", 0), ("pz", 1), ("pst", 0), ("pst", 1)),
                     (ptr[:, 0, :], ptr[:, 1, :], po_f[:, 0:512], po_f[:, 512:1024], ("ptr", 0), ("ptr", 1), ("pof", 0), ("pof", 1))]
            blocks = [(XT_O, 0, 512), (XT_O + 512, 512, 512), (XT_S, 1024, NS)]
            it = 0
            for c in range(8):
                wga, kga = load_w(w_in, 6144 + c * 256, 256)
                wgb, kgb = load_w(w_in, 8192 + c * 256, 256)
                wa, ka_ = load_w(wba, c * 256, 256, nk=8)
                wb_, kb_ = load_w(wbb, c * 256, 256, nk=4)
                for j in range(2):
                    dm = c * 2 + j
                    for (toff, ooff, n) in blocks:
                        s_ = it % 2
                        it += 1
                        GA, GB, YA, YB, kGA, kGB, kYA, kYB = psets[s_]
                        for (dst, dkey, wt_, wk_, nk_, src, skey_) in ((GA, kGA, wga, kga, 16, lambda k, toff=toff, n=n: xnT[:, k, toff:toff + n], "xn"),
                                                                       (GB, kGB, wgb, kgb, 16, lambda k, toff=toff, n=n: xnT[:, k, toff:toff + n], "xn"),
                                                                       (YA, kYA, wa, ka_, 8, lambda k, ooff=ooff, n=n: oaT[:, k, ooff:ooff + n], "oa"),
                                                                       (YB, kYB, wb_, kb_, 4, lambda k, ooff=ooff, n=n: obT[:, k, ooff:ooff + n], "ob")):
                            for k in range(nk_):
                                P.op("pe", lambda e, dst=dst, wt_=wt_, k=k, j=j, src=src, nk_=nk_, n=n: e.matmul(
                                    dst[:, :n], lhsT=wt_[:, k, j * 128:(j + 1) * 128], rhs=src(k), start=(k == 0), stop=(k == nk_ - 1)),
                                    reads=[wk_], writes=[dkey])
                        P.op("act", lambda e, GA=GA, s_=s_, n=n: e.activation(out=sga[s_][:, :n], in_=GA[:, :n], func=AF.Sigmoid), reads=[kGA], writes=[("sga", s_)])
                        P.op("act", lambda e, GB=GB, s_=s_, n=n: e.activation(out=sgb[s_][:, :n], in_=GB[:, :n], func=AF.Sigmoid), reads=[kGB], writes=[("sgb", s_)])
                        P.op("dve", lambda e, YA=YA, s_=s_, n=n: e.tensor_tensor(out=t1[s_][:, :n], in0=YA[:, :n], in1=sga[s_][:, :n], op=ALU.mult),
                             reads=[kYA, ("sga", s_)], writes=[("t1", s_)])
                        P.op("dve", lambda e, YB=YB, s_=s_, n=n: e.tensor_tensor(out=t2[s_][:, :n], in0=YB[:, :n], in1=sgb[s_][:, :n], op=ALU.mult),
                             reads=[kYB, ("sgb", s_)], writes=[("t2", s_)])
                        P.op("dve", lambda e, s_=s_, n=n, dm=dm, ooff=ooff: e.tensor_tensor(out=mT[:, dm, ooff:ooff + n], in0=t1[s_][:, :n], in1=t2[s_][:, :n], op=ALU.add),
                             reads=[("t1", s_), ("t2", s_)], writes=[("mT", dm, ooff)])
            P.barrier()
        esA.close()

        es5 = ExitStack()
        def sb5(name, shape, dt=F32):
            return es5.enter_context(nc.sbuf_tensor(name, list(shape), dt))
        h_t = sb5("h_t", [128, 9, D])
        with ExitStack() as es5b:
            def sb5b(name, shape, dt=F32):
                return es5b.enter_context(nc.sbuf_tensor(name, list(shape), dt))
            ring[:] = [sb5b(f"wr5_{i}", [128, 16, 256], BF16) for i in range(2)]
            xblk = [sb5b(f"xblk{i}", [128, 256]) for i in range(3)]
            it = 0
            for c in range(8):
                wt, wkey = load_w(wout, c * 256, 256)
                for i in range(9):
                    n = 128 if i < 8 else NS
                    ooff = i * 128
                    b = it % 2
                    xs_ = it % 3
                    it += 1
                    srcx = xo[i * 128:(i + 1) * 128, c * 256:(c + 1) * 256] if i < 8 else xs[:, c * 256:(c + 1) * 256]
                    P.op("sp", lambda e, xs_=xs_, srcx=srcx, n=n: e.dma_start(out=xblk[xs_][:n, :], in_=srcx), writes=[("xblk", xs_)], dma=True)
                    for k in range(16):
                        P.op("pe", lambda e, k=k, b=b, n=n, ooff=ooff, wt=wt: e.matmul(pz[:n, b, :256], lhsT=mT[:, k, ooff:ooff + n], rhs=wt[:, k, :256],
                                                                                  start=(k == 0), stop=(k == 15)),
                             reads=[wkey], writes=[("pz", b)])
                    P.op("dve", lambda e, b=b, n=n, i=i, c=c, xs_=xs_: e.tensor_tensor(out=h_t[:n, i, c * 256:(c + 1) * 256], in0=pz[:n, b, :256],
                                                                                    in1=xblk[xs_][:n, :], op=ALU.add),
                         reads=[("pz", b), ("xblk", xs_)], writes=[("h", i)])
            P.barrier()
        esR.close()
        if debug:
            P.op("sp", lambda e: e.dma_start(out=yp, in_=h_t[:, 0:8, :].rearrange("p i d -> p i d")) if False else e.dma_start(out=yp.rearrange("(i p) d -> p i d", p=128), in_=h_t[:, 0:8, :]),
                 reads=[], writes=["dbgh"], dma=True)
            P.op("sp", lambda e: e.dma_start(out=ys, in_=h_t[:NS, 8, :]), reads=[], writes=["dbghs"], dma=True)
            P.barrier(); P.emit(); es5.close(); return nc

        with ExitStack() as es6:
            def sb6(name, shape, dt=F32):
                return es6.enter_context(nc.sbuf_tensor(name, list(shape), dt))
            wqb = sb6("wqb", [128, 16, 1024], BF16)
            w2rep = sb6("w2rep", [128, D])
            skb = sb6("skb", [128, 256])
            iota16 = sb6("iota16t", [128, 16]); thr16 = sb6("thr16", [128, 16])
            hn = sb6("hn", [128, D])
            hnT = sb6("hnT", [128, 16, 128], BF16)
            qf = sb6("qf", [128, 1024])
            qT = sb6("qT", [128, 8, 128])
            big = sb6("big", [128, 2048])
            sS = big[:]
            s2 = sb6("s2", [128, 256])
            vals = sb6("vals", [128, 16, 16]); idxu = sb6("idxu", [128, 16, 16], U32); idxf = sb6("idxf", [128, 16, 16])
            cand = big[:].rearrange("p (h c) -> p h c", c=256)
            best = sb6("best", [128, 8, 16]); bcu = sb6("bcu", [128, 8, 16], U32); bcf = sb6("bcf", [128, 128])
            akf = sb6("akf", [128, 128]); bkf = sb6("bkf", [128, 128])
            eq = big[:].rearrange("p (s a) -> p s a", a=16)
            i1f = sb6("i1f", [128, 128]); i2f = sb6("i2f", [128, 128]); ef = sb6("ef", [128, 128]); eidx = sb6("eidx", [128, 128], U32)
            gate = sb6("gate", [128, 8, 16]); gsum = sb6("gsum", [128, 8])
            act_ = sb6("act_", [128, 128]); wgt = sb6("wgt", [128, 128])
            NUB = 10
            ubuf = [sb6(f"ubuf{i}", [128, D], BF16) for i in range(NUB)]
            junk6 = sb6("junk6", [128, D], BF16)
            st6 = sb6("st6", [128, 8])
            for c4 in range(4):
                P.op("pool", lambda e, c4=c4: e.dma_start(out=wqb[:, :, c4 * 256:(c4 + 1) * 256],
                                                         in_=wq[:, c4 * 256:(c4 + 1) * 256].rearrange("(k p) n -> p k n", p=128)),
                     writes=[("wqb", c4)], dma=True)
            P.op("sp", lambda e: e.dma_start(out=w2rep[:], in_=norm2_w.partition_broadcast(128)), writes=["w2rep"], dma=True)
            P.op("sp", lambda e: e.dma_start(out=iota16[:], in_=iota16_d), writes=["iota16"], dma=True)
            P.op("dve", lambda e: e.tensor_scalar(out=thr16[:], in0=iota16[:], scalar1=16.0, scalar2=None, op0=ALU.mult), reads=["iota16"], writes=["thr16"])
            P.op("dve", lambda e: e.memset(skb[:], 0.0), writes=["skb"])
            P.op("sp", lambda e: e.dma_start(out=skb[0:64, 0:128], in_=subkT[0]), reads=["skb"], writes=["skb"], dma=True)
            P.op("sp", lambda e: e.dma_start(out=skb[64:128, 128:256], in_=subkT[1]), reads=["skb"], writes=["skb"], dma=True)
            ucnt = [0]
            for i in range(9):
                n = 128 if i < 8 else NS
                hi = h_t[:n, i, :]
                hk = ("h", i)
                P.op("act", lambda e, hi=hi, n=n: e.activation(out=junk6[:n, :], in_=hi, func=AF.Square, accum_out=st6[:n, 0:1]), reads=[hk], writes=["junk6", "st6"])
                P.op("act", lambda e, n=n: e.activation(out=st6[:n, 1:2], in_=st6[:n, 0:1], func=AF.Ln, scale=1.0 / D, bias=EPS), reads=["st6"], writes=["st6"])
                P.op("act", lambda e, n=n: e.activation(out=st6[:n, 2:3], in_=st6[:n, 1:2], func=AF.Exp, scale=-0.5), reads=["st6"], writes=["st6"])
                P.op("dve", lambda e, hi=hi, n=n: e.scalar_tensor_tensor(out=hn[:n, :], in0=hi, scalar=st6[:n, 2:3], in1=w2rep[:n, :], op0=ALU.mult, op1=ALU.mult),
                     reads=[hk, "st6", "w2rep"], writes=["hn"])
                for k4 in range(4):
                    bank = k4 % 2
                    for kk in range(4):
                        k = k4 * 4 + kk
                        P.op("pe", lambda e, k=k, kk=kk, bank=bank, n=n: e.transpose(out=ptr[:, bank, kk * 128:kk * 128 + n], in_=hn[:n, k * 128:(k + 1) * 128],
                                                                                     identity=ident[:n, :n]), reads=["hn", "ident"], writes=[("ptr", bank)])
                    P.op("act", lambda e, k4=k4, bank=bank, n=n: e.activation(out=hnT[:, k4 * 4:(k4 + 1) * 4, :n],
                                                                            in_=ptr[:, bank, :].rearrange("p (a b) -> p a b", b=128)[:, :, :n], func=AF.Copy),
                         reads=[("ptr", bank)], writes=["hnT"])
                for half in range(2):
                    for k in range(16):
                        P.op("pe", lambda e, k=k, half=half, n=n: e.matmul(pz[:n, half, :], lhsT=hnT[:, k, :n], rhs=wqb[:, k, half * 512:(half + 1) * 512],
                                                                        start=(k == 0), stop=(k == 15)),
                             reads=["hnT"] + [("wqb", c4) for c4 in range(4)], writes=[("pz", half)])
                P.op("act", lambda e, n=n: e.activation(out=qf[:n, :].rearrange("p (a b) -> p a b", a=2), in_=pz[:n, :, :], func=AF.Copy),
                     reads=[("pz", 0), ("pz", 1)], writes=["qf"])
                for h4 in range(2):
                    for hh in range(4):
                        h = h4 * 4 + hh
                        P.op("pe", lambda e, h=h, hh=hh, h4=h4, n=n: e.transpose(out=ptr[:, h4, hh * 128:hh * 128 + n], in_=qf[:n, h * 128:(h + 1) * 128],
                                                                                identity=ident[:n, :n]), reads=["qf", "ident"], writes=[("ptr", h4)])
                    P.op("act", lambda e, h4=h4, n=n: e.activation(out=qT[:, h4 * 4:(h4 + 1) * 4, :n],
                                                                 in_=ptr[:, h4, :].rearrange("p (a b) -> p a b", b=128)[:, :, :n], func=AF.Copy),
                         reads=[("ptr", h4)], writes=["qT"])
                for h in range(8):
                    dst = pst_f[:n, h * 256:(h + 1) * 256] if h < 4 else po_f[:n, (h - 4) * 256:(h - 3) * 256]
                    P.op("pe", lambda e, h=h, dst=dst, n=n: e.matmul(dst, lhsT=qT[:, h, :n], rhs=skb[:, :], start=True, stop=True),
                         reads=["qT", "skb"], writes=["pstf" if h < 4 else "pof"])
                P.op("act", lambda e, n=n: e.activation(out=sS[:n, 0:1024], in_=pst_f[:n, :], func=AF.Copy), reads=["pstf"], writes=["big"])
                P.op("dve", lambda e, n=n: e.tensor_copy(out=sS[:n, 1024:2048], in_=po_f[:n, :]), reads=["pof"], writes=["big"])
                for hc in range(16):
                    src = sS[:n, hc * 128:(hc + 1) * 128]
                    P.op("dve", lambda e, src=src, hc=hc, n=n: e.max(out=vals[:n, hc, 0:8], in_=src), reads=["big"], writes=["vals"])
                    P.op("dve", lambda e, src=src, hc=hc, n=n: e.max_index(out=idxu[:n, hc, 0:8], in_max=vals[:n, hc, 0:8], in_values=src),
                         reads=["big", "vals"], writes=["idxu"])
                    P.op("dve", lambda e, src=src, hc=hc, n=n: e.match_replace(out=s2[:n, 0:128], in_to_replace=vals[:n, hc, 0:8], in_values=src, imm_value=-1e30),
                         reads=["big", "vals"], writes=["s2"])
                    P.op("dve", lambda e, hc=hc, n=n: e.max(out=vals[:n, hc, 8:16], in_=s2[:n, 0:128]), reads=["s2"], writes=["vals"])
                    P.op("dve", lambda e, hc=hc, n=n: e.max_index(out=idxu[:n, hc, 8:16], in_max=vals[:n, hc, 8:16], in_values=s2[:n, 0:128]),
                         reads=["s2", "vals"], writes=["idxu"])
                P.op("dve", lambda e, n=n: e.tensor_copy(out=idxf[:n], in_=idxu[:n]), reads=["idxu"], writes=["idxf"])
                v4 = vals[:n].rearrange("p (h c) k -> p h c k", c=2)
                P.op("dve", lambda e, v4=v4, n=n: e.tensor_tensor(out=cand[:n].rearrange("p h (a b) -> p h a b", b=16),
                                                                 in0=v4[:, :, 0, :].unsqueeze(3).to_broadcast([n, 8, 16, 16]),
                                                                 in1=v4[:, :, 1, :].unsqueeze(2).to_broadcast([n, 8, 16, 16]), op=ALU.add),
                     reads=["vals"], writes=["big"])
                for h in range(8):
                    src = cand[:n, h, :]
                    P.op("dve", lambda e, src=src, h=h, n=n: e.max(out=best[:n, h, 0:8], in_=src), reads=["big"], writes=["best"])
                    P.op("dve", lambda e, src=src, h=h, n=n: e.max_index(out=bcu[:n, h, 0:8], in_max=best[:n, h, 0:8], in_values=src),
                         reads=["big", "best"], writes=["bcu"])
                    P.op("dve", lambda e, src=src, h=h, n=n: e.match_replace(out=s2[:n, :], in_to_replace=best[:n, h, 0:8], in_values=src, imm_value=-1e30),
                         reads=["big", "best"], writes=["s2"])
                    P.op("dve", lambda e, h=h, n=n: e.max(out=best[:n, h, 8:16], in_=s2[:n, :]), reads=["s2"], writes=["best"])
                    P.op("dve", lambda e, h=h, n=n: e.max_index(out=bcu[:n, h, 8:16], in_max=best[:n, h, 8:16], in_values=s2[:n, :]),
                         reads=["s2", "best"], writes=["bcu"])
                P.op("dve", lambda e, n=n: e.tensor_copy(out=bcf[:n, :], in_=bcu[:n].rearrange("p h k -> p (h k)")), reads=["bcu"], writes=["bcf"])
                P.op("dve", lambda e, n=n: e.tensor_tensor(out=eq[:n], in0=bcf[:n, :].unsqueeze(2).to_broadcast([n, 128, 16]),
                                                          in1=thr16[:n, :].unsqueeze(1).to_broadcast([n, 128, 16]), op=ALU.is_ge),
                     reads=["bcf", "thr16"], writes=["big"])
                P.op("dve", lambda e, n=n: e.tensor_reduce(out=akf[:n, :], in_=eq[:n], axis=AX.X, op=ALU.add), reads=["big"], writes=["akf"])
                P.op("dve", lambda e, n=n: e.tensor_scalar(out=akf[:n, :], in0=akf[:n, :], scalar1=-1.0, scalar2=None, op0=ALU.add), reads=["akf"], writes=["akf"])
                P.op("dve", lambda e, n=n: e.scalar_tensor_tensor(out=bkf[:n, :], in0=akf[:n, :], scalar=-16.0, in1=bcf[:n, :], op0=ALU.mult, op1=ALU.add),
                     reads=["akf", "bcf"], writes=["bkf"])
                i4 = idxf[:n].rearrange("p (h c) k -> p h c k", c=2)
                for (sel_f, cidx, dst_i, nm) in ((akf, 0, i1f, "i1f"), (bkf, 1, i2f, "i2f")):
                    P.op("dve", lambda e, sel_f=sel_f, n=n: e.tensor_tensor(out=eq[:n], in0=iota16[:n, :].unsqueeze(1).to_broadcast([n, 128, 16]),
                                                                           in1=sel_f[:n, :].unsqueeze(2).to_broadcast([n, 128, 16]), op=ALU.is_equal),
                         reads=["iota16", "akf", "bkf"], writes=["big"])
                    P.op("dve", lambda e, cidx=cidx, i4=i4, n=n: e.tensor_tensor(out=eq[:n].rearrange("p (h k) a -> p h k a", k=16),
                                                                               in0=eq[:n].rearrange("p (h k) a -> p h k a", k=16),
                                                                               in1=i4[:, :, cidx, :].unsqueeze(2).to_broadcast([n, 8, 16, 16]), op=ALU.mult),
                         reads=["big", "idxf"], writes=["big"])
                    P.op("dve", lambda e, dst_i=dst_i, n=n: e.tensor_reduce(out=dst_i[:n, :], in_=eq[:n], axis=AX.X, op=ALU.add), reads=["big"], writes=[nm])
                P.op("dve", lambda e, n=n: e.scalar_tensor_tensor(out=ef[:n, :], in0=i1f[:n, :], scalar=128.0, in1=i2f[:n, :], op0=ALU.mult, op1=ALU.add),
                     reads=["i1f", "i2f"], writes=["ef"])
                P.op("dve", lambda e, n=n: e.tensor_copy(out=eidx[:n, :], in_=ef[:n, :]), reads=["ef"], writes=["eidx"])
                P.op("dve", lambda e, n=n: e.tensor_tensor(out=gate[:n], in0=best[:n], in1=best[:n, :, 0:1].to_broadcast([n, 8, 16]), op=ALU.subtract),
                     reads=["best"], writes=["gate"])
                P.op("act", lambda e, n=n: e.activation(out=gate[:n], in_=gate[:n], func=AF.Exp), reads=["gate"], writes=["gate"])
                P.op("dve", lambda e, n=n: e.tensor_reduce(out=gsum[:n, :], in_=gate[:n], axis=AX.X, op=ALU.add), reads=["gate"], writes=["gsum"])
                P.op("dve", lambda e, n=n: e.reciprocal(out=gsum[:n, :], in_=gsum[:n, :]), reads=["gsum"], writes=["gsum"])
                P.op("dve", lambda e, n=n: e.tensor_tensor(out=gate[:n], in0=gate[:n], in1=gsum[:n, :].unsqueeze(2).to_broadcast([n, 8, 16]), op=ALU.mult),
                     reads=["gate", "gsum"], writes=["gate"])
                for slot in range(128):
                    ub = ucnt[0] % NUB
                    ucnt[0] += 1
                    P.op("pool", lambda e, ub=ub, slot=slot, n=n: e.indirect_dma_start(
                        out=ubuf[ub][:n, :], out_offset=None, in_=pu, in_offset=bass.IndirectOffsetOnAxis(ap=eidx[:n, slot:slot + 1], axis=0)),
                        reads=["eidx"], writes=[("ubuf", ub)], dma=True)
                    P.op("dve", lambda e, ub=ub, slot=slot, n=n: e.scalar_tensor_tensor(
                        out=junk6[:n, :], in0=ubuf[ub][:n, :], scalar=1.0, in1=hn[:n, :], op0=ALU.mult, op1=ALU.mult,
                        accum_out=act_[:n, slot:slot + 1]),
                        reads=[("ubuf", ub), "hn"], writes=["junk6", "act_"])
                P.op("act", lambda e, n=n: e.activation(out=wgt[:n, :], in_=act_[:n, :], func=AF.Gelu), reads=["act_"], writes=["wgt"])
                P.op("dve", lambda e, n=n: e.tensor_tensor(out=wgt[:n, :], in0=wgt[:n, :], in1=gate[:n].rearrange("p h k -> p (h k)"), op=ALU.mult),
                     reads=["wgt", "gate"], writes=["wgt"])
                for slot in range(128):
                    ub = ucnt[0] % NUB
                    ucnt[0] += 1
                    P.op("pool", lambda e, ub=ub, slot=slot, n=n: e.indirect_dma_start(
                        out=ubuf[ub][:n, :], out_offset=None, in_=pv, in_offset=bass.IndirectOffsetOnAxis(ap=eidx[:n, slot:slot + 1], axis=0)),
                        reads=["eidx"], writes=[("ubuf", ub)], dma=True)
                    P.op("dve", lambda e, ub=ub, slot=slot, n=n, hi=hi: e.scalar_tensor_tensor(
                        out=hi, in0=ubuf[ub][:n, :], scalar=wgt[:n, slot:slot + 1], in1=hi, op0=ALU.mult, op1=ALU.add),
                        reads=[("ubuf", ub), "wgt", hk], writes=[hk])
                dsty = yp[i * 128:(i + 1) * 128, :] if i < 8 else ys
                P.op("sp", lambda e, dsty=dsty, hi=hi: e.dma_start(out=dsty, in_=hi), reads=[hk], writes=[("y", i)], dma=True)
            P.barrier()
        es5.close()
        P.barrier()
        P.emit()
    return nc


def make_in_maps(inp, with_peer=True):
    xp = np.asarray(inp["x_prompt"], np.float32)
    xs = np.asarray(inp["x_sample"], np.float32)[:, 0, :]
    tabs = {"tab_" + u["name"]: np.ascontiguousarray(unit_variants(u)[2].reshape(128, -1)) for u in UNITS}
    shared = {
        "norm1_w": inp["norm1_w"][0], "w_in": inp["w_in"][0], "qna": inp["q_norm_a"][0], "kna": inp["k_norm_a"][0],
        "sink": inp["sink_a"][0], "qnb": inp["q_norm_b"][0], "knb": inp["k_norm_b"][0],
        "wba": inp["w_branch_a"][0], "wbb": inp["w_branch_b"][0], "wout": inp["w_out"][0],
        "norm2_w": inp["norm2_w"][0], "wq": inp["peer_wq"][0], "subk": inp["peer_subkeys"][0],
    }
    if with_peer:
        shared["pu"] = inp["peer_u"][0]; shared["pv"] = inp["peer_v"][0]
    shared = {k: np.ascontiguousarray(np.asarray(v, np.float32)) for k, v in shared.items()}
    shared.update(tabs)
    sb_ = np.zeros((128, 20), np.float32)
    ii = np.arange(128, dtype=np.float64)
    for h in range(20):
        dil = 1 if h < 12 else (4 if h < 16 else 16)
        sb_[:, h] = -SLOPES[h] * dil * (128.0 - ii)
    shared["sbias"] = sb_
    shared["dmask"] = np.ascontiguousarray(np.broadcast_to(np.eye(NS, dtype=np.float32).reshape(1, NS * NS), (128, NS * NS)))
    shared["iota16"] = np.ascontiguousarray(np.broadcast_to(np.arange(16, dtype=np.float32)[None, :], (128, 16)))
    shared["subkT"] = np.ascontiguousarray(np.asarray(inp["peer_subkeys"], np.float32)[0].transpose(0, 2, 1))
    maps = []
    for c in range(8):
        b, half = c // 2, c % 2
        m = dict(shared)
        m["xo"] = np.ascontiguousarray(xp[b, half * 1024:(half + 1) * 1024])
        m["xh"] = np.ascontiguousarray(xp[b, 0:1024]) if half == 1 else np.zeros((1024, D), np.float32)
        m["xs"] = np.ascontiguousarray(xs[c * NS:(c + 1) * NS])
        m["flag"] = np.ascontiguousarray(np.broadcast_to(np.array([[1.0, float(half)]], np.float32), (128, 2)))
        for nm, key in (("ca", "cache_a_kv"), ("cb1", "cache_b1_kv"), ("cb2", "cache_b2_kv"), ("cb3", "cache_b3_kv")):
            a = np.asarray(inp[key], np.float32)[0, c * NS:(c + 1) * NS]
            m[nm] = np.ascontiguousarray(a.reshape(NS, a.shape[1], -1))
        maps.append(m)
    return maps


_NC_CACHE = {}


def kernel(**inputs):
    if "nc" not in _NC_CACHE:
        _NC_CACHE["nc"] = build()
    nc = _NC_CACHE["nc"]
    maps = make_in_maps(inputs)
    res = run_bass_kernel_spmd(nc, maps, core_ids=list(range(8))).results
    y_p = np.zeros((4, 2048, D), np.float32)
    y_s = np.zeros((128, 1, D), np.float32)
    a_p = np.zeros((1, 4, 128, 2, 2, 128), np.float32)
    b1_p = np.zeros((1, 4, 128, 2, 4, 128), np.float32)
    b2_p = np.zeros((1, 4, 512, 2, 4, 128), np.float32)
    b3_p = np.zeros((1, 4, 2048, 2, 4, 128), np.float32)
    a_s = np.zeros((1, 128, 128, 2, 2, 128), np.float32)
    b1_s = np.zeros((1, 128, 128, 2, 4, 128), np.float32)
    b2_s = np.zeros((1, 128, 512, 2, 4, 128), np.float32)
    b3_s = np.zeros((1, 128, 2048, 2, 4, 128), np.float32)
    for c in range(8):
        b, half = c // 2, c % 2
        r = res[c]
        y_p[b, half * 1024:(half + 1) * 1024] = r["yp"]
        y_s[c * NS:(c + 1) * NS, 0] = r["ys"]
        b3_p[0, b, half * 1024:(half + 1) * 1024] = r["kvb3_p"].reshape(1024, 2, 4, 128)
        if half == 1:
            a_p[0, b] = r["kva_p"].reshape(128, 2, 2, 128)
            b1_p[0, b] = r["kvb1_p"].reshape(128, 2, 4, 128)
            b2_p[0, b] = r["kvb2_p"].reshape(512, 2, 4, 128)
        a_s[0, c * NS:(c + 1) * NS] = r["kva_s"].reshape(NS, 128, 2, 2, 128)
        b1_s[0, c * NS:(c + 1) * NS] = r["kvb1_s"].reshape(NS, 128, 2, 4, 128)
        b2_s[0, c * NS:(c + 1) * NS] = r["kvb2_s"].reshape(NS, 512, 2, 4, 128)
        b3_s[0, c * NS:(c + 1) * NS] = r["kvb3_s"].reshape(NS, 2048, 2, 4, 128)
    return (y_p, y_s, a_p, b1_p, b2_p, b3_p, a_s, b1_s, b2_s, b3_s)
```

```python
import numpy as np
from contextlib import ExitStack
import concourse.bass as bass
import concourse.mybir as mybir
from concourse.bass_utils import run_bass_kernel_spmd

F32 = mybir.dt.float32
BF16 = mybir.dt.bfloat16
I32 = mybir.dt.int32
U32 = mybir.dt.uint32
AF = mybir.ActivationFunctionType
ALU = mybir.AluOpType
AX = mybir.AxisListType

STREAMS = ("pe", "act", "dve", "pool", "sp")
NDMA_SEMS = 8


class Prog:
    def __init__(self, nc):
        self.nc = nc
        self.ops = {s: [] for s in STREAMS}
        self.semnames = list(STREAMS) + [f"d{s}{j}" for s in ("sp", "act", "pool") for j in range(NDMA_SEMS)]
        self.count = {n: 0 for n in self.semnames}
        self.dma_i = {"sp": 0, "act": 0, "pool": 0}
        self.last_w = {}
        self.readers = {}
        self.waited = {s: {} for s in STREAMS}
        self.n_ops = 0

    def _deps(self, reads, writes):
        deps = {}

        def add(d):
            if d is not None and d[1] > deps.get(d[0], 0):
                deps[d[0]] = d[1]

        for k in reads:
            add(self.last_w.get(k))
        for k in writes:
            add(self.last_w.get(k))
            for r in self.readers.get(k, ()):
                add(r)
        return deps

    def op(self, stream, fn, reads=(), writes=(), dma=False):
        deps = self._deps(reads, writes)
        if dma:
            dname = f"d{stream}{self.dma_i[stream] % NDMA_SEMS}"
            if self.count[dname] > 0:
                deps[dname] = max(deps.get(dname, 0), self.count[dname])
        if stream == "pe":
            deps.pop("pe", None)
        w = self.waited[stream]
        waits = []
        for sname, v in deps.items():
            if v > w.get(sname, 0):
                waits.append((sname, v))
                w[sname] = v
        if dma:
            j = self.dma_i[stream] % NDMA_SEMS
            self.dma_i[stream] += 1
            sname = f"d{stream}{j}"
            inc = 16
        else:
            sname = stream
            inc = 1
        self.count[sname] += inc
        val = self.count[sname]
        self.ops[stream].append((waits, fn, sname, inc))
        for k in reads:
            self.readers.setdefault(k, []).append((sname, val))
        for k in writes:
            self.last_w[k] = (sname, val)
            self.readers[k] = []
        self.n_ops += 1
        return (sname, val)

    def barrier(self):
        waits = [(n, c) for n, c in self.count.items() if c > 0]
        for s in STREAMS:
            w = self.waited[s]
            ws = [(n, c) for n, c in waits if c > w.get(n, 0)]
            for n, c in ws:
                w[n] = c
            if ws:
                self.ops[s].append((ws, None, None, 0))
        self.last_w = {}
        self.readers = {}

    def emit(self):
        nc = self.nc
        with ExitStack() as es:
            sems = {n: es.enter_context(nc.semaphore(n)) for n in self.semnames}
            block = es.enter_context(nc.Block())

            def make(stream):
                def body(eng):
                    for waits, fn, sname, inc in self.ops[stream]:
                        for wn, wv in waits:
                            eng.wait_ge(sems[wn], wv)
                        if fn is not None:
                            fn(eng).then_inc(sems[sname], inc)
                return body

            block.tensor(make("pe"))
            block.scalar(make("act"))
            block.vector(make("dve"))
            block.gpsimd(make("pool"))
            block.sync(make("sp"))


D = 2048
NTOK = 1024
NT = 8
NS = 16
NIN = 10240
EPS = 1e-6
SCALE = 128.0 ** -0.5
SLOPES = [float(2.0 ** (-8.0 * i / 20.0)) for i in range(1, 21)]
XT_H, XT_O, XT_S = 0, 1024, 2048
NXT = 2064

GROUPS = {
    "A": dict(nh=1, nd=2, dil=1, win=128, H=2, L=128),
    "B1": dict(nh=1, nd=2, dil=1, win=128, H=4, L=128),
    "B2": dict(nh=4, nd=5, dil=4, win=512, H=4, L=512),
    "B3": dict(nh=8, nd=16, dil=16, win=2048, H=4, L=2048),
}


def make_units():
    units = []
    for kvh in range(2):
        units.append(dict(name=f"A{kvh}", grp="A", nq=4, nk=1,
                          qjobs=[(kvh * 512, 256, 0), (kvh * 512 + 256, 256, 2)],
                          kcol=1024 + kvh * 128, vcol=1280 + kvh * 128, kvhead0=kvh,
                          slopes=[SLOPES[kvh * 4 + g] for g in range(4)], ohead0=kvh * 4))
    for g, gname in enumerate(("B1", "B2", "B3")):
        for hp in range(2):
            units.append(dict(name=f"{gname}_{hp}", grp=gname, nq=2, nk=2,
                              qjobs=[(1536 + g * 512 + hp * 256, 256, 0)],
                              kcol=3072 + g * 512 + hp * 256, vcol=4608 + g * 512 + hp * 256,
                              kvhead0=hp * 2,
                              slopes=[SLOPES[8 + g * 4 + hp * 2 + j] for j in range(2)], ohead0=hp * 2))
    return units


UNITS = make_units()


def unit_variants(u):
    G = GROUPS[u["grp"]]
    nd, dil, win = G["nd"], G["dil"], G["win"]
    s = np.arange(128)[:, None].astype(np.float64)
    q = np.arange(128)[None, :].astype(np.float64)
    if u["grp"] == "B3":
        nvar = 2
        vmap = [(0, [0.0] * u["nq"])] + [(1, [-sl * 128.0 * (dl - 1) for sl in u["slopes"]]) for dl in range(1, nd)]
        var_delta = [0, 1]
    else:
        nvar = nd
        vmap = [(dl, [0.0] * u["nq"]) for dl in range(nd)]
        var_delta = list(range(nd))
    tab = np.zeros((128, u["nq"], nvar, 128), np.float32)
    for j, sl in enumerate(u["slopes"]):
        for v, dl in enumerate(var_delta):
            dist = 128.0 * dl + q - s
            ok = (dist >= 0) & (dist <= win) & (np.mod(dist, dil) == 0)
            tab[:, j, v, :] = np.where(ok, np.exp(-sl * np.where(ok, dist, 0.0)), 0.0)
    return nvar, vmap, tab


def build(stop_after=None, debug=False, bis=None):
    nc = bass.Bass("TRN2", target_bir_lowering=False)

    def din(name, shape, dt=F32):
        return nc.dram_tensor(name, list(shape), dt, kind="ExternalInput").ap()

    def dout(name, shape, dt=F32):
        return nc.dram_tensor(name, list(shape), dt, kind="ExternalOutput").ap()

    xo = din("xo", [NTOK, D]); xh = din("xh", [NTOK, D]); xs = din("xs", [NS, D])
    flag = din("flag", [128, 2])
    caches = {"A": din("ca", [NS, 128, 512]), "B1": din("cb1", [NS, 128, 1024]),
              "B2": din("cb2", [NS, 512, 1024]), "B3": din("cb3", [NS, 2048, 1024])}
    norm1_w = din("norm1_w", [D]); w_in = din("w_in", [D, NIN])
    qna = din("qna", [128]); kna = din("kna", [128]); sink = din("sink", [8])
    qnb = din("qnb", [128]); knb = din("knb", [128])
    wba = din("wba", [1024, D]); wbb = din("wbb", [512, D]); wout = din("wout", [D, D])
    norm2_w = din("norm2_w", [D]); wq = din("wq", [D, 1024]); subk = din("subk", [2, 128, 64])
    with_peer = stop_after is None and not debug
    if with_peer:
        pu = din("pu", [16384, D]); pv = din("pv", [16384, D])
    sbias_d = din("sbias", [128, 20]); dmask_d = din("dmask", [128, NS * NS]); iota16_d = din("iota16", [128, 16])
    subkT = din("subkT", [2, 64, 128])
    tabs_d = {}
    for u in UNITS:
        nvar, _, _ = unit_variants(u)
        tabs_d[u["name"]] = din("tab_" + u["name"], [128, u["nq"] * nvar * 128])

    yp = dout("yp", [NTOK, D]); ys = dout("ys", [NS, D])
    kvp = {"A": dout("kva_p", [128, 512]), "B1": dout("kvb1_p", [128, 1024]),
           "B2": dout("kvb2_p", [512, 1024]), "B3": dout("kvb3_p", [1024, 1024])}
    kvs = {"A": dout("kva_s", [NS, 128, 512]), "B1": dout("kvb1_s", [NS, 128, 1024]),
           "B2": dout("kvb2_s", [NS, 512, 1024]), "B3": dout("kvb3_s", [NS, 2048, 1024])}
    dbg = {}
    if debug:
        dbg["oaT"] = dout("dbg_oaT", [128, 8, 1040], BF16)
        dbg["obT"] = dout("dbg_obT", [128, 4, 1040], BF16)

    P = Prog(nc)
    with ExitStack() as es:
        def sb(name, shape, dt=F32):
            return es.enter_context(nc.sbuf_tensor(name, list(shape), dt))

        def ps(name, shape, dt=F32):
            return es.enter_context(nc.psum_tensor(name, list(shape), dt))

        ident = sb("ident", [128, 128])
        flag_t = sb("flag_t", [128, 2])
        gains = sb("gains", [128, 4, 128])
        esink = sb("esink", [128, 8])
        esA = ExitStack()

        def sbA(name, shape, dt=F32):
            return esA.enter_context(nc.sbuf_tensor(name, list(shape), dt))
        xnT = sbA("xnT", [128, 16, NXT], BF16)
        wring = [sbA(f"wring{i}", [128, 16, 256], BF16) for i in range(2)]
        oaT = sbA("oaT", [128, 8, 1040], BF16)
        obT = sbA("obT", [128, 4, 1040], BF16)
        pz = ps("pz", [128, 2, 512])
        ptr = ps("ptr", [128, 2, 512])
        pst = ps("pst", [128, 2, 512])
        po = ps("po", [128, 4, 256])

        P.op("pool", lambda e: e.memset(ident[:], 0.0), writes=["ident"])
        P.op("pool", lambda e: e.affine_select(out=ident[:], in_=ident[:], pattern=[[-1, 128]],
                                               compare_op=ALU.not_equal, fill=1.0, base=0, channel_multiplier=1),
             reads=["ident"], writes=["ident"])
        P.op("sp", lambda e: e.dma_start(out=flag_t[:], in_=flag), writes=["flag"], dma=True)
        for i, g in enumerate((qna, kna, qnb, knb)):
            P.op("sp", lambda e, i=i, g=g: e.dma_start(out=gains[:, i, :], in_=g.partition_broadcast(128)),
                 writes=[("gains", i)], dma=True)
        P.op("sp", lambda e: e.dma_start(out=esink[:], in_=sink.partition_broadcast(128)), writes=["esink"], dma=True)
        P.op("act", lambda e: e.activation(out=esink[:], in_=esink[:], func=AF.Exp), reads=["esink"], writes=["esink"])
        copy_jobs = [("A", 0, NS), ("B1", 0, NS)] + [("B2", t0, 4) for t0 in range(0, NS, 4)] + [("B3", t0, 1) for t0 in range(NS)]

        def issue_copies(k):
            for (gname, t0, nt_) in copy_jobs[k::8]:
                L = GROUPS[gname]["L"]
                P.op("act", lambda e, gname=gname, L=L, t0=t0, nt_=nt_: e.dma_start(
                    out=kvs[gname][t0:t0 + nt_, 0:L - 1, :], in_=caches[gname][t0:t0 + nt_, 1:L, :]),
                    writes=[("kvs_copy", gname, t0)], dma=True)

        with ExitStack() as es1:
            def sb1(name, shape, dt=F32):
                return es1.enter_context(nc.sbuf_tensor(name, list(shape), dt))
            w1rep = sb1("w1rep", [128, D])
            xbuf = [sb1(f"xbuf{i}", [128, D]) for i in range(2)]
            xnb = [sb1(f"xnb{i}", [128, D]) for i in range(2)]
            junk = sb1("junk1", [128, D], BF16)
            st1 = [sb1(f"st1_{i}", [128, 4]) for i in range(2)]
            P.op("sp", lambda e: e.dma_start(out=w1rep[:], in_=norm1_w.partition_broadcast(128)), writes=["w1rep"], dma=True)
            tiles = [(xh, i, XT_H + i * 128, 128) for i in range(8)] + [(xo, i, XT_O + i * 128, 128) for i in range(8)] + [(xs, 0, XT_S, NS)]
            for ti, (src, i, toff, n) in enumerate(tiles):
                s = ti % 2
                xb, xn_, st = xbuf[s], xnb[s], st1[s]
                P.op("sp", lambda e, xb=xb, src=src, i=i, n=n: e.dma_start(out=xb[:n, :], in_=src[i * 128:i * 128 + n, :]),
                     writes=[("xbuf", s)], dma=True)
                P.op("act", lambda e, xb=xb, st=st, n=n: e.activation(out=junk[:n, :], in_=xb[:n, :], func=AF.Square, accum_out=st[:n, 0:1]),
                     reads=[("xbuf", s)], writes=["junk1", ("st1", s)])
                P.op("act", lambda e, st=st, n=n: e.activation(out=st[:n, 1:2], in_=st[:n, 0:1], func=AF.Ln, scale=1.0 / D, bias=EPS),
                     reads=[("st1", s)], writes=[("st1", s)])
                P.op("act", lambda e, st=st, n=n: e.activation(out=st[:n, 2:3], in_=st[:n, 1:2], func=AF.Exp, scale=-0.5),
                     reads=[("st1", s)], writes=[("st1", s)])
                P.op("dve", lambda e, xb=xb, xn_=xn_, st=st, n=n: e.scalar_tensor_tensor(
                    out=xn_[:n, :], in0=xb[:n, :], scalar=st[:n, 2:3], in1=w1rep[:n, :], op0=ALU.mult, op1=ALU.mult),
                    reads=[("xbuf", s), ("st1", s), "w1rep"], writes=[("xnb", s)])
                for k4 in range(4):
                    bank = (ti * 4 + k4) % 2
                    for kk in range(4):
                        k = k4 * 4 + kk
                        P.op("pe", lambda e, xn_=xn_, k=k, kk=kk, bank=bank, n=n: e.transpose(
                            out=ptr[:, bank, kk * 128:kk * 128 + n], in_=xn_[:n, k * 128:(k + 1) * 128], identity=ident[:n, :n]),
                            reads=[("xnb", s), "ident"], writes=[("ptr", bank)])
                    eng = "act" if k4 % 2 == 0 else "dve"
                    if eng == "act":
                        P.op("act", lambda e, k4=k4, bank=bank, toff=toff, n=n: e.activation(
                            out=xnT[:, k4 * 4:(k4 + 1) * 4, toff:toff + n],
                            in_=ptr[:, bank, :].rearrange("p (a b) -> p a b", b=128)[:, :, :n], func=AF.Copy),
                            reads=[("ptr", bank)], writes=[("xnT", toff)])
                    else:
                        P.op("dve", lambda e, k4=k4, bank=bank, toff=toff, n=n: e.tensor_copy(
                            out=xnT[:, k4 * 4:(k4 + 1) * 4, toff:toff + n],
                            in_=ptr[:, bank, :].rearrange("p (a b) -> p a b", b=128)[:, :, :n]),
                            reads=[("ptr", bank)], writes=[("xnT", toff)])
            P.barrier()

        wcount = [0]
        ring = list(wring)

        def load_w(src, col0, ncols, nk=16):
            s = wcount[0] % len(ring)
            wcount[0] += 1
            wt = ring[s]
            P.op("pool", lambda e: e.dma_start(out=wt[:, :nk, :ncols],
                                               in_=src[0:nk * 128, col0:col0 + ncols].rearrange("(k p) n -> p k n", p=128)),
                 writes=[("wring", s)], dma=True)
            return wt, ("wring", s)

        if stop_after == 1:
            P.barrier(); P.emit(); esA.close(); return nc

        with ExitStack() as es2:
            def sb2(name, shape, dt=F32):
                return es2.enter_context(nc.sbuf_tensor(name, list(shape), dt))
            QT = sb2("QT", [128, 4, NTOK], BF16)
            accBv = sb2("accBv", [128, NT, 4, 128])
            accBd = sb2("accBd", [128, NT, 4])
            onesc = sb2("onesc", [128, 2], BF16)
            po2 = po[:].rearrange("p a b -> p (a b)")
            KT = sb2("KT", [128, 2, 16 * 128], BF16)
            VA = sb2("VA", [128, 16, 2, 128], BF16)
            tabt = sb2("tabt", [128, 2 * 5 * 128], BF16)
            sq = sb2("sq", [128, 256])
            zn = [sb2(f"zn{i}", [128, 256]) for i in range(2)]
            vf = [sb2(f"vf{i}", [128, 256]) for i in range(2)]
            st2 = [sb2(f"st2_{i}", [128, 8]) for i in range(2)]
            Eb = [sb2(f"Eb{i}", [128, 512]) for i in range(2)]
            Pm = [sb2(f"Pm{i}", [128, 4096], BF16) for i in range(2)]
            ot = [sb2(f"ot{i}", [128, 4, 128]) for i in range(2)]
            rc = [sb2(f"rc{i}", [128, 4]) for i in range(2)]
            cnt = dict(z=0, tr=0, zn=0, vf=0, st=0, E=0, pm=0, ot=0)

            def proj_tile(wt, wkey, ncols, toff, n):
                b = cnt["z"] % 2
                cnt["z"] += 1
                for k in range(16):
                    P.op("pe", lambda e, k=k, b=b: e.matmul(pz[:n, b, :ncols], lhsT=xnT[:, k, toff:toff + n], rhs=wt[:, k, :ncols],
                                                         start=(k == 0), stop=(k == 15)),
                         reads=[("xnT", toff), wkey], writes=[("pz", b)])
                return b

            def qk_norm(b, nheads, gain_idx, n):
                ncols = nheads * 128
                s = cnt["zn"] % 2
                cnt["zn"] += 1
                st = st2[s]
                P.op("act", lambda e: e.activation(out=sq[:n, :ncols], in_=pz[:n, b, :ncols], func=AF.Square),
                     reads=[("pz", b)], writes=["sq"])
                P.op("dve", lambda e: e.tensor_reduce(out=st[:n, 0:nheads], in_=sq[:n, :ncols].rearrange("p (h d) -> p h d", d=128),
                                                      axis=AX.X, op=ALU.add),
                     reads=["sq"], writes=[("st2", s)])
                P.op("act", lambda e: e.activation(out=st[:n, 2:2 + nheads], in_=st[:n, 0:nheads], func=AF.Ln, scale=1.0 / 128, bias=EPS),
                     reads=[("st2", s)], writes=[("st2", s)])
                P.op("act", lambda e: e.activation(out=st[:n, 4:4 + nheads], in_=st[:n, 2:2 + nheads], func=AF.Exp, scale=-0.5),
                     reads=[("st2", s)], writes=[("st2", s)])
                for h in range(nheads):
                    P.op("dve", lambda e, h=h: e.scalar_tensor_tensor(
                        out=zn[s][:n, h * 128:(h + 1) * 128], in0=pz[:n, b, h * 128:(h + 1) * 128], scalar=st[:n, 4 + h:5 + h],
                        in1=gains[:n, gain_idx, :], op0=ALU.mult, op1=ALU.mult),
                        reads=[("pz", b), ("st2", s), ("gains", gain_idx)], writes=[("zn", s)])
                return s

            def transpose_to(src_ap_fn, nheads, n, dst_ap, dkey, rkeys):
                bank = cnt["tr"] % 2
                cnt["tr"] += 1
                for h in range(nheads):
                    P.op("pe", lambda e, h=h: e.transpose(out=ptr[:, bank, h * 128:h * 128 + n], in_=src_ap_fn(h), identity=ident[:n, :n]),
                         reads=list(rkeys) + ["ident"], writes=[("ptr", bank)])
                P.op("act", lambda e: e.activation(out=dst_ap, in_=ptr[:, bank, :nheads * 128].rearrange("p (a b) -> p a b", b=128)[:, :, :n],
                                                   func=AF.Copy),
                     reads=[("ptr", bank)], writes=[dkey])

            for ui, u in enumerate(UNITS):
                if bis is not None and ui >= bis[0]:
                    break
                issue_copies(ui)
                G = GROUPS[u["grp"]]
                nh, nd = G["nh"], G["nd"]
                nq, nk = u["nq"], u["nk"]
                gq = nq // nk
                isA = u["grp"] == "A"
                nvar, vmap, _ = unit_variants(u)
                W = G["H"] * 128
                P.op("pool", lambda e, u=u, nvar=nvar, nq=nq: e.dma_start(out=tabt[:, :nq * nvar * 128], in_=tabs_d[u["name"]]),
                     writes=["tabt"], dma=True)
                for (c0, nc_, h0) in u["qjobs"]:
                    wt, wkey = load_w(w_in, c0, nc_)
                    for i in range(NT):
                        if bis is not None and len(bis) > 2 and bis[2] < 2:
                            break
                        b = proj_tile(wt, wkey, nc_, XT_O + i * 128, 128)
                        s = qk_norm(b, nc_ // 128, 0 if isA else 2, 128)
                        transpose_to(lambda h, s=s: zn[s][:, h * 128:(h + 1) * 128], nc_ // 128, 128,
                                     QT[:, h0:h0 + nc_ // 128, i * 128:(i + 1) * 128], ("QT", i), [("zn", s)])
                if bis is not None and len(bis) > 2 and bis[2] < 3:
                    continue
                nkc = nk * 128
                wt, wkey = load_w(w_in, u["kcol"], nkc)
                ktiles = [(XT_H + (8 - nh + j) * 128, j, None) for j in range(nh)] + [(XT_O + i * 128, nh + i, i) for i in range(NT)]
                out_tiles = {"A": [7], "B1": [7], "B2": [4, 5, 6, 7], "B3": list(range(8))}[u["grp"]]
                for (toff, kti, own_i) in ktiles:
                    b = proj_tile(wt, wkey, nkc, toff, 128)
                    s = qk_norm(b, nk, 1 if isA else 3, 128)
                    if own_i is not None and own_i in out_tiles:
                        r0 = (own_i - out_tiles[0]) * 128
                        cc = u["kvhead0"] * 128
                        P.op("sp", lambda e, s=s, r0=r0, cc=cc, nkc=nkc, grp=u["grp"]: e.dma_start(
                            out=kvp[grp][r0:r0 + 128, cc:cc + nkc], in_=zn[s][:, :nkc]),
                            reads=[("zn", s)], writes=[("kvp", u["name"], "k", own_i)], dma=True)
                    transpose_to(lambda h, s=s: zn[s][:, h * 128:(h + 1) * 128], nk, 128,
                                 KT[:, 0:nk, kti * 128:(kti + 1) * 128], ("KT", kti), [("zn", s)])
                if bis is not None and len(bis) > 2 and bis[2] == 3.5:
                    load_w(w_in, u["vcol"], nkc)
                    load_w(w_in, u["vcol"], nkc)
                    continue
                if bis is not None and len(bis) > 2 and bis[2] < 4:
                    continue
                vm = bis[3] if (bis is not None and len(bis) > 3) else 15
                if vm & 1:
                    P.op("dve", lambda e: e.tensor_copy(out=onesc[:, 0:2], in_=flag_t[:, 0:2]), reads=["flag"], writes=["onesc"])
                wt, wkey = load_w(w_in, u["vcol"], nkc)
                for (toff, kti, own_i) in ktiles:
                    b = proj_tile(wt, wkey, nkc, toff, 128)
                    is_out = own_i is not None and own_i in out_tiles
                    if not is_out:
                        P.op("dve", lambda e, b=b, kti=kti, nk=nk, nkc=nkc: e.tensor_copy(
                            out=VA[:, kti, 0:nk, :], in_=pz[:, b, :nkc].rearrange("p (h d) -> p h d", d=128)),
                            reads=[("pz", b)], writes=[("VA", kti)])
                    else:
                        s = cnt["vf"] % 2
                        cnt["vf"] += 1
                        r0 = (own_i - out_tiles[0]) * 128
                        cc = W + u["kvhead0"] * 128
                        P.op("dve", lambda e, b=b, s=s, nkc=nkc: e.tensor_copy(out=vf[s][:, :nkc], in_=pz[:, b, :nkc]),
                             reads=[("pz", b)], writes=[("vf", s)])
                        P.op("pool", lambda e, s=s, kti=kti, nk=nk, nkc=nkc: e.tensor_copy(
                            out=VA[:, kti, 0:nk, :], in_=vf[s][:, :nkc].rearrange("p (h d) -> p h d", d=128)),
                            reads=[("vf", s)], writes=[("VA", kti)])
                        P.op("sp", lambda e, s=s, r0=r0, cc=cc, nkc=nkc, grp=u["grp"]: e.dma_start(
                            out=kvp[grp][r0:r0 + 128, cc:cc + nkc], in_=vf[s][:, :nkc]),
                            reads=[("vf", s)], writes=[("kvp", u["name"], "v", own_i)], dma=True)
                for qt in range(NT):
                    if bis is not None and bis[1] == 0:
                        break
                    deltas = [dl for dl in range(nd) if qt - dl >= -nh]
                    ps_ = cnt["pm"] % 2
                    cnt["pm"] += 1
                    for di, dl in enumerate(deltas):
                        kti = qt - dl + nh
                        var, biases = vmap[dl]
                        sb_ = cnt["E"] % 2
                        cnt["E"] += 1
                        for k in range(nk):
                            P.op("pe", lambda e, k=k, kti=kti, sb_=sb_, qt=qt, gq=gq: e.matmul(
                                pst[:, sb_, k * gq * 128:(k + 1) * gq * 128], lhsT=KT[:, k, kti * 128:(kti + 1) * 128],
                                rhs=QT[:, k * gq:(k + 1) * gq, qt * 128:(qt + 1) * 128], start=True, stop=True),
                                reads=[("KT", kti), ("QT", qt)], writes=[("pst", sb_)])
                        if all(bv == 0.0 for bv in biases):
                            P.op("act", lambda e, sb_=sb_, nq=nq: e.activation(out=Eb[sb_][:, :nq * 128], in_=pst[:, sb_, :nq * 128],
                                                                             func=AF.Exp, scale=SCALE),
                                 reads=[("pst", sb_)], writes=[("Eb", sb_)])
                        else:
                            for j in range(nq):
                                P.op("act", lambda e, sb_=sb_, j=j, bv=biases[j]: e.activation(
                                    out=Eb[sb_][:, j * 128:(j + 1) * 128], in_=pst[:, sb_, j * 128:(j + 1) * 128],
                                    func=AF.Exp, scale=SCALE, bias=bv),
                                    reads=[("pst", sb_)], writes=[("Eb", sb_)])
                        P.op("dve", lambda e, sb_=sb_, ps_=ps_, di=di, var=var, nq=nq, nvar=nvar: e.tensor_tensor(
                            out=Pm[ps_][:, di * nq * 128:(di + 1) * nq * 128].rearrange("p (j q) -> p j q", q=128),
                            in0=Eb[sb_][:, :nq * 128].rearrange("p (j q) -> p j q", q=128),
                            in1=tabt[:, :nq * nvar * 128].rearrange("p (j v q) -> p j v q", v=nvar, q=128)[:, :, var, :],
                            op=ALU.mult),
                            reads=[("Eb", sb_), "tabt"], writes=[("Pm", ps_, di)])
                    for j in range(nq):
                        for di, dl in enumerate(deltas):
                            kti = qt - dl + nh
                            P.op("pe", lambda e, j=j, di=di, kti=kti, ps_=ps_, gq=gq, nq=nq, first=(di == 0), last=(di == len(deltas) - 1): e.matmul(
                                po2[:, j * 128:(j + 1) * 128], lhsT=Pm[ps_][:, (di * nq + j) * 128:(di * nq + j + 1) * 128], rhs=VA[:, kti, j // gq, :],
                                start=first, stop=last),
                                reads=[("Pm", ps_, di), ("VA", kti)], writes=["po_num"])
                    for j in range(nq):
                        for di, dl in enumerate(deltas):
                            kti = qt - dl + nh
                            oc = 1 if kti < nh else 0
                            P.op("pe", lambda e, j=j, di=di, oc=oc, ps_=ps_, nq=nq, first=(di == 0), last=(di == len(deltas) - 1): e.matmul(
                                po2[:, 512 + j:513 + j], lhsT=Pm[ps_][:, (di * nq + j) * 128:(di * nq + j + 1) * 128], rhs=onesc[:, oc:oc + 1],
                                start=first, stop=last),
                                reads=[("Pm", ps_, di), "onesc"], writes=["po_den"])
                    if isA:
                        so = cnt["ot"] % 2
                        cnt["ot"] += 1
                        h0 = u["ohead0"]
                        P.op("dve", lambda e, so=so, h0=h0: e.tensor_tensor(out=rc[so][:, 0:4], in0=po2[:, 512:516], in1=esink[:, h0:h0 + 4], op=ALU.add),
                             reads=["po_den", "esink"], writes=[("rc", so)])
                        P.op("dve", lambda e, so=so: e.reciprocal(out=rc[so][:, 0:4], in_=rc[so][:, 0:4]), reads=[("rc", so)], writes=[("rc", so)])
                        P.op("dve", lambda e, so=so: e.tensor_tensor(out=ot[so][:], in0=po2[:, 0:512].rearrange("p (h d) -> p h d", d=128),
                                                                    in1=rc[so][:, 0:4].unsqueeze(2).to_broadcast([128, 4, 128]), op=ALU.mult),
                             reads=["po_num", ("rc", so)], writes=[("ot", so)])
                        transpose_to(lambda h, so=so: ot[so][:, h, :], 4, 128, oaT[:, h0:h0 + 4, qt * 128:(qt + 1) * 128],
                                     ("oaT", h0, qt), [("ot", so)])
                    else:
                        h0 = u["ohead0"]
                        if u["grp"] == "B1":
                            P.op("dve", lambda e, h0=h0, qt=qt: e.tensor_copy(out=accBv[:, qt, h0:h0 + 2, :], in_=po2[:, 0:256].rearrange("p (h d) -> p h d", d=128)),
                                 reads=["po_num"], writes=[("accBv", qt, h0)])
                            P.op("dve", lambda e, h0=h0, qt=qt: e.tensor_copy(out=accBd[:, qt, h0:h0 + 2], in_=po2[:, 512:514]),
                                 reads=["po_den"], writes=[("accBd", qt, h0)])
                        else:
                            P.op("dve", lambda e, h0=h0, qt=qt: e.tensor_tensor(out=accBv[:, qt, h0:h0 + 2, :], in0=po2[:, 0:256].rearrange("p (h d) -> p h d", d=128),
                                                                               in1=accBv[:, qt, h0:h0 + 2, :], op=ALU.add),
                                 reads=["po_num", ("accBv", qt, h0)], writes=[("accBv", qt, h0)])
                            P.op("dve", lambda e, h0=h0, qt=qt: e.tensor_tensor(out=accBd[:, qt, h0:h0 + 2], in0=po2[:, 512:514],
                                                                               in1=accBd[:, qt, h0:h0 + 2], op=ALU.add),
                                 reads=["po_den", ("accBd", qt, h0)], writes=[("accBd", qt, h0)])
            for qt in range(NT):
                if bis is not None:
                    break
                so = cnt["ot"] % 2
                cnt["ot"] += 1
                P.op("dve", lambda e, so=so, qt=qt: e.reciprocal(out=rc[so][:, 0:4], in_=accBd[:, qt, :]),
                     reads=[("accBd", qt, 0), ("accBd", qt, 2)], writes=[("rc", so)])
                P.op("dve", lambda e, so=so, qt=qt: e.tensor_tensor(out=ot[so][:], in0=accBv[:, qt, :, :],
                                                                   in1=rc[so][:, 0:4].unsqueeze(2).to_broadcast([128, 4, 128]), op=ALU.mult),
                     reads=[("accBv", qt, 0), ("accBv", qt, 2), ("rc", so)], writes=[("ot", so)])
                transpose_to(lambda h, so=so: ot[so][:, h, :], 4, 128, obT[:, 0:4, qt * 128:(qt + 1) * 128], ("obT", qt), [("ot", so)])
            P.barrier()

        if stop_after == 2 or bis is not None:
            if debug and bis is None:
                P.op("sp", lambda e: e.dma_start(out=dbg["oaT"][:, :, 0:1024], in_=oaT[:, :, 0:1024]), reads=[], writes=["dbg1"], dma=True)
                P.op("sp", lambda e: e.dma_start(out=dbg["obT"][:, :, 0:1024], in_=obT[:, :, 0:1024]), reads=[], writes=["dbg2"], dma=True)
            P.barrier(); P.emit(); esA.close(); return nc

        with ExitStack() as es3:
            def sb3(name, shape, dt=F32):
                return es3.enter_context(nc.sbuf_tensor(name, list(shape), dt))
            qs = sb3("qs", [NS, 20 * 128]); ks = sb3("ks", [NS, 14 * 128]); vs = sb3("vs", [NS, 14 * 128])
            qsb = sb3("qsb", [NS, 20 * 128], BF16)
            sel = sb3("sel", [NS, NS, 128], BF16)
            kvc = sb3("kvc", [128, NS, 1024], BF16)
            qrep = sb3("qrep", [128, 1024], BF16)
            prod = sb3("prod", [128, 1024])
            Ssb = sb3("Ssb", [128, NS * 8]); S2 = sb3("S2", [128, NS * 8])
            Pb = sb3("Pb", [128, 8, NS], BF16)
            Pexp = sb3("Pexp", [128, 8, NS, NS], BF16)
            sbias_t = sb3("sbias_t", [128, 20]); dmask_t = sb3("dmask_t", [128, NS, NS])
            onesb = sb3("onesb", [128, 1], BF16)
            sq3 = sb3("sq3", [NS, 256]); st3 = [sb3(f"st3_{i}", [NS, 8]) for i in range(2)]
            lg = sb3("lg", [NS, 20]); enew = sb3("enew", [NS, 20])
            tmpv = sb3("tmpv", [NS, 8, 128])
            numA = sb3("numA", [NS, 8, 128]); denA = sb3("denA", [NS, 8])
            numB = sb3("numB", [NS, 4, 128]); denB = sb3("denB", [NS, 4])
            pst_f = pst[:].rearrange("p a b -> p (a b)")
            po_f = po[:].rearrange("p a b -> p (a b)")
            P.op("sp", lambda e: e.dma_start(out=sbias_t[:], in_=sbias_d), writes=["sbias"], dma=True)
            P.op("sp", lambda e: e.dma_start(out=dmask_t[:].rearrange("p a b -> p (a b)"), in_=dmask_d), writes=["dmask"], dma=True)
            P.op("pool", lambda e: e.memset(onesb[:], 1.0), writes=["onesb"])
            P.op("dve", lambda e: e.tensor_copy(out=sel[:], in_=ident[:NS, :NS].unsqueeze(2).to_broadcast([NS, NS, 128])),
                 reads=["ident"], writes=["sel"])
            for c in range(24):
                wt, wkey = load_w(w_in, c * 256, 256)
                b = c % 2
                for k in range(16):
                    P.op("pe", lambda e, k=k, b=b, wt=wt: e.matmul(pz[:NS, b, :256], lhsT=xnT[:, k, XT_S:XT_S + NS], rhs=wt[:, k, :256],
                                                                  start=(k == 0), stop=(k == 15)),
                         reads=[("xnT", XT_S), wkey], writes=[("pz", b)])
                if c < 4:
                    kind, dst, gi = "n", qs[:, c * 256:(c + 1) * 256], 0
                elif c == 4:
                    kind, dst, gi = "n", ks[:, 0:256], 1
                elif c == 5:
                    kind, dst, gi = "v", vs[:, 0:256], None
                elif c < 12:
                    kind, dst, gi = "n", qs[:, 1024 + (c - 6) * 256:1024 + (c - 5) * 256], 2
                elif c < 18:
                    kind, dst, gi = "n", ks[:, 256 + (c - 12) * 256:256 + (c - 11) * 256], 3
                else:
                    kind, dst, gi = "v", vs[:, 256 + (c - 18) * 256:256 + (c - 17) * 256], None
                dkey = ("sdst", c)
                if kind == "v":
                    P.op("act", lambda e, b=b, dst=dst: e.activation(out=dst, in_=pz[:NS, b, :256], func=AF.Copy),
                         reads=[("pz", b)], writes=[dkey, "svs"])
                else:
                    st = st3[c % 2]
                    skey = ("st3", c % 2)
                    P.op("act", lambda e, b=b: e.activation(out=sq3[:, :], in_=pz[:NS, b, :256], func=AF.Square), reads=[("pz", b)], writes=["sq3"])
                    P.op("dve", lambda e, st=st: e.tensor_reduce(out=st[:, 0:2], in_=sq3[:, :].rearrange("p (h d) -> p h d", d=128), axis=AX.X, op=ALU.add),
                         reads=["sq3"], writes=[skey])
                    P.op("act", lambda e, st=st: e.activation(out=st[:, 2:4], in_=st[:, 0:2], func=AF.Ln, scale=1.0 / 128, bias=EPS), reads=[skey], writes=[skey])
                    P.op("act", lambda e, st=st: e.activation(out=st[:, 4:6], in_=st[:, 2:4], func=AF.Exp, scale=-0.5), reads=[skey], writes=[skey])
                    for h in range(2):
                        P.op("dve", lambda e, h=h, b=b, st=st, dst=dst, gi=gi: e.scalar_tensor_tensor(
                            out=dst[:, h * 128:(h + 1) * 128], in0=pz[:NS, b, h * 128:(h + 1) * 128], scalar=st[:, 4 + h:5 + h],
                            in1=gains[:NS, gi, :], op0=ALU.mult, op1=ALU.mult),
                            reads=[("pz", b), skey, ("gains", gi)], writes=[dkey, "sqk"])
            P.op("dve", lambda e: e.tensor_copy(out=qsb[:], in_=qs[:]), reads=["sqk"], writes=["qsb"])
            P.op("dve", lambda e: e.tensor_tensor(out=prod[:NS, 0:1024].rearrange("p (k g d) -> p k g d", k=2, g=4),
                                                  in0=qs[:, 0:1024].rearrange("p (k g d) -> p k g d", k=2, g=4),
                                                  in1=ks[:, 0:256].rearrange("p (k d) -> p k d", k=2).unsqueeze(2).to_broadcast([NS, 2, 4, 128]),
                                                  op=ALU.mult), reads=["sqk"], writes=["prod"])
            P.op("dve", lambda e: e.tensor_reduce(out=lg[:, 0:8], in_=prod[:NS, 0:1024].rearrange("p (h d) -> p h d", d=128), axis=AX.X, op=ALU.add),
                 reads=["prod"], writes=["lg"])
            for (c0, c1) in ((1024, 2048), (2048, 2560)):
                n_ = c1 - c0
                P.op("dve", lambda e, c0=c0, c1=c1, n_=n_: e.tensor_tensor(out=prod[:NS, 0:n_], in0=qs[:, c0:c1], in1=ks[:, c0 - 768:c1 - 768], op=ALU.mult),
                     reads=["sqk"], writes=["prod"])
                P.op("dve", lambda e, c0=c0, c1=c1, n_=n_: e.tensor_reduce(out=lg[:, c0 // 128:c1 // 128], in_=prod[:NS, 0:n_].rearrange("p (h d) -> p h d", d=128),
                                                                      axis=AX.X, op=ALU.add),
                     reads=["prod"], writes=["lg"])
            P.op("act", lambda e: e.activation(out=enew[:], in_=lg[:], func=AF.Exp, scale=SCALE), reads=["lg"], writes=["enew"])
            for gi_, gname in enumerate(("A", "B1", "B2", "B3")):
                G = GROUPS[gname]
                H, L, dil = G["H"], G["L"], G["dil"]
                W = 2 * H * 128
                isA = gname == "A"
                hq = 8 if isA else 4
                hb = 0 if isA else 8 + 4 * (gi_ - 1)
                kb = 0 if isA else 2 + 4 * (gi_ - 1)
                qoff = hb * 128
                csrc = bass.AP(caches[gname].tensor, 0, [[dil * W, 128], [L * W, NS], [1, W]])
                P.op("pool", lambda e, csrc=csrc, W=W: e.dma_start(out=kvc[:, :, :W], in_=csrc), writes=["kvc"], dma=True)
                P.op("sp", lambda e, gname=gname, L=L, H=H, kb=kb: e.dma_start(out=kvs[gname][:, L - 1, 0:H * 128], in_=ks[:, kb * 128:(kb + H) * 128]),
                     reads=["sqk"], writes=[("kvs_newk", gname)], dma=True)
                P.op("sp", lambda e, gname=gname, L=L, H=H, kb=kb: e.dma_start(out=kvs[gname][:, L - 1, H * 128:2 * H * 128], in_=vs[:, kb * 128:(kb + H) * 128]),
                     reads=["svs"], writes=[("kvs_newv", gname)], dma=True)
                tc_ = 1 if isA else 2
                for t0 in range(0, NS, tc_):
                    for j in range(tc_):
                        for half in range(hq * 128 // 512):
                            P.op("pe", lambda e, t0=t0, j=j, half=half, hq=hq, qoff=qoff: e.matmul(
                                pst_f[:, j * hq * 128 + half * 512:j * hq * 128 + (half + 1) * 512], lhsT=sel[:, t0 + j, :],
                                rhs=qsb[:, qoff + half * 512:qoff + (half + 1) * 512], start=True, stop=True),
                                reads=["sel", "qsb"], writes=["pstf"])
                    P.op("act", lambda e: e.activation(out=qrep[:], in_=pst_f, func=AF.Copy), reads=["pstf"], writes=["qrep"])
                    if isA:
                        P.op("dve", lambda e, t0=t0: e.tensor_tensor(
                            out=prod[:].rearrange("p (k g d) -> p k g d", k=2, g=4),
                            in0=kvc[:, t0, 0:256].rearrange("p (k d) -> p k d", k=2).unsqueeze(2).to_broadcast([128, 2, 4, 128]),
                            in1=qrep[:].rearrange("p (k g d) -> p k g d", k=2, g=4), op=ALU.mult),
                            reads=["kvc", "qrep"], writes=["prod"])
                    else:
                        P.op("dve", lambda e, t0=t0: e.tensor_tensor(
                            out=prod[:].rearrange("p (t c) -> p t c", t=2), in0=kvc[:, t0:t0 + 2, 0:512],
                            in1=qrep[:].rearrange("p (t c) -> p t c", t=2), op=ALU.mult),
                            reads=["kvc", "qrep"], writes=["prod"])
                    P.op("dve", lambda e, t0=t0, hq=hq, tc_=tc_: e.tensor_reduce(
                        out=Ssb[:, t0 * hq:(t0 + tc_) * hq], in_=prod[:].rearrange("p (h d) -> p h d", d=128), axis=AX.X, op=ALU.add),
                        reads=["prod"], writes=["Ssb"])
                P.op("dve", lambda e, hq=hq, hb=hb: e.scalar_tensor_tensor(
                    out=S2[:, :NS * hq].rearrange("p (t h) -> p t h", h=hq), in0=Ssb[:, :NS * hq].rearrange("p (t h) -> p t h", h=hq),
                    scalar=SCALE, in1=sbias_t[:, hb:hb + hq].unsqueeze(1).to_broadcast([128, NS, hq]), op0=ALU.mult, op1=ALU.add),
                    reads=["Ssb", "sbias"], writes=["S2"])
                P.op("act", lambda e, hq=hq: e.activation(out=Pb[:, 0:hq, :], in_=S2[:, :NS * hq].rearrange("p (t h) -> p h t", h=hq), func=AF.Exp),
                     reads=["S2"], writes=["Pb"])
                P.op("dve", lambda e, hq=hq: e.tensor_tensor(
                    out=Pexp[:, 0:hq, :, :], in0=Pb[:, 0:hq, :].unsqueeze(2).to_broadcast([128, hq, NS, NS]),
                    in1=dmask_t[:].unsqueeze(1).to_broadcast([128, hq, NS, NS]), op=ALU.mult),
                    reads=["Pb", "dmask"], writes=["Pexp"])
                for h in range(hq):
                    kvh = h // 4 if isA else h
                    for tp in range(NS):
                        P.op("pe", lambda e, h=h, tp=tp, kvh=kvh, H=H: e.matmul(
                            po_f[:NS, h * 128:(h + 1) * 128], lhsT=Pexp[:, h, tp, :], rhs=kvc[:, tp, (H + kvh) * 128:(H + kvh + 1) * 128],
                            start=(tp == 0), stop=(tp == NS - 1)),
                            reads=["Pexp", "kvc"], writes=["pof"])
                    P.op("pe", lambda e, h=h: e.matmul(pz[:NS, 1, h:h + 1], lhsT=Pb[:, h, :], rhs=onesb[:, 0:1], start=True, stop=True),
                         reads=["Pb", "onesb"], writes=[("pz", 1)])
                if isA:
                    P.op("dve", lambda e: e.tensor_tensor(
                        out=tmpv[:].rearrange("p (k g) d -> p k g d", k=2),
                        in0=vs[:, 0:256].rearrange("p (k d) -> p k d", k=2).unsqueeze(2).to_broadcast([NS, 2, 4, 128]),
                        in1=enew[:, 0:8].rearrange("p (k g) -> p k g", k=2).unsqueeze(3).to_broadcast([NS, 2, 4, 128]), op=ALU.mult),
                        reads=["svs", "enew"], writes=["tmpv"])
                    P.op("dve", lambda e: e.tensor_tensor(out=numA[:], in0=po_f[:NS, 0:1024].rearrange("p (h d) -> p h d", d=128), in1=tmpv[:], op=ALU.add),
                         reads=["pof", "tmpv"], writes=["numA"])
                    P.op("dve", lambda e: e.tensor_tensor(out=denA[:], in0=pz[:NS, 1, 0:8], in1=enew[:, 0:8], op=ALU.add),
                         reads=[("pz", 1), "enew"], writes=["denA"])
                    P.op("dve", lambda e: e.tensor_tensor(out=denA[:], in0=denA[:], in1=esink[:NS, 0:8], op=ALU.add),
                         reads=["denA", "esink"], writes=["denA"])
                else:
                    P.op("dve", lambda e, kb=kb, hb=hb: e.tensor_tensor(
                        out=tmpv[:, 0:4, :], in0=vs[:, kb * 128:(kb + 4) * 128].rearrange("p (h d) -> p h d", d=128),
                        in1=enew[:, hb:hb + 4].unsqueeze(2).to_broadcast([NS, 4, 128]), op=ALU.mult),
                        reads=["svs", "enew"], writes=["tmpv"])
                    if gname == "B1":
                        P.op("dve", lambda e: e.tensor_tensor(out=numB[:], in0=po_f[:NS, 0:512].rearrange("p (h d) -> p h d", d=128), in1=tmpv[:, 0:4, :], op=ALU.add),
                             reads=["pof", "tmpv"], writes=["numB"])
                        P.op("dve", lambda e, hb=hb: e.tensor_tensor(out=denB[:], in0=pz[:NS, 1, 0:4], in1=enew[:, hb:hb + 4], op=ALU.add),
                             reads=[("pz", 1), "enew"], writes=["denB"])
                    else:
                        P.op("dve", lambda e: e.tensor_tensor(out=numB[:], in0=numB[:], in1=tmpv[:, 0:4, :], op=ALU.add), reads=["numB", "tmpv"], writes=["numB"])
                        P.op("dve", lambda e: e.tensor_tensor(out=numB[:], in0=po_f[:NS, 0:512].rearrange("p (h d) -> p h d", d=128), in1=numB[:], op=ALU.add),
                             reads=["pof", "numB"], writes=["numB"])
                        P.op("dve", lambda e, hb=hb: e.tensor_tensor(out=denB[:], in0=denB[:], in1=enew[:, hb:hb + 4], op=ALU.add), reads=["denB", "enew"], writes=["denB"])
                        P.op("dve", lambda e: e.tensor_tensor(out=denB[:], in0=pz[:NS, 1, 0:4], in1=denB[:], op=ALU.add), reads=[("pz", 1), "denB"], writes=["denB"])
            for (num, den, nh_, dstT, nm) in ((numA, denA, 8, oaT, "oaTs"), (numB, denB, 4, obT, "obTs")):
                P.op("dve", lambda e, den=den: e.reciprocal(out=den[:], in_=den[:]), reads=["denA", "denB"], writes=["denA", "denB"])
                P.op("dve", lambda e, num=num, den=den, nh_=nh_: e.tensor_tensor(out=num[:], in0=num[:], in1=den[:].unsqueeze(2).to_broadcast([NS, nh_, 128]), op=ALU.mult),
                     reads=["numA", "numB", "denA", "denB"], writes=["numA", "numB"])
                for h0 in range(0, nh_, 4):
                    for h in range(4):
                        P.op("pe", lambda e, num=num, h0=h0, h=h: e.transpose(out=ptr[:, 0, h * 128:h * 128 + NS], in_=num[:, h0 + h, :], identity=ident[:NS, :NS]),
                             reads=["numA", "numB", "ident"], writes=[("ptr", 0)])
                    P.op("act", lambda e, dstT=dstT, h0=h0: e.activation(
                        out=dstT[:, h0:h0 + 4, 1024:1024 + NS], in_=ptr[:, 0, :].rearrange("p (a b) -> p a b", b=128)[:, :, :NS], func=AF.Copy),
                        reads=[("ptr", 0)], writes=[(nm, h0)])
            P.barrier()

        if debug:
            P.op("sp", lambda e: e.dma_start(out=dbg["oaT"], in_=oaT[:]), reads=[], writes=["dbg1"], dma=True)
            P.op("sp", lambda e: e.dma_start(out=dbg["obT"], in_=obT[:]), reads=[], writes=["dbg2"], dma=True)
        if stop_after == 3:
            P.barrier(); P.emit(); esA.close(); return nc
        esR = ExitStack()
        mT = esR.enter_context(nc.sbuf_tensor("mT", [128, 16, 1040], BF16, side="right"))
        pst_f = pst[:].rearrange("p a b -> p (a b)")
        po_f = po[:].rearrange("p a b -> p (a b)")
        with ExitStack() as es4:
            def sb4(name, shape, dt=F32):
                return es4.enter_context(nc.sbuf_tensor(name, list(shape), dt))
            wr4 = [sb4(f"wr4_{i}", [128, 16, 256], BF16) for i in range(2)]
            ring[:] = list(wring) + wr4
            sga = [sb4(f"sga{i}", [128, 512]) for i in range(2)]
            sgb = [sb4(f"sgb{i}", [128, 512]) for i in range(2)]
            t1 = [sb4(f"t1_{i}", [128, 512]) for i in range(2)]
            t2 = [sb4(f"t2_{i}", [128, 512]) for i in range(2)]
            psets = [(pz[:, 0, :], pz[:, 1, :], pst[:, 0, :], pst[:, 1, :], ("pz", 0), ("pz", 1), ("pst", 0), ("pst", 1)),
                     (ptr[:, 0, :], ptr[:, 1, :], po_f[:, 0:512], po_f[:, 512:1024], ("ptr", 0), ("ptr", 1), ("pof", 0), ("pof", 1))]
            blocks = [(XT_O, 0, 512), (XT_O + 512, 512, 512), (XT_S, 1024, NS)]
            it = 0
            for c in range(8):
                wga, kga = load_w(w_in, 6144 + c * 256, 256)
                wgb, kgb = load_w(w_in, 8192 + c * 256, 256)
                wa, ka_ = load_w(wba, c * 256, 256, nk=8)
                wb_, kb_ = load_w(wbb, c * 256, 256, nk=4)
                for j in range(2):
                    dm = c * 2 + j
                    for (toff, ooff, n) in blocks:
                        s_ = it % 2
                        it += 1
                        GA, GB, YA, YB, kGA, kGB, kYA, kYB = psets[s_]
                        for (dst, dkey, wt_, wk_, nk_, src, skey_) in ((GA, kGA, wga, kga, 16, lambda k, toff=toff, n=n: xnT[:, k, toff:toff + n], "xn"),
                                                                       (GB, kGB, wgb, kgb, 16, lambda k, toff=toff, n=n: xnT[:, k, toff:toff + n], "xn"),
                                                                       (YA, kYA, wa, ka_, 8, lambda k, ooff=ooff, n=n: oaT[:, k, ooff:ooff + n], "oa"),
                                                                       (YB, kYB, wb_, kb_, 4, lambda k, ooff=ooff, n=n: obT[:, k, ooff:ooff + n], "ob")):
                            for k in range(nk_):
                                P.op("pe", lambda e, dst=dst, wt_=wt_, k=k, j=j, src=src, nk_=nk_, n=n: e.matmul(
                                    dst[:, :n], lhsT=wt_[:, k, j * 128:(j + 1) * 128], rhs=src(k), start=(k == 0), stop=(k == nk_ - 1)),
                                    reads=[wk_], writes=[dkey])
                        P.op("act", lambda e, GA=GA, s_=s_, n=n: e.activation(out=sga[s_][:, :n], in_=GA[:, :n], func=AF.Sigmoid), reads=[kGA], writes=[("sga", s_)])
                        P.op("act", lambda e, GB=GB, s_=s_, n=n: e.activation(out=sgb[s_][:, :n], in_=GB[:, :n], func=AF.Sigmoid), reads=[kGB], writes=[("sgb", s_)])
                        P.op("dve", lambda e, YA=YA, s_=s_, n=n: e.tensor_tensor(out=t1[s_][:, :n], in0=YA[:, :n], in1=sga[s_][:, :n], op=ALU.mult),
                             reads=[kYA, ("sga", s_)], writes=[("t1", s_)])
                        P.op("dve", lambda e, YB=YB, s_=s_, n=n: e.tensor_tensor(out=t2[s_][:, :n], in0=YB[:, :n], in1=sgb[s_][:, :n], op=ALU.mult),
                             reads=[kYB, ("sgb", s_)], writes=[("t2", s_)])
                        P.op("dve", lambda e, s_=s_, n=n, dm=dm, ooff=ooff: e.tensor_tensor(out=mT[:, dm, ooff:ooff + n], in0=t1[s_][:, :n], in1=t2[s_][:, :n], op=ALU.add),
                             reads=[("t1", s_), ("t2", s_)], writes=[("mT", dm, ooff)])
            P.barrier()
        esA.close()

        es5 = ExitStack()
        def sb5(name, shape, dt=F32):
            return es5.enter_context(nc.sbuf_tensor(name, list(shape), dt))
        h_t = sb5("h_t", [128, 9, D])
        with ExitStack() as es5b:
            def sb5b(name, shape, dt=F32):
                return es5b.enter_context(nc.sbuf_tensor(name, list(shape), dt))
            ring[:] = [sb5b(f"wr5_{i}", [128, 16, 256], BF16) for i in range(2)]
            xblk = [sb5b(f"xblk{i}", [128, 256]) for i in range(3)]
            it = 0
            for c in range(8):
                wt, wkey = load_w(wout, c * 256, 256)
                for i in range(9):
                    n = 128 if i < 8 else NS
                    ooff = i * 128
                    b = it % 2
                    xs_ = it % 3
                    it += 1
                    srcx = xo[i * 128:(i + 1) * 128, c * 256:(c + 1) * 256] if i < 8 else xs[:, c * 256:(c + 1) * 256]
                    P.op("sp", lambda e, xs_=xs_, srcx=srcx, n=n: e.dma_start(out=xblk[xs_][:n, :], in_=srcx), writes=[("xblk", xs_)], dma=True)
                    for k in range(16):
                        P.op("pe", lambda e, k=k, b=b, n=n, ooff=ooff, wt=wt: e.matmul(pz[:n, b, :256], lhsT=mT[:, k, ooff:ooff + n], rhs=wt[:, k, :256],
                                                                                  start=(k == 0), stop=(k == 15)),
                             reads=[wkey], writes=[("pz", b)])
                    P.op("dve", lambda e, b=b, n=n, i=i, c=c, xs_=xs_: e.tensor_tensor(out=h_t[:n, i, c * 256:(c + 1) * 256], in0=pz[:n, b, :256],
                                                                                    in1=xblk[xs_][:n, :], op=ALU.add),
                         reads=[("pz", b), ("xblk", xs_)], writes=[("h", i)])
            P.barrier()
        esR.close()
        if debug:
            P.op("sp", lambda e: e.dma_start(out=yp, in_=h_t[:, 0:8, :].rearrange("p i d -> p i d")) if False else e.dma_start(out=yp.rearrange("(i p) d -> p i d", p=128), in_=h_t[:, 0:8, :]),
                 reads=[], writes=["dbgh"], dma=True)
            P.op("sp", lambda e: e.dma_start(out=ys, in_=h_t[:NS, 8, :]), reads=[], writes=["dbghs"], dma=True)
            P.barrier(); P.emit(); es5.close(); return nc

        with ExitStack() as es6:
            def sb6(name, shape, dt=F32):
                return es6.enter_context(nc.sbuf_tensor(name, list(shape), dt))
            wqb = sb6("wqb", [128, 16, 1024], BF16)
            w2rep = sb6("w2rep", [128, D])
            skb = sb6("skb", [128, 256])
            iota16 = sb6("iota16t", [128, 16]); thr16 = sb6("thr16", [128, 16])
            hn = sb6("hn", [128, D])
            hnT = sb6("hnT", [128, 16, 128], BF16)
            qf = sb6("qf", [128, 1024])
            qT = sb6("qT", [128, 8, 128])
            big = sb6("big", [128, 2048])
            sS = big[:]
            s2 = sb6("s2", [128, 256])
            vals = sb6("vals", [128, 16, 16]); idxu = sb6("idxu", [128, 16, 16], U32); idxf = sb6("idxf", [128, 16, 16])
            cand = big[:].rearrange("p (h c) -> p h c", c=256)
            best = sb6("best", [128, 8, 16]); bcu = sb6("bcu", [128, 8, 16], U32); bcf = sb6("bcf", [128, 128])
            akf = sb6("akf", [128, 128]); bkf = sb6("bkf", [128, 128])
            eq = big[:].rearrange("p (s a) -> p s a", a=16)
            i1f = sb6("i1f", [128, 128]); i2f = sb6("i2f", [128, 128]); ef = sb6("ef", [128, 128]); eidx = sb6("eidx", [128, 128], U32)
            gate = sb6("gate", [128, 8, 16]); gsum = sb6("gsum", [128, 8])
            act_ = sb6("act_", [128, 128]); wgt = sb6("wgt", [128, 128])
            NUB = 10
            ubuf = [sb6(f"ubuf{i}", [128, D], BF16) for i in range(NUB)]
            junk6 = sb6("junk6", [128, D], BF16)
            st6 = sb6("st6", [128, 8])
            for c4 in range(4):
                P.op("pool", lambda e, c4=c4: e.dma_start(out=wqb[:, :, c4 * 256:(c4 + 1) * 256],
                                                         in_=wq[:, c4 * 256:(c4 + 1) * 256].rearrange("(k p) n -> p k n", p=128)),
                     writes=[("wqb", c4)], dma=True)
            P.op("sp", lambda e: e.dma_start(out=w2rep[:], in_=norm2_w.partition_broadcast(128)), writes=["w2rep"], dma=True)
            P.op("sp", lambda e: e.dma_start(out=iota16[:], in_=iota16_d), writes=["iota16"], dma=True)
            P.op("dve", lambda e: e.tensor_scalar(out=thr16[:], in0=iota16[:], scalar1=16.0, scalar2=None, op0=ALU.mult), reads=["iota16"], writes=["thr16"])
            P.op("dve", lambda e: e.memset(skb[:], 0.0), writes=["skb"])
            P.op("sp", lambda e: e.dma_start(out=skb[0:64, 0:128], in_=subkT[0]), reads=["skb"], writes=["skb"], dma=True)
            P.op("sp", lambda e: e.dma_start(out=skb[64:128, 128:256], in_=subkT[1]), reads=["skb"], writes=["skb"], dma=True)
            ucnt = [0]
            for i in range(9):
                n = 128 if i < 8 else NS
                hi = h_t[:n, i, :]
                hk = ("h", i)
                P.op("act", lambda e, hi=hi, n=n: e.activation(out=junk6[:n, :], in_=hi, func=AF.Square, accum_out=st6[:n, 0:1]), reads=[hk], writes=["junk6", "st6"])
                P.op("act", lambda e, n=n: e.activation(out=st6[:n, 1:2], in_=st6[:n, 0:1], func=AF.Ln, scale=1.0 / D, bias=EPS), reads=["st6"], writes=["st6"])
                P.op("act", lambda e, n=n: e.activation(out=st6[:n, 2:3], in_=st6[:n, 1:2], func=AF.Exp, scale=-0.5), reads=["st6"], writes=["st6"])
                P.op("dve", lambda e, hi=hi, n=n: e.scalar_tensor_tensor(out=hn[:n, :], in0=hi, scalar=st6[:n, 2:3], in1=w2rep[:n, :], op0=ALU.mult, op1=ALU.mult),
                     reads=[hk, "st6", "w2rep"], writes=["hn"])
                for k4 in range(4):
                    bank = k4 % 2
                    for kk in range(4):
                        k = k4 * 4 + kk
                        P.op("pe", lambda e, k=k, kk=kk, bank=bank, n=n: e.transpose(out=ptr[:, bank, kk * 128:kk * 128 + n], in_=hn[:n, k * 128:(k + 1) * 128],
                                                                                     identity=ident[:n, :n]), reads=["hn", "ident"], writes=[("ptr", bank)])
                    P.op("act", lambda e, k4=k4, bank=bank, n=n: e.activation(out=hnT[:, k4 * 4:(k4 + 1) * 4, :n],
                                                                            in_=ptr[:, bank, :].rearrange("p (a b) -> p a b", b=128)[:, :, :n], func=AF.Copy),
                         reads=[("ptr", bank)], writes=["hnT"])
                for half in range(2):
                    for k in range(16):
                        P.op("pe", lambda e, k=k, half=half, n=n: e.matmul(pz[:n, half, :], lhsT=hnT[:, k, :n], rhs=wqb[:, k, half * 512:(half + 1) * 512],
                                                                        start=(k == 0), stop=(k == 15)),
                             reads=["hnT"] + [("wqb", c4) for c4 in range(4)], writes=[("pz", half)])
                P.op("act", lambda e, n=n: e.activation(out=qf[:n, :].rearrange("p (a b) -> p a b", a=2), in_=pz[:n, :, :], func=AF.Copy),
                     reads=[("pz", 0), ("pz", 1)], writes=["qf"])
                for h4 in range(2):
                    for hh in range(4):
                        h = h4 * 4 + hh
                        P.op("pe", lambda e, h=h, hh=hh, h4=h4, n=n: e.transpose(out=ptr[:, h4, hh * 128:hh * 128 + n], in_=qf[:n, h * 128:(h + 1) * 128],
                                                                                identity=ident[:n, :n]), reads=["qf", "ident"], writes=[("ptr", h4)])
                    P.op("act", lambda e, h4=h4, n=n: e.activation(out=qT[:, h4 * 4:(h4 + 1) * 4, :n],
                                                                 in_=ptr[:, h4, :].rearrange("p (a b) -> p a b", b=128)[:, :, :n], func=AF.Copy),
                         reads=[("ptr", h4)], writes=["qT"])
                for h in range(8):
                    dst = pst_f[:n, h * 256:(h + 1) * 256] if h < 4 else po_f[:n, (h - 4) * 256:(h - 3) * 256]
                    P.op("pe", lambda e, h=h, dst=dst, n=n: e.matmul(dst, lhsT=qT[:, h, :n], rhs=skb[:, :], start=True, stop=True),
                         reads=["qT", "skb"], writes=["pstf" if h < 4 else "pof"])
                P.op("act", lambda e, n=n: e.activation(out=sS[:n, 0:1024], in_=pst_f[:n, :], func=AF.Copy), reads=["pstf"], writes=["big"])
                P.op("dve", lambda e, n=n: e.tensor_copy(out=sS[:n, 1024:2048], in_=po_f[:n, :]), reads=["pof"], writes=["big"])
                for hc in range(16):
                    src = sS[:n, hc * 128:(hc + 1) * 128]
                    P.op("dve", lambda e, src=src, hc=hc, n=n: e.max(out=vals[:n, hc, 0:8], in_=src), reads=["big"], writes=["vals"])
                    P.op("dve", lambda e, src=src, hc=hc, n=n: e.max_index(out=idxu[:n, hc, 0:8], in_max=vals[:n, hc, 0:8], in_values=src),
                         reads=["big", "vals"], writes=["idxu"])
                    P.op("dve", lambda e, src=src, hc=hc, n=n: e.match_replace(out=s2[:n, 0:128], in_to_replace=vals[:n, hc, 0:8], in_values=src, imm_value=-1e30),
                         reads=["big", "vals"], writes=["s2"])
                    P.op("dve", lambda e, hc=hc, n=n: e.max(out=vals[:n, hc, 8:16], in_=s2[:n, 0:128]), reads=["s2"], writes=["vals"])
                    P.op("dve", lambda e, hc=hc, n=n: e.max_index(out=idxu[:n, hc, 8:16], in_max=vals[:n, hc, 8:16], in_values=s2[:n, 0:128]),
                         reads=["s2", "vals"], writes=["idxu"])
                P.op("dve", lambda e, n=n: e.tensor_copy(out=idxf[:n], in_=idxu[:n]), reads=["idxu"], writes=["idxf"])
                v4 = vals[:n].rearrange("p (h c) k -> p h c k", c=2)
                P.op("dve", lambda e, v4=v4, n=n: e.tensor_tensor(out=cand[:n].rearrange("p h (a b) -> p h a b", b=16),
                                                                 in0=v4[:, :, 0, :].unsqueeze(3).to_broadcast([n, 8, 16, 16]),
                                                                 in1=v4[:, :, 1, :].unsqueeze(2).to_broadcast([n, 8, 16, 16]), op=ALU.add),
                     reads=["vals"], writes=["big"])
                for h in range(8):
                    src = cand[:n, h, :]
                    P.op("dve", lambda e, src=src, h=h, n=n: e.max(out=best[:n, h, 0:8], in_=src), reads=["big"], writes=["best"])
                    P.op("dve", lambda e, src=src, h=h, n=n: e.max_index(out=bcu[:n, h, 0:8], in_max=best[:n, h, 0:8], in_values=src),
                         reads=["big", "best"], writes=["bcu"])
                    P.op("dve", lambda e, src=src, h=h, n=n: e.match_replace(out=s2[:n, :], in_to_replace=best[:n, h, 0:8], in_values=src, imm_value=-1e30),
                         reads=["big", "best"], writes=["s2"])
                    P.op("dve", lambda e, h=h, n=n: e.max(out=best[:n, h, 8:16], in_=s2[:n, :]), reads=["s2"], writes=["best"])
                    P.op("dve", lambda e, h=h, n=n: e.max_index(out=bcu[:n, h, 8:16], in_max=best[:n, h, 8:16], in_values=s2[:n, :]),
                         reads=["s2", "best"], writes=["bcu"])
                P.op("dve", lambda e, n=n: e.tensor_copy(out=bcf[:n, :], in_=bcu[:n].rearrange("p h k -> p (h k)")), reads=["bcu"], writes=["bcf"])
                P.op("dve", lambda e, n=n: e.tensor_tensor(out=eq[:n], in0=bcf[:n, :].unsqueeze(2).to_broadcast([n, 128, 16]),
                                                          in1=thr16[:n, :].unsqueeze(1).to_broadcast([n, 128, 16]), op=ALU.is_ge),
                     reads=["bcf", "thr16"], writes=["big"])
                P.op("dve", lambda e, n=n: e.tensor_reduce(out=akf[:n, :], in_=eq[:n], axis=AX.X, op=ALU.add), reads=["big"], writes=["akf"])
                P.op("dve", lambda e, n=n: e.tensor_scalar(out=akf[:n, :], in0=akf[:n, :], scalar1=-1.0, scalar2=None, op0=ALU.add), reads=["akf"], writes=["akf"])
                P.op("dve", lambda e, n=n: e.scalar_tensor_tensor(out=bkf[:n, :], in0=akf[:n, :], scalar=-16.0, in1=bcf[:n, :], op0=ALU.mult, op1=ALU.add),
                     reads=["akf", "bcf"], writes=["bkf"])
                i4 = idxf[:n].rearrange("p (h c) k -> p h c k", c=2)
                for (sel_f, cidx, dst_i, nm) in ((akf, 0, i1f, "i1f"), (bkf, 1, i2f, "i2f")):
                    P.op("dve", lambda e, sel_f=sel_f, n=n: e.tensor_tensor(out=eq[:n], in0=iota16[:n, :].unsqueeze(1).to_broadcast([n, 128, 16]),
                                                                           in1=sel_f[:n, :].unsqueeze(2).to_broadcast([n, 128, 16]), op=ALU.is_equal),
                         reads=["iota16", "akf", "bkf"], writes=["big"])
                    P.op("dve", lambda e, cidx=cidx, i4=i4, n=n: e.tensor_tensor(out=eq[:n].rearrange("p (h k) a -> p h k a", k=16),
                                                                               in0=eq[:n].rearrange("p (h k) a -> p h k a", k=16),
                                                                               in1=i4[:, :, cidx, :].unsqueeze(2).to_broadcast([n, 8, 16, 16]), op=ALU.mult),
                         reads=["big", "idxf"], writes=["big"])
                    P.op("dve", lambda e, dst_i=dst_i, n=n: e.tensor_reduce(out=dst_i[:n, :], in_=eq[:n], axis=AX.X, op=ALU.add), reads=["big"], writes=[nm])
                P.op("dve", lambda e, n=n: e.scalar_tensor_tensor(out=ef[:n, :], in0=i1f[:n, :], scalar=128.0, in1=i2f[:n, :], op0=ALU.mult, op1=ALU.add),
                     reads=["i1f", "i2f"], writes=["ef"])
                P.op("dve", lambda e, n=n: e.tensor_copy(out=eidx[:n, :], in_=ef[:n, :]), reads=["ef"], writes=["eidx"])
                P.op("dve", lambda e, n=n: e.tensor_tensor(out=gate[:n], in0=best[:n], in1=best[:n, :, 0:1].to_broadcast([n, 8, 16]), op=ALU.subtract),
                     reads=["best"], writes=["gate"])
                P.op("act", lambda e, n=n: e.activation(out=gate[:n], in_=gate[:n], func=AF.Exp), reads=["gate"], writes=["gate"])
                P.op("dve", lambda e, n=n: e.tensor_reduce(out=gsum[:n, :], in_=gate[:n], axis=AX.X, op=ALU.add), reads=["gate"], writes=["gsum"])
                P.op("dve", lambda e, n=n: e.reciprocal(out=gsum[:n, :], in_=gsum[:n, :]), reads=["gsum"], writes=["gsum"])
                P.op("dve", lambda e, n=n: e.tensor_tensor(out=gate[:n], in0=gate[:n], in1=gsum[:n, :].unsqueeze(2).to_broadcast([n, 8, 16]), op=ALU.mult),
                     reads=["gate", "gsum"], writes=["gate"])
                for slot in range(128):
                    ub = ucnt[0] % NUB
                    ucnt[0] += 1
                    P.op("pool", lambda e, ub=ub, slot=slot, n=n: e.indirect_dma_start(
                        out=ubuf[ub][:n, :], out_offset=None, in_=pu, in_offset=bass.IndirectOffsetOnAxis(ap=eidx[:n, slot:slot + 1], axis=0)),
                        reads=["eidx"], writes=[("ubuf", ub)], dma=True)
                    P.op("dve", lambda e, ub=ub, slot=slot, n=n: e.scalar_tensor_tensor(
                        out=junk6[:n, :], in0=ubuf[ub][:n, :], scalar=1.0, in1=hn[:n, :], op0=ALU.mult, op1=ALU.mult,
                        accum_out=act_[:n, slot:slot + 1]),
                        reads=[("ubuf", ub), "hn"], writes=["junk6", "act_"])
                P.op("act", lambda e, n=n: e.activation(out=wgt[:n, :], in_=act_[:n, :], func=AF.Gelu), reads=["act_"], writes=["wgt"])
                P.op("dve", lambda e, n=n: e.tensor_tensor(out=wgt[:n, :], in0=wgt[:n, :], in1=gate[:n].rearrange("p h k -> p (h k)"), op=ALU.mult),
                     reads=["wgt", "gate"], writes=["wgt"])
                for slot in range(128):
                    ub = ucnt[0] % NUB
                    ucnt[0] += 1
                    P.op("pool", lambda e, ub=ub, slot=slot, n=n: e.indirect_dma_start(
                        out=ubuf[ub][:n, :], out_offset=None, in_=pv, in_offset=bass.IndirectOffsetOnAxis(ap=eidx[:n, slot:slot + 1], axis=0)),
                        reads=["eidx"], writes=[("ubuf", ub)], dma=True)
                    P.op("dve", lambda e, ub=ub, slot=slot, n=n, hi=hi: e.scalar_tensor_tensor(
                        out=hi, in0=ubuf[ub][:n, :], scalar=wgt[:n, slot:slot + 1], in1=hi, op0=ALU.mult, op1=ALU.add),
                        reads=[("ubuf", ub), "wgt", hk], writes=[hk])
                dsty = yp[i * 128:(i + 1) * 128, :] if i < 8 else ys
                P.op("sp", lambda e, dsty=dsty, hi=hi: e.dma_start(out=dsty, in_=hi), reads=[hk], writes=[("y", i)], dma=True)
            P.barrier()
        es5.close()
        P.barrier()
        P.emit()
    return nc


def make_in_maps(inp, with_peer=True):
    xp = np.asarray(inp["x_prompt"], np.float32)
    xs = np.asarray(inp["x_sample"], np.float32)[:, 0, :]
    tabs = {"tab_" + u["name"]: np.ascontiguousarray(unit_variants(u)[2].reshape(128, -1)) for u in UNITS}
    shared = {
        "norm1_w": inp["norm1_w"][0], "w_in": inp["w_in"][0], "qna": inp["q_norm_a"][0], "kna": inp["k_norm_a"][0],
        "sink": inp["sink_a"][0], "qnb": inp["q_norm_b"][0], "knb": inp["k_norm_b"][0],
        "wba": inp["w_branch_a"][0], "wbb": inp["w_branch_b"][0], "wout": inp["w_out"][0],
        "norm2_w": inp["norm2_w"][0], "wq": inp["peer_wq"][0], "subk": inp["peer_subkeys"][0],
    }
    if with_peer:
        shared["pu"] = inp["peer_u"][0]; shared["pv"] = inp["peer_v"][0]
    shared = {k: np.ascontiguousarray(np.asarray(v, np.float32)) for k, v in shared.items()}
    shared.update(tabs)
    sb_ = np.zeros((128, 20), np.float32)
    ii = np.arange(128, dtype=np.float64)
    for h in range(20):
        dil = 1 if h < 12 else (4 if h < 16 else 16)
        sb_[:, h] = -SLOPES[h] * dil * (128.0 - ii)
    shared["sbias"] = sb_
    shared["dmask"] = np.ascontiguousarray(np.broadcast_to(np.eye(NS, dtype=np.float32).reshape(1, NS * NS), (128, NS * NS)))
    shared["iota16"] = np.ascontiguousarray(np.broadcast_to(np.arange(16, dtype=np.float32)[None, :], (128, 16)))
    shared["subkT"] = np.ascontiguousarray(np.asarray(inp["peer_subkeys"], np.float32)[0].transpose(0, 2, 1))
    maps = []
    for c in range(8):
        b, half = c // 2, c % 2
        m = dict(shared)
        m["xo"] = np.ascontiguousarray(xp[b, half * 1024:(half + 1) * 1024])
        m["xh"] = np.ascontiguousarray(xp[b, 0:1024]) if half == 1 else np.zeros((1024, D), np.float32)
        m["xs"] = np.ascontiguousarray(xs[c * NS:(c + 1) * NS])
        m["flag"] = np.ascontiguousarray(np.broadcast_to(np.array([[1.0, float(half)]], np.float32), (128, 2)))
        for nm, key in (("ca", "cache_a_kv"), ("cb1", "cache_b1_kv"), ("cb2", "cache_b2_kv"), ("cb3", "cache_b3_kv")):
            a = np.asarray(inp[key], np.float32)[0, c * NS:(c + 1) * NS]
            m[nm] = np.ascontiguousarray(a.reshape(NS, a.shape[1], -1))
        maps.append(m)
    return maps


_NC_CACHE = {}


def kernel(**inputs):
    if "nc" not in _NC_CACHE:
        _NC_CACHE["nc"] = build()
    nc = _NC_CACHE["nc"]
    maps = make_in_maps(inputs)
    res = run_bass_kernel_spmd(nc, maps, core_ids=list(range(8))).results
    y_p = np.zeros((4, 2048, D), np.float32)
    y_s = np.zeros((128, 1, D), np.float32)
    a_p = np.zeros((1, 4, 128, 2, 2, 128), np.float32)
    b1_p = np.zeros((1, 4, 128, 2, 4, 128), np.float32)
    b2_p = np.zeros((1, 4, 512, 2, 4, 128), np.float32)
    b3_p = np.zeros((1, 4, 2048, 2, 4, 128), np.float32)
    a_s = np.zeros((1, 128, 128, 2, 2, 128), np.float32)
    b1_s = np.zeros((1, 128, 128, 2, 4, 128), np.float32)
    b2_s = np.zeros((1, 128, 512, 2, 4, 128), np.float32)
    b3_s = np.zeros((1, 128, 2048, 2, 4, 128), np.float32)
    for c in range(8):
        b, half = c // 2, c % 2
        r = res[c]
        y_p[b, half * 1024:(half + 1) * 1024] = r["yp"]
        y_s[c * NS:(c + 1) * NS, 0] = r["ys"]
        b3_p[0, b, half * 1024:(half + 1) * 1024] = r["kvb3_p"].reshape(1024, 2, 4, 128)
        if half == 1:
            a_p[0, b] = r["kva_p"].reshape(128, 2, 2, 128)
            b1_p[0, b] = r["kvb1_p"].reshape(128, 2, 4, 128)
            b2_p[0, b] = r["kvb2_p"].reshape(512, 2, 4, 128)
        a_s[0, c * NS:(c + 1) * NS] = r["kva_s"].reshape(NS, 128, 2, 2, 128)
        b1_s[0, c * NS:(c + 1) * NS] = r["kvb1_s"].reshape(NS, 128, 2, 4, 128)
        b2_s[0, c * NS:(c + 1) * NS] = r["kvb2_s"].reshape(NS, 512, 2, 4, 128)
        b3_s[0, c * NS:(c + 1) * NS] = r["kvb3_s"].reshape(NS, 2048, 2, 4, 128)
    return (y_p, y_s, a_p, b1_p, b2_p, b3_p, a_s, b1_s, b2_s, b3_s)
```

```python
import numpy as np
from contextlib import ExitStack
import concourse.bass as bass
import concourse.mybir as mybir
from concourse.bass_utils import run_bass_kernel_spmd

F32 = mybir.dt.float32
BF16 = mybir.dt.bfloat16
I32 = mybir.dt.int32
U32 = mybir.dt.uint32
AF = mybir.ActivationFunctionType
ALU = mybir.AluOpType
AX = mybir.AxisListType

STREAMS = ("pe", "act", "dve", "pool", "sp")
NDMA_SEMS = 8


class Prog:
    def __init__(self, nc):
        self.nc = nc
        self.ops = {s: [] for s in STREAMS}
        self.semnames = list(STREAMS) + [f"d{s}{j}" for s in ("sp", "act", "pool") for j in range(NDMA_SEMS)]
        self.count = {n: 0 for n in self.semnames}
        self.dma_i = {"sp": 0, "act": 0, "pool": 0}
        self.last_w = {}
        self.readers = {}
        self.waited = {s: {} for s in STREAMS}
        self.n_ops = 0

    def _deps(self, reads, writes):
        deps = {}

        def add(d):
            if d is not None and d[1] > deps.get(d[0], 0):
                deps[d[0]] = d[1]

        for k in reads:
            add(self.last_w.get(k))
        for k in writes:
            add(self.last_w.get(k))
            for r in self.readers.get(k, ()):
                add(r)
        return deps

    def op(self, stream, fn, reads=(), writes=(), dma=False):
        deps = self._deps(reads, writes)
        if dma:
            dname = f"d{stream}{self.dma_i[stream] % NDMA_SEMS}"
            if self.count[dname] > 0:
                deps[dname] = max(deps.get(dname, 0), self.count[dname])
        if stream == "pe":
            deps.pop("pe", None)
        w = self.waited[stream]
        waits = []
        for sname, v in deps.items():
            if v > w.get(sname, 0):
                waits.append((sname, v))
                w[sname] = v
        if dma:
            j = self.dma_i[stream] % NDMA_SEMS
            self.dma_i[stream] += 1
            sname = f"d{stream}{j}"
            inc = 16
        else:
            sname = stream
            inc = 1
        self.count[sname] += inc
        val = self.count[sname]
        self.ops[stream].append((waits, fn, sname, inc))
        for k in reads:
            self.readers.setdefault(k, []).append((sname, val))
        for k in writes:
            self.last_w[k] = (sname, val)
            self.readers[k] = []
        self.n_ops += 1
        return (sname, val)

    def barrier(self):
        waits = [(n, c) for n, c in self.count.items() if c > 0]
        for s in STREAMS:
            w = self.waited[s]
            ws = [(n, c) for n, c in waits if c > w.get(n, 0)]
            for n, c in ws:
                w[n] = c
            if ws:
                self.ops[s].append((ws, None, None, 0))
        self.last_w = {}
        self.readers = {}

    def emit(self):
        nc = self.nc
        with ExitStack() as es:
            sems = {n: es.enter_context(nc.semaphore(n)) for n in self.semnames}
            block = es.enter_context(nc.Block())

            def make(stream):
                def body(eng):
                    for waits, fn, sname, inc in self.ops[stream]:
                        for wn, wv in waits:
                            eng.wait_ge(sems[wn], wv)
                        if fn is not None:
                            fn(eng).then_inc(sems[sname], inc)
                return body

            block.tensor(make("pe"))
            block.scalar(make("act"))
            block.vector(make("dve"))
            block.gpsimd(make("pool"))
            block.sync(make("sp"))


D = 2048
NTOK = 1024
NT = 8
NS = 16
NIN = 10240
EPS = 1e-6
SCALE = 128.0 ** -0.5
SLOPES = [float(2.0 ** (-8.0 * i / 20.0)) for i in range(1, 21)]
XT_H, XT_O, XT_S = 0, 1024, 2048
NXT = 2064

GROUPS = {
    "A": dict(nh=1, nd=2, dil=1, win=128, H=2, L=128),
    "B1": dict(nh=1, nd=2, dil=1, win=128, H=4, L=128),
    "B2": dict(nh=4, nd=5, dil=4, win=512, H=4, L=512),
    "B3": dict(nh=8, nd=16, dil=16, win=2048, H=4, L=2048),
}


def make_units():
    units = []
    for kvh in range(2):
        units.append(dict(name=f"A{kvh}", grp="A", nq=4, nk=1,
                          qjobs=[(kvh * 512, 256, 0), (kvh * 512 + 256, 256, 2)],
                          kcol=1024 + kvh * 128, vcol=1280 + kvh * 128, kvhead0=kvh,
                          slopes=[SLOPES[kvh * 4 + g] for g in range(4)], ohead0=kvh * 4))
    for g, gname in enumerate(("B1", "B2", "B3")):
        for hp in range(2):
            units.append(dict(name=f"{gname}_{hp}", grp=gname, nq=2, nk=2,
                              qjobs=[(1536 + g * 512 + hp * 256, 256, 0)],
                              kcol=3072 + g * 512 + hp * 256, vcol=4608 + g * 512 + hp * 256,
                              kvhead0=hp * 2,
                              slopes=[SLOPES[8 + g * 4 + hp * 2 + j] for j in range(2)], ohead0=hp * 2))
    return units


UNITS = make_units()


def unit_variants(u):
    G = GROUPS[u["grp"]]
    nd, dil, win = G["nd"], G["dil"], G["win"]
    s = np.arange(128)[:, None].astype(np.float64)
    q = np.arange(128)[None, :].astype(np.float64)
    if u["grp"] == "B3":
        nvar = 2
        vmap = [(0, [0.0] * u["nq"])] + [(1, [-sl * 128.0 * (dl - 1) for sl in u["slopes"]]) for dl in range(1, nd)]
        var_delta = [0, 1]
    else:
        nvar = nd
        vmap = [(dl, [0.0] * u["nq"]) for dl in range(nd)]
        var_delta = list(range(nd))
    tab = np.zeros((128, u["nq"], nvar, 128), np.float32)
    for j, sl in enumerate(u["slopes"]):
        for v, dl in enumerate(var_delta):
            dist = 128.0 * dl + q - s
            ok = (dist >= 0) & (dist <= win) & (np.mod(dist, dil) == 0)
            tab[:, j, v, :] = np.where(ok, np.exp(-sl * np.where(ok, dist, 0.0)), 0.0)
    return nvar, vmap, tab


def build(stop_after=None, debug=False, bis=None):
    nc = bass.Bass("TRN2", target_bir_lowering=False)

    def din(name, shape, dt=F32):
        return nc.dram_tensor(name, list(shape), dt, kind="ExternalInput").ap()

    def dout(name, shape, dt=F32):
        return nc.dram_tensor(name, list(shape), dt, kind="ExternalOutput").ap()

    xo = din("xo", [NTOK, D]); xh = din("xh", [NTOK, D]); xs = din("xs", [NS, D])
    flag = din("flag", [128, 2])
    caches = {"A": din("ca", [NS, 128, 512]), "B1": din("cb1", [NS, 128, 1024]),
              "B2": din("cb2", [NS, 512, 1024]), "B3": din("cb3", [NS, 2048, 1024])}
    norm1_w = din("norm1_w", [D]); w_in = din("w_in", [D, NIN])
    qna = din("qna", [128]); kna = din("kna", [128]); sink = din("sink", [8])
    qnb = din("qnb", [128]); knb = din("knb", [128])
    wba = din("wba", [1024, D]); wbb = din("wbb", [512, D]); wout = din("wout", [D, D])
    norm2_w = din("norm2_w", [D]); wq = din("wq", [D, 1024]); subk = din("subk", [2, 128, 64])
    with_peer = stop_after is None and not debug
    if with_peer:
        pu = din("pu", [16384, D]); pv = din("pv", [16384, D])
    sbias_d = din("sbias", [128, 20]); dmask_d = din("dmask", [128, NS * NS]); iota16_d = din("iota16", [128, 16])
    subkT = din("subkT", [2, 64, 128])
    tabs_d = {}
    for u in UNITS:
        nvar, _, _ = unit_variants(u)
        tabs_d[u["name"]] = din("tab_" + u["name"], [128, u["nq"] * nvar * 128])

    yp = dout("yp", [NTOK, D]); ys = dout("ys", [NS, D])
    kvp = {"A": dout("kva_p", [128, 512]), "B1": dout("kvb1_p", [128, 1024]),
           "B2": dout("kvb2_p", [512, 1024]), "B3": dout("kvb3_p", [1024, 1024])}
    kvs = {"A": dout("kva_s", [NS, 128, 512]), "B1": dout("kvb1_s", [NS, 128, 1024]),
           "B2": dout("kvb2_s", [NS, 512, 1024]), "B3": dout("kvb3_s", [NS, 2048, 1024])}
    dbg = {}
    if debug:
        dbg["oaT"] = dout("dbg_oaT", [128, 8, 1040], BF16)
        dbg["obT"] = dout("dbg_obT", [128, 4, 1040], BF16)

    P = Prog(nc)
    with ExitStack() as es:
        def sb(name, shape, dt=F32):
            return es.enter_context(nc.sbuf_tensor(name, list(shape), dt))

        def ps(name, shape, dt=F32):
            return es.enter_context(nc.psum_tensor(name, list(shape), dt))

        ident = sb("ident", [128, 128])
        flag_t = sb("flag_t", [128, 2])
        gains = sb("gains", [128, 4, 128])
        esink = sb("esink", [128, 8])
        esA = ExitStack()

        def sbA(name, shape, dt=F32):
            return esA.enter_context(nc.sbuf_tensor(name, list(shape), dt))
        xnT = sbA("xnT", [128, 16, NXT], BF16)
        wring = [sbA(f"wring{i}", [128, 16, 256], BF16) for i in range(2)]
        oaT = sbA("oaT", [128, 8, 1040], BF16)
        obT = sbA("obT", [128, 4, 1040], BF16)
        pz = ps("pz", [128, 2, 512])
        ptr = ps("ptr", [128, 2, 512])
        pst = ps("pst", [128, 2, 512])
        po = ps("po", [128, 4, 256])

        P.op("pool", lambda e: e.memset(ident[:], 0.0), writes=["ident"])
        P.op("pool", lambda e: e.affine_select(out=ident[:], in_=ident[:], pattern=[[-1, 128]],
                                               compare_op=ALU.not_equal, fill=1.0, base=0, channel_multiplier=1),
             reads=["ident"], writes=["ident"])
        P.op("sp", lambda e: e.dma_start(out=flag_t[:], in_=flag), writes=["flag"], dma=True)
        for i, g in enumerate((qna, kna, qnb, knb)):
            P.op("sp", lambda e, i=i, g=g: e.dma_start(out=gains[:, i, :], in_=g.partition_broadcast(128)),
                 writes=[("gains", i)], dma=True)
        P.op("sp", lambda e: e.dma_start(out=esink[:], in_=sink.partition_broadcast(128)), writes=["esink"], dma=True)
        P.op("act", lambda e: e.activation(out=esink[:], in_=esink[:], func=AF.Exp), reads=["esink"], writes=["esink"])
        copy_jobs = [("A", 0, NS), ("B1", 0, NS)] + [("B2", t0, 4) for t0 in range(0, NS, 4)] + [("B3", t0, 1) for t0 in range(NS)]

        def issue_copies(k):
            for (gname, t0, nt_) in copy_jobs[k::8]:
                L = GROUPS[gname]["L"]
                P.op("act", lambda e, gname=gname, L=L, t0=t0, nt_=nt_: e.dma_start(
                    out=kvs[gname][t0:t0 + nt_, 0:L - 1, :], in_=caches[gname][t0:t0 + nt_, 1:L, :]),
                    writes=[("kvs_copy", gname, t0)], dma=True)

        with ExitStack() as es1:
            def sb1(name, shape, dt=F32):
                return es1.enter_context(nc.sbuf_tensor(name, list(shape), dt))
            w1rep = sb1("w1rep", [128, D])
            xbuf = [sb1(f"xbuf{i}", [128, D]) for i in range(2)]
            xnb = [sb1(f"xnb{i}", [128, D]) for i in range(2)]
            junk = sb1("junk1", [128, D], BF16)
            st1 = [sb1(f"st1_{i}", [128, 4]) for i in range(2)]
            P.op("sp", lambda e: e.dma_start(out=w1rep[:], in_=norm1_w.partition_broadcast(128)), writes=["w1rep"], dma=True)
            tiles = [(xh, i, XT_H + i * 128, 128) for i in range(8)] + [(xo, i, XT_O + i * 128, 128) for i in range(8)] + [(xs, 0, XT_S, NS)]
            for ti, (src, i, toff, n) in enumerate(tiles):
                s = ti % 2
                xb, xn_, st = xbuf[s], xnb[s], st1[s]
                P.op("sp", lambda e, xb=xb, src=src, i=i, n=n: e.dma_start(out=xb[:n, :], in_=src[i * 128:i * 128 + n, :]),
                     writes=[("xbuf", s)], dma=True)
                P.op("act", lambda e, xb=xb, st=st, n=n: e.activation(out=junk[:n, :], in_=xb[:n, :], func=AF.Square, accum_out=st[:n, 0:1]),
                     reads=[("xbuf", s)], writes=["junk1", ("st1", s)])
                P.op("act", lambda e, st=st, n=n: e.activation(out=st[:n, 1:2], in_=st[:n, 0:1], func=AF.Ln, scale=1.0 / D, bias=EPS),
                     reads=[("st1", s)], writes=[("st1", s)])
                P.op("act", lambda e, st=st, n=n: e.activation(out=st[:n, 2:3], in_=st[:n, 1:2], func=AF.Exp, scale=-0.5),
                     reads=[("st1", s)], writes=[("st1", s)])
                P.op("dve", lambda e, xb=xb, xn_=xn_, st=st, n=n: e.scalar_tensor_tensor(
                    out=xn_[:n, :], in0=xb[:n, :], scalar=st[:n, 2:3], in1=w1rep[:n, :], op0=ALU.mult, op1=ALU.mult),
                    reads=[("xbuf", s), ("st1", s), "w1rep"], writes=[("xnb", s)])
                for k4 in range(4):
                    bank = (ti * 4 + k4) % 2
                    for kk in range(4):
                        k = k4 * 4 + kk
                        P.op("pe", lambda e, xn_=xn_, k=k, kk=kk, bank=bank, n=n: e.transpose(
                            out=ptr[:, bank, kk * 128:kk * 128 + n], in_=xn_[:n, k * 128:(k + 1) * 128], identity=ident[:n, :n]),
                            reads=[("xnb", s), "ident"], writes=[("ptr", bank)])
                    eng = "act" if k4 % 2 == 0 else "dve"
                    if eng == "act":
                        P.op("act", lambda e, k4=k4, bank=bank, toff=toff, n=n: e.activation(
                            out=xnT[:, k4 * 4:(k4 + 1) * 4, toff:toff + n],
                            in_=ptr[:, bank, :].rearrange("p (a b) -> p a b", b=128)[:, :, :n], func=AF.Copy),
                            reads=[("ptr", bank)], writes=[("xnT", toff)])
                    else:
                        P.op("dve", lambda e, k4=k4, bank=bank, toff=toff, n=n: e.tensor_copy(
                            out=xnT[:, k4 * 4:(k4 + 1) * 4, toff:toff + n],
                            in_=ptr[:, bank, :].rearrange("p (a b) -> p a b", b=128)[:, :, :n]),
                            reads=[("ptr", bank)], writes=[("xnT", toff)])
            P.barrier()

        wcount = [0]
        ring = list(wring)

        def load_w(src, col0, ncols, nk=16):
            s = wcount[0] % len(ring)
            wcount[0] += 1
            wt = ring[s]
            P.op("pool", lambda e: e.dma_start(out=wt[:, :nk, :ncols],
                                               in_=src[0:nk * 128, col0:col0 + ncols].rearrange("(k p) n -> p k n", p=128)),
                 writes=[("wring", s)], dma=True)
            return wt, ("wring", s)

        if stop_after == 1:
            P.barrier(); P.emit(); esA.close(); return nc

        with ExitStack() as es2:
            def sb2(name, shape, dt=F32):
                return es2.enter_context(nc.sbuf_tensor(name, list(shape), dt))
            QT = sb2("QT", [128, 4, NTOK], BF16)
            accBv = sb2("accBv", [128, NT, 4, 128])
            accBd = sb2("accBd", [128, NT, 4])
            onesc = sb2("onesc", [128, 2], BF16)
            po2 = po[:].rearrange("p a b -> p (a b)")
            KT = sb2("KT", [128, 2, 16 * 128], BF16)
            VA = sb2("VA", [128, 16, 2, 128], BF16)
            tabt = sb2("tabt", [128, 2 * 5 * 128], BF16)
            sq = sb2("sq", [128, 256])
            zn = [sb2(f"zn{i}", [128, 256]) for i in range(2)]
            vf = [sb2(f"vf{i}", [128, 256]) for i in range(2)]
            st2 = [sb2(f"st2_{i}", [128, 8]) for i in range(2)]
            Eb = [sb2(f"Eb{i}", [128, 512]) for i in range(2)]
            Pm = [sb2(f"Pm{i}", [128, 4096], BF16) for i in range(2)]
            ot = [sb2(f"ot{i}", [128, 4, 128]) for i in range(2)]
            rc = [sb2(f"rc{i}", [128, 4]) for i in range(2)]
            cnt = dict(z=0, tr=0, zn=0, vf=0, st=0, E=0, pm=0, ot=0)

            def proj_tile(wt, wkey, ncols, toff, n):
                b = cnt["z"] % 2
                cnt["z"] += 1
                for k in range(16):
                    P.op("pe", lambda e, k=k, b=b: e.matmul(pz[:n, b, :ncols], lhsT=xnT[:, k, toff:toff + n], rhs=wt[:, k, :ncols],
                                                         start=(k == 0), stop=(k == 15)),
                         reads=[("xnT", toff), wkey], writes=[("pz", b)])
                return b

            def qk_norm(b, nheads, gain_idx, n):
                ncols = nheads * 128
                s = cnt["zn"] % 2
                cnt["zn"] += 1
                st = st2[s]
                P.op("act", lambda e: e.activation(out=sq[:n, :ncols], in_=pz[:n, b, :ncols], func=AF.Square),
                     reads=[("pz", b)], writes=["sq"])
                P.op("dve", lambda e: e.tensor_reduce(out=st[:n, 0:nheads], in_=sq[:n, :ncols].rearrange("p (h d) -> p h d", d=128),
                                                      axis=AX.X, op=ALU.add),
                     reads=["sq"], writes=[("st2", s)])
                P.op("act", lambda e: e.activation(out=st[:n, 2:2 + nheads], in_=st[:n, 0:nheads], func=AF.Ln, scale=1.0 / 128, bias=EPS),
                     reads=[("st2", s)], writes=[("st2", s)])
                P.op("act", lambda e: e.activation(out=st[:n, 4:4 + nheads], in_=st[:n, 2:2 + nheads], func=AF.Exp, scale=-0.5),
                     reads=[("st2", s)], writes=[("st2", s)])
                for h in range(nheads):
                    P.op("dve", lambda e, h=h: e.scalar_tensor_tensor(
                        out=zn[s][:n, h * 128:(h + 1) * 128], in0=pz[:n, b, h * 128:(h + 1) * 128], scalar=st[:n, 4 + h:5 + h],
                        in1=gains[:n, gain_idx, :], op0=ALU.mult, op1=ALU.mult),
                        reads=[("pz", b), ("st2", s), ("gains", gain_idx)], writes=[("zn", s)])
                return s

            def transpose_to(src_ap_fn, nheads, n, dst_ap, dkey, rkeys):
                bank = cnt["tr"] % 2
                cnt["tr"] += 1
                for h in range(nheads):
                    P.op("pe", lambda e, h=h: e.transpose(out=ptr[:, bank, h * 128:h * 128 + n], in_=src_ap_fn(h), identity=ident[:n, :n]),
                         reads=list(rkeys) + ["ident"], writes=[("ptr", bank)])
                P.op("act", lambda e: e.activation(out=dst_ap, in_=ptr[:, bank, :nheads * 128].rearrange("p (a b) -> p a b", b=128)[:, :, :n],
                                                   func=AF.Copy),
                     reads=[("ptr", bank)], writes=[dkey])

            for ui, u in enumerate(UNITS):
                if bis is not None and ui >= bis[0]:
                    break
                issue_copies(ui)
                G = GROUPS[u["grp"]]
                nh, nd = G["nh"], G["nd"]
                nq, nk = u["nq"], u["nk"]
                gq = nq // nk
                isA = u["grp"] == "A"
                nvar, vmap, _ = unit_variants(u)
                W = G["H"] * 128
                P.op("pool", lambda e, u=u, nvar=nvar, nq=nq: e.dma_start(out=tabt[:, :nq * nvar * 128], in_=tabs_d[u["name"]]),
                     writes=["tabt"], dma=True)
                for (c0, nc_, h0) in u["qjobs"]:
                    wt, wkey = load_w(w_in, c0, nc_)
                    for i in range(NT):
                        if bis is not None and len(bis) > 2 and bis[2] < 2:
                            break
                        b = proj_tile(wt, wkey, nc_, XT_O + i * 128, 128)
                        s = qk_norm(b, nc_ // 128, 0 if isA else 2, 128)
                        transpose_to(lambda h, s=s: zn[s][:, h * 128:(h + 1) * 128], nc_ // 128, 128,
                                     QT[:, h0:h0 + nc_ // 128, i * 128:(i + 1) * 128], ("QT", i), [("zn", s)])
                if bis is not None and len(bis) > 2 and bis[2] < 3:
                    continue
                nkc = nk * 128
                wt, wkey = load_w(w_in, u["kcol"], nkc)
                ktiles = [(XT_H + (8 - nh + j) * 128, j, None) for j in range(nh)] + [(XT_O + i * 128, nh + i, i) for i in range(NT)]
                out_tiles = {"A": [7], "B1": [7], "B2": [4, 5, 6, 7], "B3": list(range(8))}[u["grp"]]
                for (toff, kti, own_i) in ktiles:
                    b = proj_tile(wt, wkey, nkc, toff, 128)
                    s = qk_norm(b, nk, 1 if isA else 3, 128)
                    if own_i is not None and own_i in out_tiles:
                        r0 = (own_i - out_tiles[0]) * 128
                        cc = u["kvhead0"] * 128
                        P.op("sp", lambda e, s=s, r0=r0, cc=cc, nkc=nkc, grp=u["grp"]: e.dma_start(
                            out=kvp[grp][r0:r0 + 128, cc:cc + nkc], in_=zn[s][:, :nkc]),
                            reads=[("zn", s)], writes=[("kvp", u["name"], "k", own_i)], dma=True)
                    transpose_to(lambda h, s=s: zn[s][:, h * 128:(h + 1) * 128], nk, 128,
                                 KT[:, 0:nk, kti * 128:(kti + 1) * 128], ("KT", kti), [("zn", s)])
                if bis is not None and len(bis) > 2 and bis[2] == 3.5:
                    load_w(w_in, u["vcol"], nkc)
                    load_w(w_in, u["vcol"], nkc)
                    continue
                if bis is not None and len(bis) > 2 and bis[2] < 4:
                    continue
                vm = bis[3] if (bis is not None and len(bis) > 3) else 15
                if vm & 1:
                    P.op("dve", lambda e: e.tensor_copy(out=onesc[:, 0:2], in_=flag_t[:, 0:2]), reads=["flag"], writes=["onesc"])
                wt, wkey = load_w(w_in, u["vcol"], nkc)
                for (toff, kti, own_i) in ktiles:
                    b = proj_tile(wt, wkey, nkc, toff, 128)
                    is_out = own_i is not None and own_i in out_tiles
                    if not is_out:
                        P.op("dve", lambda e, b=b, kti=kti, nk=nk, nkc=nkc: e.tensor_copy(
                            out=VA[:, kti, 0:nk, :], in_=pz[:, b, :nkc].rearrange("p (h d) -> p h d", d=128)),
                            reads=[("pz", b)], writes=[("VA", kti)])
                    else:
                        s = cnt["vf"] % 2
                        cnt["vf"] += 1
                        r0 = (own_i - out_tiles[0]) * 128
                        cc = W + u["kvhead0"] * 128
                        P.op("dve", lambda e, b=b, s=s, nkc=nkc: e.tensor_copy(out=vf[s][:, :nkc], in_=pz[:, b, :nkc]),
                             reads=[("pz", b)], writes=[("vf", s)])
                        P.op("pool", lambda e, s=s, kti=kti, nk=nk, nkc=nkc: e.tensor_copy(
                            out=VA[:, kti, 0:nk, :], in_=vf[s][:, :nkc].rearrange("p (h d) -> p h d", d=128)),
                            reads=[("vf", s)], writes=[("VA", kti)])
                        P.op("sp", lambda e, s=s, r0=r0, cc=cc, nkc=nkc, grp=u["grp"]: e.dma_start(
                            out=kvp[grp][r0:r0 + 128, cc:cc + nkc], in_=vf[s][:, :nkc]),
                            reads=[("vf", s)], writes=[("kvp", u["name"], "v", own_i)], dma=True)
                for qt in range(NT):
                    if bis is not None and bis[1] == 0:
                        break
                    deltas = [dl for dl in range(nd) if qt - dl >= -nh]
                    ps_ = cnt["pm"] % 2
                    cnt["pm"] += 1
                    for di, dl in enumerate(deltas):
                        kti = qt - dl + nh
                        var, biases = vmap[dl]
                        sb_ = cnt["E"] % 2
                        cnt["E"] += 1
                        for k in range(nk):
                            P.op("pe", lambda e, k=k, kti=kti, sb_=sb_, qt=qt, gq=gq: e.matmul(
                                pst[:, sb_, k * gq * 128:(k + 1) * gq * 128], lhsT=KT[:, k, kti * 128:(kti + 1) * 128],
                                rhs=QT[:, k * gq:(k + 1) * gq, qt * 128:(qt + 1) * 128], start=True, stop=True),
                                reads=[("KT", kti), ("QT", qt)], writes=[("pst", sb_)])
                        if all(bv == 0.0 for bv in biases):
                            P.op("act", lambda e, sb_=sb_, nq=nq: e.activation(out=Eb[sb_][:, :nq * 128], in_=pst[:, sb_, :nq * 128],
                                                                             func=AF.Exp, scale=SCALE),
                                 reads=[("pst", sb_)], writes=[("Eb", sb_)])
                        else:
                            for j in range(nq):
                                P.op("act", lambda e, sb_=sb_, j=j, bv=biases[j]: e.activation(
                                    out=Eb[sb_][:, j * 128:(j + 1) * 128], in_=pst[:, sb_, j * 128:(j + 1) * 128],
                                    func=AF.Exp, scale=SCALE, bias=bv),
                                    reads=[("pst", sb_)], writes=[("Eb", sb_)])
                        P.op("dve", lambda e, sb_=sb_, ps_=ps_, di=di, var=var, nq=nq, nvar=nvar: e.tensor_tensor(
                            out=Pm[ps_][:, di * nq * 128:(di + 1) * nq * 128].rearrange("p (j q) -> p j q", q=128),
                            in0=Eb[sb_][:, :nq * 128].rearrange("p (j q) -> p j q", q=128),
                            in1=tabt[:, :nq * nvar * 128].rearrange("p (j v q) -> p j v q", v=nvar, q=128)[:, :, var, :],
                            op=ALU.mult),
                            reads=[("Eb", sb_), "tabt"], writes=[("Pm", ps_, di)])
                    for j in range(nq):
                        for di, dl in enumerate(deltas):
                            kti = qt - dl + nh
                            P.op("pe", lambda e, j=j, di=di, kti=kti, ps_=ps_, gq=gq, nq=nq, first=(di == 0), last=(di == len(deltas) - 1): e.matmul(
                                po2[:, j * 128:(j + 1) * 128], lhsT=Pm[ps_][:, (di * nq + j) * 128:(di * nq + j + 1) * 128], rhs=VA[:, kti, j // gq, :],
                                start=first, stop=last),
                                reads=[("Pm", ps_, di), ("VA", kti)], writes=["po_num"])
                    for j in range(nq):
                        for di, dl in enumerate(deltas):
                            kti = qt - dl + nh
                            oc = 1 if kti < nh else 0
                            P.op("pe", lambda e, j=j, di=di, oc=oc, ps_=ps_, nq=nq, first=(di == 0), last=(di == len(deltas) - 1): e.matmul(
                                po2[:, 512 + j:513 + j], lhsT=Pm[ps_][:, (di * nq + j) * 128:(di * nq + j + 1) * 128], rhs=onesc[:, oc:oc + 1],
                                start=first, stop=last),
                                reads=[("Pm", ps_, di), "onesc"], writes=["po_den"])
                    if isA:
                        so = cnt["ot"] % 2
                        cnt["ot"] += 1
                        h0 = u["ohead0"]
                        P.op("dve", lambda e, so=so, h0=h0: e.tensor_tensor(out=rc[so][:, 0:4], in0=po2[:, 512:516], in1=esink[:, h0:h0 + 4], op=ALU.add),
                             reads=["po_den", "esink"], writes=[("rc", so)])
                        P.op("dve", lambda e, so=so: e.reciprocal(out=rc[so][:, 0:4], in_=rc[so][:, 0:4]), reads=[("rc", so)], writes=[("rc", so)])
                        P.op("dve", lambda e, so=so: e.tensor_tensor(out=ot[so][:], in0=po2[:, 0:512].rearrange("p (h d) -> p h d", d=128),
                                                                    in1=rc[so][:, 0:4].unsqueeze(2).to_broadcast([128, 4, 128]), op=ALU.mult),
                             reads=["po_num", ("rc", so)], writes=[("ot", so)])
                        transpose_to(lambda h, so=so: ot[so][:, h, :], 4, 128, oaT[:, h0:h0 + 4, qt * 128:(qt + 1) * 128],
                                     ("oaT", h0, qt), [("ot", so)])
                    else:
                        h0 = u["ohead0"]
                        if u["grp"] == "B1":
                            P.op("dve", lambda e, h0=h0, qt=qt: e.tensor_copy(out=accBv[:, qt, h0:h0 + 2, :], in_=po2[:, 0:256].rearrange("p (h d) -> p h d", d=128)),
                                 reads=["po_num"], writes=[("accBv", qt, h0)])
                            P.op("dve", lambda e, h0=h0, qt=qt: e.tensor_copy(out=accBd[:, qt, h0:h0 + 2], in_=po2[:, 512:514]),
                                 reads=["po_den"], writes=[("accBd", qt, h0)])
                        else:
                            P.op("dve", lambda e, h0=h0, qt=qt: e.tensor_tensor(out=accBv[:, qt, h0:h0 + 2, :], in0=po2[:, 0:256].rearrange("p (h d) -> p h d", d=128),
                                                                               in1=accBv[:, qt, h0:h0 + 2, :], op=ALU.add),
                                 reads=["po_num", ("accBv", qt, h0)], writes=[("accBv", qt, h0)])
                            P.op("dve", lambda e, h0=h0, qt=qt: e.tensor_tensor(out=accBd[:, qt, h0:h0 + 2], in0=po2[:, 512:514],
                                                                               in1=accBd[:, qt, h0:h0 + 2], op=ALU.add),
                                 reads=["po_den", ("accBd", qt, h0)], writes=[("accBd", qt, h0)])
            for qt in range(NT):
                if bis is not None:
                    break
                so = cnt["ot"] % 2
                cnt["ot"] += 1
                P.op("dve", lambda e, so=so, qt=qt: e.reciprocal(out=rc[so][:, 0:4], in_=accBd[:, qt, :]),
                     reads=[("accBd", qt, 0), ("accBd", qt, 2)], writes=[("rc", so)])
                P.op("dve", lambda e, so=so, qt=qt: e.tensor_tensor(out=ot[so][:], in0=accBv[:, qt, :, :],
                                                                   in1=rc[so][:, 0:4].unsqueeze(2).to_broadcast([128, 4, 128]), op=ALU.mult),
                     reads=[("accBv", qt, 0), ("accBv", qt, 2), ("rc", so)], writes=[("ot", so)])
                transpose_to(lambda h, so=so: ot[so][:, h, :], 4, 128, obT[:, 0:4, qt * 128:(qt + 1) * 128], ("obT", qt), [("ot", so)])
            P.barrier()

        if stop_after == 2 or bis is not None:
            if debug and bis is None:
                P.op("sp", lambda e: e.dma_start(out=dbg["oaT"][:, :, 0:1024], in_=oaT[:, :, 0:1024]), reads=[], writes=["dbg1"], dma=True)
                P.op("sp", lambda e: e.dma_start(out=dbg["obT"][:, :, 0:1024], in_=obT[:, :, 0:1024]), reads=[], writes=["dbg2"], dma=True)
            P.barrier(); P.emit(); esA.close(); return nc

        with ExitStack() as es3:
            def sb3(name, shape, dt=F32):
                return es3.enter_context(nc.sbuf_tensor(name, list(shape), dt))
            qs = sb3("qs", [NS, 20 * 128]); ks = sb3("ks", [NS, 14 * 128]); vs = sb3("vs", [NS, 14 * 128])
            qsb = sb3("qsb", [NS, 20 * 128], BF16)
            sel = sb3("sel", [NS, NS, 128], BF16)
            kvc = sb3("kvc", [128, NS, 1024], BF16)
            qrep = sb3("qrep", [128, 1024], BF16)
            prod = sb3("prod", [128, 1024])
            Ssb = sb3("Ssb", [128, NS * 8]); S2 = sb3("S2", [128, NS * 8])
            Pb = sb3("Pb", [128, 8, NS], BF16)
            Pexp = sb3("Pexp", [128, 8, NS, NS], BF16)
            sbias_t = sb3("sbias_t", [128, 20]); dmask_t = sb3("dmask_t", [128, NS, NS])
            onesb = sb3("onesb", [128, 1], BF16)
            sq3 = sb3("sq3", [NS, 256]); st3 = [sb3(f"st3_{i}", [NS, 8]) for i in range(2)]
            lg = sb3("lg", [NS, 20]); enew = sb3("enew", [NS, 20])
            tmpv = sb3("tmpv", [NS, 8, 128])
            numA = sb3("numA", [NS, 8, 128]); denA = sb3("denA", [NS, 8])
            numB = sb3("numB", [NS, 4, 128]); denB = sb3("denB", [NS, 4])
            pst_f = pst[:].rearrange("p a b -> p (a b)")
            po_f = po[:].rearrange("p a b -> p (a b)")
            P.op("sp", lambda e: e.dma_start(out=sbias_t[:], in_=sbias_d), writes=["sbias"], dma=True)
            P.op("sp", lambda e: e.dma_start(out=dmask_t[:].rearrange("p a b -> p (a b)"), in_=dmask_d), writes=["dmask"], dma=True)
            P.op("pool", lambda e: e.memset(onesb[:], 1.0), writes=["onesb"])
            P.op("dve", lambda e: e.tensor_copy(out=sel[:], in_=ident[:NS, :NS].unsqueeze(2).to_broadcast([NS, NS, 128])),
                 reads=["ident"], writes=["sel"])
            for c in range(24):
                wt, wkey = load_w(w_in, c * 256, 256)
                b = c % 2
                for k in range(16):
                    P.op("pe", lambda e, k=k, b=b, wt=wt: e.matmul(pz[:NS, b, :256], lhsT=xnT[:, k, XT_S:XT_S + NS], rhs=wt[:, k, :256],
                                                                  start=(k == 0), stop=(k == 15)),
                         reads=[("xnT", XT_S), wkey], writes=[("pz", b)])
                if c < 4:
                    kind, dst, gi = "n", qs[:, c * 256:(c + 1) * 256], 0
                elif c == 4:
                    kind, dst, gi = "n", ks[:, 0:256], 1
                elif c == 5:
                    kind, dst, gi = "v", vs[:, 0:256], None
                elif c < 12:
                    kind, dst, gi = "n", qs[:, 1024 + (c - 6) * 256:1024 + (c - 5) * 256], 2
                elif c < 18:
                    kind, dst, gi = "n", ks[:, 256 + (c - 12) * 256:256 + (c - 11) * 256], 3
                else:
                    kind, dst, gi = "v", vs[:, 256 + (c - 18) * 256:256 + (c - 17) * 256], None
                dkey = ("sdst", c)
                if kind == "v":
                    P.op("act", lambda e, b=b, dst=dst: e.activation(out=dst, in_=pz[:NS, b, :256], func=AF.Copy),
                         reads=[("pz", b)], writes=[dkey, "svs"])
                else:
                    st = st3[c % 2]
                    skey = ("st3", c % 2)
                    P.op("act", lambda e, b=b: e.activation(out=sq3[:, :], in_=pz[:NS, b, :256], func=AF.Square), reads=[("pz", b)], writes=["sq3"])
                    P.op("dve", lambda e, st=st: e.tensor_reduce(out=st[:, 0:2], in_=sq3[:, :].rearrange("p (h d) -> p h d", d=128), axis=AX.X, op=ALU.add),
                         reads=["sq3"], writes=[skey])
                    P.op("act", lambda e, st=st: e.activation(out=st[:, 2:4], in_=st[:, 0:2], func=AF.Ln, scale=1.0 / 128, bias=EPS), reads=[skey], writes=[skey])
                    P.op("act", lambda e, st=st: e.activation(out=st[:, 4:6], in_=st[:, 2:4], func=AF.Exp, scale=-0.5), reads=[skey], writes=[skey])
                    for h in range(2):
                        P.op("dve", lambda e, h=h, b=b, st=st, dst=dst, gi=gi: e.scalar_tensor_tensor(
                            out=dst[:, h * 128:(h + 1) * 128], in0=pz[:NS, b, h * 128:(h + 1) * 128], scalar=st[:, 4 + h:5 + h],
                            in1=gains[:NS, gi, :], op0=ALU.mult, op1=ALU.mult),
                            reads=[("pz", b), skey, ("gains", gi)], writes=[dkey, "sqk"])
            P.op("dve", lambda e: e.tensor_copy(out=qsb[:], in_=qs[:]), reads=["sqk"], writes=["qsb"])
            P.op("dve", lambda e: e.tensor_tensor(out=prod[:NS, 0:1024].rearrange("p (k g d) -> p k g d", k=2, g=4),
                                                  in0=qs[:, 0:1024].rearrange("p (k g d) -> p k g d", k=2, g=4),
                                                  in1=ks[:, 0:256].rearrange("p (k d) -> p k d", k=2).unsqueeze(2).to_broadcast([NS, 2, 4, 128]),
                                                  op=ALU.mult), reads=["sqk"], writes=["prod"])
            P.op("dve", lambda e: e.tensor_reduce(out=lg[:, 0:8], in_=prod[:NS, 0:1024].rearrange("p (h d) -> p h d", d=128), axis=AX.X, op=ALU.add),
                 reads=["prod"], writes=["lg"])
            for (c0, c1) in ((1024, 2048), (2048, 2560)):
                n_ = c1 - c0
                P.op("dve", lambda e, c0=c0, c1=c1, n_=n_: e.tensor_tensor(out=prod[:NS, 0:n_], in0=qs[:, c0:c1], in1=ks[:, c0 - 768:c1 - 768], op=ALU.mult),
                     reads=["sqk"], writes=["prod"])
                P.op("dve", lambda e, c0=c0, c1=c1, n_=n_: e.tensor_reduce(out=lg[:, c0 // 128:c1 // 128], in_=prod[:NS, 0:n_].rearrange("p (h d) -> p h d", d=128),
                                                                      axis=AX.X, op=ALU.add),
                     reads=["prod"], writes=["lg"])
            P.op("act", lambda e: e.activation(out=enew[:], in_=lg[:], func=AF.Exp, scale=SCALE), reads=["lg"], writes=["enew"])
            for gi_, gname in enumerate(("A", "B1", "B2", "B3")):
                G = GROUPS[gname]
                H, L, dil = G["H"], G["L"], G["dil"]
                W = 2 * H * 128
                isA = gname == "A"
                hq = 8 if isA else 4
                hb = 0 if isA else 8 + 4 * (gi_ - 1)
                kb = 0 if isA else 2 + 4 * (gi_ - 1)
                qoff = hb * 128
                csrc = bass.AP(caches[gname].tensor, 0, [[dil * W, 128], [L * W, NS], [1, W]])
                P.op("pool", lambda e, csrc=csrc, W=W: e.dma_start(out=kvc[:, :, :W], in_=csrc), writes=["kvc"], dma=True)
                P.op("sp", lambda e, gname=gname, L=L, H=H, kb=kb: e.dma_start(out=kvs[gname][:, L - 1, 0:H * 128], in_=ks[:, kb * 128:(kb + H) * 128]),
                     reads=["sqk"], writes=[("kvs_newk", gname)], dma=True)
                P.op("sp", lambda e, gname=gname, L=L, H=H, kb=kb: e.dma_start(out=kvs[gname][:, L - 1, H * 128:2 * H * 128], in_=vs[:, kb * 128:(kb + H) * 128]),
                     reads=["svs"], writes=[("kvs_newv", gname)], dma=True)
                tc_ = 1 if isA else 2
                for t0 in range(0, NS, tc_):
                    for j in range(tc_):
                        for half in range(hq * 128 // 512):
                            P.op("pe", lambda e, t0=t0, j=j, half=half, hq=hq, qoff=qoff: e.matmul(
                                pst_f[:, j * hq * 128 + half * 512:j * hq * 128 + (half + 1) * 512], lhsT=sel[:, t0 + j, :],
                                rhs=qsb[:, qoff + half * 512:qoff + (half + 1) * 512], start=True, stop=True),
                                reads=["sel", "qsb"], writes=["pstf"])
                    P.op("act", lambda e: e.activation(out=qrep[:], in_=pst_f, func=AF.Copy), reads=["pstf"], writes=["qrep"])
                    if isA:
                        P.op("dve", lambda e, t0=t0: e.tensor_tensor(
                            out=prod[:].rearrange("p (k g d) -> p k g d", k=2, g=4),
                            in0=kvc[:, t0, 0:256].rearrange("p (k d) -> p k d", k=2).unsqueeze(2).to_broadcast([128, 2, 4, 128]),
                            in1=qrep[:].rearrange("p (k g d) -> p k g d", k=2, g=4), op=ALU.mult),
                            reads=["kvc", "qrep"], writes=["prod"])
                    else:
                        P.op("dve", lambda e, t0=t0: e.tensor_tensor(
                            out=prod[:].rearrange("p (t c) -> p t c", t=2), in0=kvc[:, t0:t0 + 2, 0:512],
                            in1=qrep[:].rearrange("p (t c) -> p t c", t=2), op=ALU.mult),
                            reads=["kvc", "qrep"], writes=["prod"])
                    P.op("dve", lambda e, t0=t0, hq=hq, tc_=tc_: e.tensor_reduce(
                        out=Ssb[:, t0 * hq:(t0 + tc_) * hq], in_=prod[:].rearrange("p (h d) -> p h d", d=128), axis=AX.X, op=ALU.add),
                        reads=["prod"], writes=["Ssb"])
                P.op("dve", lambda e, hq=hq, hb=hb: e.scalar_tensor_tensor(
                    out=S2[:, :NS * hq].rearrange("p (t h) -> p t h", h=hq), in0=Ssb[:, :NS * hq].rearrange("p (t h) -> p t h", h=hq),
                    scalar=SCALE, in1=sbias_t[:, hb:hb + hq].unsqueeze(1).to_broadcast([128, NS, hq]), op0=ALU.mult, op1=ALU.add),
                    reads=["Ssb", "sbias"], writes=["S2"])
                P.op("act", lambda e, hq=hq: e.activation(out=Pb[:, 0:hq, :], in_=S2[:, :NS * hq].rearrange("p (t h) -> p h t", h=hq), func=AF.Exp),
                     reads=["S2"], writes=["Pb"])
                P.op("dve", lambda e, hq=hq: e.tensor_tensor(
                    out=Pexp[:, 0:hq, :, :], in0=Pb[:, 0:hq, :].unsqueeze(2).to_broadcast([128, hq, NS, NS]),
                    in1=dmask_t[:].unsqueeze(1).to_broadcast([128, hq, NS, NS]), op=ALU.mult),
                    reads=["Pb", "dmask"], writes=["Pexp"])
                for h in range(hq):
                    kvh = h // 4 if isA else h
                    for tp in range(NS):
                        P.op("pe", lambda e, h=h, tp=tp, kvh=kvh, H=H: e.matmul(
                            po_f[:NS, h * 128:(h + 1) * 128], lhsT=Pexp[:, h, tp, :], rhs=kvc[:, tp, (H + kvh) * 128:(H + kvh + 1) * 128],
                            start=(tp == 0), stop=(tp == NS - 1)),
                            reads=["Pexp", "kvc"], writes=["pof"])
                    P.op("pe", lambda e, h=h: e.matmul(pz[:NS, 1, h:h + 1], lhsT=Pb[:, h, :], rhs=onesb[:, 0:1], start=True, stop=True),
                         reads=["Pb", "onesb"], writes=[("pz", 1)])
                if isA:
                    P.op("dve", lambda e: e.tensor_tensor(
                        out=tmpv[:].rearrange("p (k g) d -> p k g d", k=2),
                        in0=vs[:, 0:256].rearrange("p (k d) -> p k d", k=2).unsqueeze(2).to_broadcast([NS, 2, 4, 128]),
                        in1=enew[:, 0:8].rearrange("p (k g) -> p k g", k=2).unsqueeze(3).to_broadcast([NS, 2, 4, 128]), op=ALU.mult),
                        reads=["svs", "enew"], writes=["tmpv"])
                    P.op("dve", lambda e: e.tensor_tensor(out=numA[:], in0=po_f[:NS, 0:1024].rearrange("p (h d) -> p h d", d=128), in1=tmpv[:], op=ALU.add),
                         reads=["pof", "tmpv"], writes=["numA"])
                    P.op("dve", lambda e: e.tensor_tensor(out=denA[:], in0=pz[:NS, 1, 0:8], in1=enew[:, 0:8], op=ALU.add),
                         reads=[("pz", 1), "enew"], writes=["denA"])
                    P.op("dve", lambda e: e.tensor_tensor(out=denA[:], in0=denA[:], in1=esink[:NS, 0:8], op=ALU.add),
                         reads=["denA", "esink"], writes=["denA"])
                else:
                    P.op("dve", lambda e, kb=kb, hb=hb: e.tensor_tensor(
                        out=tmpv[:, 0:4, :], in0=vs[:, kb * 128:(kb + 4) * 128].rearrange("p (h d) -> p h d", d=128),
                        in1=enew[:, hb:hb + 4].unsqueeze(2).to_broadcast([NS, 4, 128]), op=ALU.mult),
                        reads=["svs", "enew"], writes=["tmpv"])
                    if gname == "B1":
                        P.op("dve", lambda e: e.tensor_tensor(out=numB[:], in0=po_f[:NS, 0:512].rearrange("p (h d) -> p h d", d=128), in1=tmpv[:, 0:4, :], op=ALU.add),
                             reads=["pof", "tmpv"], writes=["numB"])
                        P.op("dve", lambda e, hb=hb: e.tensor_tensor(out=denB[:], in0=pz[:NS, 1, 0:4], in1=enew[:, hb:hb + 4], op=ALU.add),
                             reads=[("pz", 1), "enew"], writes=["denB"])
                    else:
                        P.op("dve", lambda e: e.tensor_tensor(out=numB[:], in0=numB[:], in1=tmpv[:, 0:4, :], op=ALU.add), reads=["numB", "tmpv"], writes=["numB"])
                        P.op("dve", lambda e: e.tensor_tensor(out=numB[:], in0=po_f[:NS, 0:512].rearrange("p (h d) -> p h d", d=128), in1=numB[:], op=ALU.add),
                             reads=["pof", "numB"], writes=["numB"])
                        P.op("dve", lambda e, hb=hb: e.tensor_tensor(out=denB[:], in0=denB[:], in1=enew[:, hb:hb + 4], op=ALU.add), reads=["denB", "enew"], writes=["denB"])
                        P.op("dve", lambda e: e.tensor_tensor(out=denB[:], in0=pz[:NS, 1, 0:4], in1=denB[:], op=ALU.add), reads=[("pz", 1), "denB"], writes=["denB"])
            for (num, den, nh_, dstT, nm) in ((numA, denA, 8, oaT, "oaTs"), (numB, denB, 4, obT, "obTs")):
                P.op("dve", lambda e, den=den: e.reciprocal(out=den[:], in_=den[:]), reads=["denA", "denB"], writes=["denA", "denB"])
                P.op("dve", lambda e, num=num, den=den, nh_=nh_: e.tensor_tensor(out=num[:], in0=num[:], in1=den[:].unsqueeze(2).to_broadcast([NS, nh_, 128]), op=ALU.mult),
                     reads=["numA", "numB", "denA", "denB"], writes=["numA", "numB"])
                for h0 in range(0, nh_, 4):
                    for h in range(4):
                        P.op("pe", lambda e, num=num, h0=h0, h=h: e.transpose(out=ptr[:, 0, h * 128:h * 128 + NS], in_=num[:, h0 + h, :], identity=ident[:NS, :NS]),
                             reads=["numA", "numB", "ident"], writes=[("ptr", 0)])
                    P.op("act", lambda e, dstT=dstT, h0=h0: e.activation(
                        out=dstT[:, h0:h0 + 4, 1024:1024 + NS], in_=ptr[:, 0, :].rearrange("p (a b) -> p a b", b=128)[:, :, :NS], func=AF.Copy),
                        reads=[("ptr", 0)], writes=[(nm, h0)])
            P.barrier()

        if debug:
            P.op("sp", lambda e: e.dma_start(out=dbg["oaT"], in_=oaT[:]), reads=[], writes=["dbg1"], dma=True)
            P.op("sp", lambda e: e.dma_start(out=dbg["obT"], in_=obT[:]), reads=[], writes=["dbg2"], dma=True)
        if stop_after == 3:
            P.barrier(); P.emit(); esA.close(); return nc
        esR = ExitStack()
        mT = esR.enter_context(nc.sbuf_tensor("mT", [128, 16, 1040], BF16, side="right"))
        pst_f = pst[:].rearrange("p a b -> p (a b)")
        po_f = po[:].rearrange("p a b -> p (a b)")
        with ExitStack() as es4:
            def sb4(name, shape, dt=F32):
                return es4.enter_context(nc.sbuf_tensor(name, list(shape), dt))
            wr4 = [sb4(f"wr4_{i}", [128, 16, 256], BF16) for i in range(2)]
            ring[:] = list(wring) + wr4
            sga = [sb4(f"sga{i}", [128, 512]) for i in range(2)]
            sgb = [sb4(f"sgb{i}", [128, 512]) for i in range(2)]
            t1 = [sb4(f"t1_{i}", [128, 512]) for i in range(2)]
            t2 = [sb4(f"t2_{i}", [128, 512]) for i in range(2)]
            psets = [(pz[:, 0, :], pz[:, 1, :], pst[:, 0, :], pst[:, 1, :], ("pz", 0), ("pz", 1), ("pst", 0), ("pst", 1)),
                     (ptr[:, 0, :], ptr[:, 1, :], po_f[:, 0:512], po_f[:, 512:1024], ("ptr", 0), ("ptr", 1), ("pof", 0), ("pof", 1))]
            blocks = [(XT_O, 0, 512), (XT_O + 512, 512, 512), (XT_S, 1024, NS)]
            it = 0
            for c in range(8):
                wga, kga = load_w(w_in, 6144 + c * 256, 256)
                wgb, kgb = load_w(w_in, 8192 + c * 256, 256)
                wa, ka_ = load_w(wba, c * 256, 256, nk=8)
                wb_, kb_ = load_w(wbb, c * 256, 256, nk=4)
                for j in range(2):
                    dm = c * 2 + j
                    for (toff, ooff, n) in blocks:
                        s_ = it % 2
                        it += 1
                        GA, GB, YA, YB, kGA, kGB, kYA, kYB = psets[s_]
                        for (dst, dkey, wt_, wk_, nk_, src, skey_) in ((GA, kGA, wga, kga, 16, lambda k, toff=toff, n=n: xnT[:, k, toff:toff + n], "xn"),
                                                                       (GB, kGB, wgb, kgb, 16, lambda k, toff=toff, n=n: xnT[:, k, toff:toff + n], "xn"),
                                                                       (YA, kYA, wa, ka_, 8, lambda k, ooff=ooff, n=n: oaT[:, k, ooff:ooff + n], "oa"),
                                                                       (YB, kYB, wb_, kb_, 4, lambda k, ooff=ooff, n=n: obT[:, k, ooff:ooff + n], "ob")):
                            for k in range(nk_):
                                P.op("pe", lambda e, dst=dst, wt_=wt_, k=k, j=j, src=src, nk_=nk_, n=n: e.matmul(
                                    dst[:, :n], lhsT=wt_[:, k, j * 128:(j + 1) * 128], rhs=src(k), start=(k == 0), stop=(k == nk_ - 1)),
                                    reads=[wk_], writes=[dkey])
                        P.op("act", lambda e, GA=GA, s_=s_, n=n: e.activation(out=sga[s_][:, :n], in_=GA[:, :n], func=AF.Sigmoid), reads=[kGA], writes=[("sga", s_)])
                        P.op("act", lambda e, GB=GB, s_=s_, n=n: e.activation(out=sgb[s_][:, :n], in_=GB[:, :n], func=AF.Sigmoid), reads=[kGB], writes=[("sgb", s_)])
                        P.op("dve", lambda e, YA=YA, s_=s_, n=n: e.tensor_tensor(out=t1[s_][:, :n], in0=YA[:, :n], in1=sga[s_][:, :n], op=ALU.mult),
                             reads=[kYA, ("sga", s_)], writes=[("t1", s_)])
                        P.op("dve", lambda e, YB=YB, s_=s_, n=n: e.tensor_tensor(out=t2[s_][:, :n], in0=YB[:, :n], in1=sgb[s_][:, :n], op=ALU.mult),
                             reads=[kYB, ("sgb", s_)], writes=[("t2", s_)])
                        P.op("dve", lambda e, s_=s_, n=n, dm=dm, ooff=ooff: e.tensor_tensor(out=mT[:, dm, ooff:ooff + n], in0=t1[s_][:, :n], in1=t2[s_][:, :n], op=ALU.add),
                             reads=[("t1", s_), ("t2", s_)], writes=[("mT", dm, ooff)])
            P.barrier()
        esA.close()

        es5 = ExitStack()
        def sb5(name, shape, dt=F32):
            return es5.enter_context(nc.sbuf_tensor(name, list(shape), dt))
        h_t = sb5("h_t", [128, 9, D])
        with ExitStack() as es5b:
            def sb5b(name, shape, dt=F32):
                return es5b.enter_context(nc.sbuf_tensor(name, list(shape), dt))
            ring[:] = [sb5b(f"wr5_{i}", [128, 16, 256], BF16) for i in range(2)]
            xblk = [sb5b(f"xblk{i}", [128, 256]) for i in range(3)]
            it = 0
            for c in range(8):
                wt, wkey = load_w(wout, c * 256, 256)
                for i in range(9):
                    n = 128 if i < 8 else NS
                    ooff = i * 128
                    b = it % 2
                    xs_ = it % 3
                    it += 1
                    srcx = xo[i * 128:(i + 1) * 128, c * 256:(c + 1) * 256] if i < 8 else xs[:, c * 256:(c + 1) * 256]
                    P.op("sp", lambda e, xs_=xs_, srcx=srcx, n=n: e.dma_start(out=xblk[xs_][:n, :], in_=srcx), writes=[("xblk", xs_)], dma=True)
                    for k in range(16):
                        P.op("pe", lambda e, k=k, b=b, n=n, ooff=ooff, wt=wt: e.matmul(pz[:n, b, :256], lhsT=mT[:, k, ooff:ooff + n], rhs=wt[:, k, :256],
                                                                                  start=(k == 0), stop=(k == 15)),
                             reads=[wkey], writes=[("pz", b)])
                    P.op("dve", lambda e, b=b, n=n, i=i, c=c, xs_=xs_: e.tensor_tensor(out=h_t[:n, i, c * 256:(c + 1) * 256], in0=pz[:n, b, :256],
                                                                                    in1=xblk[xs_][:n, :], op=ALU.add),
                         reads=[("pz", b), ("xblk", xs_)], writes=[("h", i)])
            P.barrier()
        esR.close()
        if debug:
            P.op("sp", lambda e: e.dma_start(out=yp, in_=h_t[:, 0:8, :].rearrange("p i d -> p i d")) if False else e.dma_start(out=yp.rearrange("(i p) d -> p i d", p=128), in_=h_t[:, 0:8, :]),
                 reads=[], writes=["dbgh"], dma=True)
            P.op("sp", lambda e: e.dma_start(out=ys, in_=h_t[:NS, 8, :]), reads=[], writes=["dbghs"], dma=True)
            P.barrier(); P.emit(); es5.close(); return nc

        with ExitStack() as es6:
            def sb6(name, shape, dt=F32):
                return es6.enter_context(nc.sbuf_tensor(name, list(shape), dt))
            wqb = sb6("wqb", [128, 16, 1024], BF16)
            w2rep = sb6("w2rep", [128, D])
            skb = sb6("skb", [128, 256])
            iota16 = sb6("iota16t", [128, 16]); thr16 = sb6("thr16", [128, 16])
            hn = sb6("hn", [128, D])
            hnT = sb6("hnT", [128, 16, 128], BF16)
            qf = sb6("qf", [128, 1024])
            qT = sb6("qT", [128, 8, 128])
            big = sb6("big", [128, 2048])
            sS = big[:]
            s2 = sb6("s2", [128, 256])
            vals = sb6("vals", [128, 16, 16]); idxu = sb6("idxu", [128, 16, 16], U32); idxf = sb6("idxf", [128, 16, 16])
            cand = big[:].rearrange("p (h c) -> p h c", c=256)
            best = sb6("best", [128, 8, 16]); bcu = sb6("bcu", [128, 8, 16], U32); bcf = sb6("bcf", [128, 128])
            akf = sb6("akf", [128, 128]); bkf = sb6("bkf", [128, 128])
            eq = big[:].rearrange("p (s a) -> p s a", a=16)
            i1f = sb6("i1f", [128, 128]); i2f = sb6("i2f", [128, 128]); ef = sb6("ef", [128, 128]); eidx2 = [sb6(f"eidx{j}", [128, 128], U32) for j in range(2)]
            gate = sb6("gate", [128, 8, 16]); gsum = sb6("gsum", [128, 8])
            act_ = sb6("act_", [128, 128]); wgt2 = [sb6(f"wgt{j}", [128, 128]) for j in range(2)]
            NUB = 10
            ubuf = [sb6(f"ubuf{i}", [128, D], BF16) for i in range(NUB)]
            junk6 = sb6("junk6", [128, D], BF16)
            st6 = sb6("st6", [128, 8])
            identb = sb6("identb", [128, 128], BF16)
            dg = [sb6(f"dg{i}", [128, 128], BF16) for i in range(4)]
            pz_f = pz[:].rearrange("p a b -> p (a b)")
            P.op("dve", lambda e: e.tensor_copy(out=identb[:], in_=ident[:]), reads=["ident"], writes=["identb"])
            for c4 in range(4):
                P.op("pool", lambda e, c4=c4: e.dma_start(out=wqb[:, :, c4 * 256:(c4 + 1) * 256],
                                                         in_=wq[:, c4 * 256:(c4 + 1) * 256].rearrange("(k p) n -> p k n", p=128)),
                     writes=[("wqb", c4)], dma=True)
            P.op("sp", lambda e: e.dma_start(out=w2rep[:], in_=norm2_w.partition_broadcast(128)), writes=["w2rep"], dma=True)
            P.op("sp", lambda e: e.dma_start(out=iota16[:], in_=iota16_d), writes=["iota16"], dma=True)
            P.op("dve", lambda e: e.tensor_scalar(out=thr16[:], in0=iota16[:], scalar1=16.0, scalar2=None, op0=ALU.mult), reads=["iota16"], writes=["thr16"])
            P.op("dve", lambda e: e.memset(skb[:], 0.0), writes=["skb"])
            P.op("sp", lambda e: e.dma_start(out=skb[0:64, 0:128], in_=subkT[0]), reads=["skb"], writes=["skb"], dma=True)
            P.op("sp", lambda e: e.dma_start(out=skb[64:128, 128:256], in_=subkT[1]), reads=["skb"], writes=["skb"], dma=True)
            ucnt = [0]
            def routing(i):
                n = 128 if i < 8 else NS
                hi = h_t[:n, i, :]
                hk = ("h", i)
                P.op("act", lambda e, hi=hi, n=n: e.activation(out=junk6[:n, :], in_=hi, func=AF.Square, accum_out=st6[:n, 0:1]), reads=[hk], writes=["junk6", "st6"])
                P.op("act", lambda e, n=n: e.activation(out=st6[:n, 1:2], in_=st6[:n, 0:1], func=AF.Ln, scale=1.0 / D, bias=EPS), reads=["st6"], writes=["st6"])
                P.op("act", lambda e, n=n: e.activation(out=st6[:n, 2:3], in_=st6[:n, 1:2], func=AF.Exp, scale=-0.5), reads=["st6"], writes=["st6"])
                P.op("dve", lambda e, hi=hi, n=n: e.scalar_tensor_tensor(out=hn[:n, :], in0=hi, scalar=st6[:n, 2:3], in1=w2rep[:n, :], op0=ALU.mult, op1=ALU.mult),
                     reads=[hk, "st6", "w2rep"], writes=["hn"])
                for k4 in range(4):
                    bank = k4 % 2
                    for kk in range(4):
                        k = k4 * 4 + kk
                        P.op("pe", lambda e, k=k, kk=kk, bank=bank, n=n: e.transpose(out=ptr[:, bank, kk * 128:kk * 128 + n], in_=hn[:n, k * 128:(k + 1) * 128],
                                                                                     identity=ident[:n, :n]), reads=["hn", "ident"], writes=[("ptr", bank)])
                    P.op("act", lambda e, k4=k4, bank=bank, n=n: e.activation(out=hnT[:, k4 * 4:(k4 + 1) * 4, :n],
                                                                            in_=ptr[:, bank, :].rearrange("p (a b) -> p a b", b=128)[:, :, :n], func=AF.Copy),
                         reads=[("ptr", bank)], writes=["hnT"])
                for half in range(2):
                    for k in range(16):
                        P.op("pe", lambda e, k=k, half=half, n=n: e.matmul(pz[:n, half, :], lhsT=hnT[:, k, :n], rhs=wqb[:, k, half * 512:(half + 1) * 512],
                                                                        start=(k == 0), stop=(k == 15)),
                             reads=["hnT"] + [("wqb", c4) for c4 in range(4)], writes=[("pz", half)])
                P.op("act", lambda e, n=n: e.activation(out=qf[:n, :].rearrange("p (a b) -> p a b", a=2), in_=pz[:n, :, :], func=AF.Copy),
                     reads=[("pz", 0), ("pz", 1)], writes=["qf"])
                for h4 in range(2):
                    for hh in range(4):
                        h = h4 * 4 + hh
                        P.op("pe", lambda e, h=h, hh=hh, h4=h4, n=n: e.transpose(out=ptr[:, h4, hh * 128:hh * 128 + n], in_=qf[:n, h * 128:(h + 1) * 128],
                                                                                identity=ident[:n, :n]), reads=["qf", "ident"], writes=[("ptr", h4)])
                    P.op("act", lambda e, h4=h4, n=n: e.activation(out=qT[:, h4 * 4:(h4 + 1) * 4, :n],
                                                                 in_=ptr[:, h4, :].rearrange("p (a b) -> p a b", b=128)[:, :, :n], func=AF.Copy),
                         reads=[("ptr", h4)], writes=["qT"])
                for half in range(2):
                    for hh in range(4):
                        h = half * 4 + hh
                        P.op("pe", lambda e, h=h, hh=hh, n=n: e.matmul(pz_f[:n, hh * 256:(hh + 1) * 256], lhsT=qT[:, h, :n], rhs=skb[:, :], start=True, stop=True),
                             reads=["qT", "skb"], writes=[("pz", hh // 2)])
                    if half == 0:
                        P.op("act", lambda e, n=n: e.activation(out=sS[:n, 0:1024], in_=pz_f[:n, :], func=AF.Copy), reads=[("pz", 0), ("pz", 1)], writes=["big"])
                    else:
                        P.op("dve", lambda e, n=n: e.tensor_copy(out=sS[:n, 1024:2048], in_=pz_f[:n, :]), reads=[("pz", 0), ("pz", 1)], writes=["big"])
                for hc in range(16):
                    src = sS[:n, hc * 128:(hc + 1) * 128]
                    P.op("dve", lambda e, src=src, hc=hc, n=n: e.max(out=vals[:n, hc, 0:8], in_=src), reads=["big"], writes=["vals"])
                    P.op("dve", lambda e, src=src, hc=hc, n=n: e.max_index(out=idxu[:n, hc, 0:8], in_max=vals[:n, hc, 0:8], in_values=src),
                         reads=["big", "vals"], writes=["idxu"])
                    P.op("dve", lambda e, src=src, hc=hc, n=n: e.match_replace(out=s2[:n, 0:128], in_to_replace=vals[:n, hc, 0:8], in_values=src, imm_value=-1e30),
                         reads=["big", "vals"], writes=["s2"])
                    P.op("dve", lambda e, hc=hc, n=n: e.max(out=vals[:n, hc, 8:16], in_=s2[:n, 0:128]), reads=["s2"], writes=["vals"])
                    P.op("dve", lambda e, hc=hc, n=n: e.max_index(out=idxu[:n, hc, 8:16], in_max=vals[:n, hc, 8:16], in_values=s2[:n, 0:128]),
                         reads=["s2", "vals"], writes=["idxu"])
                P.op("dve", lambda e, n=n: e.tensor_copy(out=idxf[:n], in_=idxu[:n]), reads=["idxu"], writes=["idxf"])
                v4 = vals[:n].rearrange("p (h c) k -> p h c k", c=2)
                P.op("dve", lambda e, v4=v4, n=n: e.tensor_tensor(out=cand[:n].rearrange("p h (a b) -> p h a b", b=16),
                                                                 in0=v4[:, :, 0, :].unsqueeze(3).to_broadcast([n, 8, 16, 16]),
                                                                 in1=v4[:, :, 1, :].unsqueeze(2).to_broadcast([n, 8, 16, 16]), op=ALU.add),
                     reads=["vals"], writes=["big"])
                for h in range(8):
                    src = cand[:n, h, :]
                    P.op("dve", lambda e, src=src, h=h, n=n: e.max(out=best[:n, h, 0:8], in_=src), reads=["big"], writes=["best"])
                    P.op("dve", lambda e, src=src, h=h, n=n: e.max_index(out=bcu[:n, h, 0:8], in_max=best[:n, h, 0:8], in_values=src),
                         reads=["big", "best"], writes=["bcu"])
                    P.op("dve", lambda e, src=src, h=h, n=n: e.match_replace(out=s2[:n, :], in_to_replace=best[:n, h, 0:8], in_values=src, imm_value=-1e30),
                         reads=["big", "best"], writes=["s2"])
                    P.op("dve", lambda e, h=h, n=n: e.max(out=best[:n, h, 8:16], in_=s2[:n, :]), reads=["s2"], writes=["best"])
                    P.op("dve", lambda e, h=h, n=n: e.max_index(out=bcu[:n, h, 8:16], in_max=best[:n, h, 8:16], in_values=s2[:n, :]),
                         reads=["s2", "best"], writes=["bcu"])
                P.op("dve", lambda e, n=n: e.tensor_copy(out=bcf[:n, :], in_=bcu[:n].rearrange("p h k -> p (h k)")), reads=["bcu"], writes=["bcf"])
                P.op("dve", lambda e, n=n: e.tensor_tensor(out=eq[:n], in0=bcf[:n, :].unsqueeze(2).to_broadcast([n, 128, 16]),
                                                          in1=thr16[:n, :].unsqueeze(1).to_broadcast([n, 128, 16]), op=ALU.is_ge),
                     reads=["bcf", "thr16"], writes=["big"])
                P.op("dve", lambda e, n=n: e.tensor_reduce(out=akf[:n, :], in_=eq[:n], axis=AX.X, op=ALU.add), reads=["big"], writes=["akf"])
                P.op("dve", lambda e, n=n: e.tensor_scalar(out=akf[:n, :], in0=akf[:n, :], scalar1=-1.0, scalar2=None, op0=ALU.add), reads=["akf"], writes=["akf"])
                P.op("dve", lambda e, n=n: e.scalar_tensor_tensor(out=bkf[:n, :], in0=akf[:n, :], scalar=-16.0, in1=bcf[:n, :], op0=ALU.mult, op1=ALU.add),
                     reads=["akf", "bcf"], writes=["bkf"])
                i4 = idxf[:n].rearrange("p (h c) k -> p h c k", c=2)
                for (sel_f, cidx, dst_i, nm) in ((akf, 0, i1f, "i1f"), (bkf, 1, i2f, "i2f")):
                    P.op("dve", lambda e, sel_f=sel_f, n=n: e.tensor_tensor(out=eq[:n], in0=iota16[:n, :].unsqueeze(1).to_broadcast([n, 128, 16]),
                                                                           in1=sel_f[:n, :].unsqueeze(2).to_broadcast([n, 128, 16]), op=ALU.is_equal),
                         reads=["iota16", "akf", "bkf"], writes=["big"])
                    P.op("dve", lambda e, cidx=cidx, i4=i4, n=n: e.tensor_tensor(out=eq[:n].rearrange("p (h k) a -> p h k a", k=16),
                                                                               in0=eq[:n].rearrange("p (h k) a -> p h k a", k=16),
                                                                               in1=i4[:, :, cidx, :].unsqueeze(2).to_broadcast([n, 8, 16, 16]), op=ALU.mult),
                         reads=["big", "idxf"], writes=["big"])
                    P.op("dve", lambda e, dst_i=dst_i, n=n: e.tensor_reduce(out=dst_i[:n, :], in_=eq[:n], axis=AX.X, op=ALU.add), reads=["big"], writes=[nm])
                P.op("dve", lambda e, n=n: e.scalar_tensor_tensor(out=ef[:n, :], in0=i1f[:n, :], scalar=128.0, in1=i2f[:n, :], op0=ALU.mult, op1=ALU.add),
                     reads=["i1f", "i2f"], writes=["ef"])
                P.op("dve", lambda e, n=n: e.tensor_copy(out=eidx2[i % 2][:n, :], in_=ef[:n, :]), reads=["ef"], writes=[("eidx", i % 2)])
                P.op("dve", lambda e, n=n: e.tensor_tensor(out=gate[:n], in0=best[:n], in1=best[:n, :, 0:1].to_broadcast([n, 8, 16]), op=ALU.subtract),
                     reads=["best"], writes=["gate"])
                P.op("act", lambda e, n=n: e.activation(out=gate[:n], in_=gate[:n], func=AF.Exp), reads=["gate"], writes=["gate"])
                P.op("dve", lambda e, n=n: e.tensor_reduce(out=gsum[:n, :], in_=gate[:n], axis=AX.X, op=ALU.add), reads=["gate"], writes=["gsum"])
                P.op("dve", lambda e, n=n: e.reciprocal(out=gsum[:n, :], in_=gsum[:n, :]), reads=["gsum"], writes=["gsum"])
                P.op("dve", lambda e, n=n: e.tensor_tensor(out=gate[:n], in0=gate[:n], in1=gsum[:n, :].unsqueeze(2).to_broadcast([n, 8, 16]), op=ALU.mult),
                     reads=["gate", "gsum"], writes=["gate"])
            def uloop(i):
                n = 128 if i < 8 else NS
                hi = h_t[:n, i, :]
                hk = ("h", i)
                for slot in range(128):
                    ub = ucnt[0] % NUB
                    ucnt[0] += 1
                    P.op("pool", lambda e, ub=ub, slot=slot, n=n: e.indirect_dma_start(
                        out=ubuf[ub][:n, :], out_offset=None, in_=pu, in_offset=bass.IndirectOffsetOnAxis(ap=eidx2[i % 2][:n, slot:slot + 1], axis=0)),
                        reads=[("eidx", i % 2)], writes=[("ubuf", ub)], dma=True)
                    P.op("dve", lambda e, ub=ub, slot=slot, n=n: e.scalar_tensor_tensor(
                        out=junk6[:n, :], in0=ubuf[ub][:n, :], scalar=1.0, in1=hn[:n, :], op0=ALU.mult, op1=ALU.mult,
                        accum_out=act_[:n, slot:slot + 1]),
                        reads=[("ubuf", ub), "hn"], writes=["junk6", "act_"])
                P.op("act", lambda e, n=n: e.activation(out=wgt2[i % 2][:n, :], in_=act_[:n, :], func=AF.Gelu), reads=["act_"], writes=[("wgt", i % 2)])
                P.op("dve", lambda e, n=n: e.tensor_tensor(out=wgt2[i % 2][:n, :], in0=wgt2[i % 2][:n, :], in1=gate[:n].rearrange("p h k -> p (h k)"), op=ALU.mult),
                     reads=[("wgt", i % 2), "gate"], writes=[("wgt", i % 2)])
            def vloop(i):
                n = 128 if i < 8 else NS
                hi = h_t[:n, i, :]
                hk = ("h", i)
                for slot in range(128):
                    ub = ucnt[0] % NUB
                    ucnt[0] += 1
                    dj = slot % 4
                    P.op("pool", lambda e, ub=ub, slot=slot, n=n: e.indirect_dma_start(
                        out=ubuf[ub][:n, :], out_offset=None, in_=pv, in_offset=bass.IndirectOffsetOnAxis(ap=eidx2[i % 2][:n, slot:slot + 1], axis=0)),
                        reads=[("eidx", i % 2)], writes=[("ubuf", ub)], dma=True)
                    P.op("act", lambda e, dj=dj, slot=slot, n=n: e.activation(out=dg[dj][:n, :n], in_=identb[:n, :n], func=AF.Copy, scale=wgt2[i % 2][:n, slot:slot + 1]),
                         reads=["identb", ("wgt", i % 2)], writes=[("dg", dj)])
                    for c in range(4):
                        dstp = pst_f[:n, c * 512:(c + 1) * 512] if c < 2 else po_f[:n, (c - 2) * 512:(c - 1) * 512]
                        P.op("pe", lambda e, dj=dj, ub=ub, c=c, dstp=dstp, slot=slot, n=n: e.matmul(
                            dstp, lhsT=dg[dj][:n, :n], rhs=ubuf[ub][:n, c * 512:(c + 1) * 512], start=(slot == 0), stop=(slot == 127)),
                            reads=[("dg", dj), ("ubuf", ub)], writes=["pstf" if c < 2 else "pof"])
                P.op("dve", lambda e, hi=hi, n=n, i=i: e.tensor_tensor(out=h_t[:n, i, 0:1024], in0=pst_f[:n, :], in1=h_t[:n, i, 0:1024], op=ALU.add),
                     reads=["pstf", hk], writes=[hk])
                P.op("dve", lambda e, hi=hi, n=n, i=i: e.tensor_tensor(out=h_t[:n, i, 1024:2048], in0=po_f[:n, :], in1=h_t[:n, i, 1024:2048], op=ALU.add),
                     reads=["pof", hk], writes=[hk])
                dsty = yp[i * 128:(i + 1) * 128, :] if i < 8 else ys
                P.op("sp", lambda e, dsty=dsty, hi=hi: e.dma_start(out=dsty, in_=hi), reads=[hk], writes=[("y", i)], dma=True)
            routing(0)
            for i in range(9):
                uloop(i)
                if i + 1 < 9:
                    routing(i + 1)
                vloop(i)
            P.barrier()
        es5.close()
        P.barrier()
        P.emit()
    return nc


def make_in_maps(inp, with_peer=True):
    xp = np.asarray(inp["x_prompt"], np.float32)
    xs = np.asarray(inp["x_sample"], np.float32)[:, 0, :]
    tabs = {"tab_" + u["name"]: np.ascontiguousarray(unit_variants(u)[2].reshape(128, -1)) for u in UNITS}
    shared = {
        "norm1_w": inp["norm1_w"][0], "w_in": inp["w_in"][0], "qna": inp["q_norm_a"][0], "kna": inp["k_norm_a"][0],
        "sink": inp["sink_a"][0], "qnb": inp["q_norm_b"][0], "knb": inp["k_norm_b"][0],
        "wba": inp["w_branch_a"][0], "wbb": inp["w_branch_b"][0], "wout": inp["w_out"][0],
        "norm2_w": inp["norm2_w"][0], "wq": inp["peer_wq"][0], "subk": inp["peer_subkeys"][0],
    }
    if with_peer:
        shared["pu"] = inp["peer_u"][0]; shared["pv"] = inp["peer_v"][0]
    shared = {k: np.ascontiguousarray(np.asarray(v, np.float32)) for k, v in shared.items()}
    shared.update(tabs)
    sb_ = np.zeros((128, 20), np.float32)
    ii = np.arange(128, dtype=np.float64)
    for h in range(20):
        dil = 1 if h < 12 else (4 if h < 16 else 16)
        sb_[:, h] = -SLOPES[h] * dil * (128.0 - ii)
    shared["sbias"] = sb_
    shared["dmask"] = np.ascontiguousarray(np.broadcast_to(np.eye(NS, dtype=np.float32).reshape(1, NS * NS), (128, NS * NS)))
    shared["iota16"] = np.ascontiguousarray(np.broadcast_to(np.arange(16, dtype=np.float32)[None, :], (128, 16)))
    shared["subkT"] = np.ascontiguousarray(np.asarray(inp["peer_subkeys"], np.float32)[0].transpose(0, 2, 1))
    maps = []
    for c in range(8):
        b, half = c // 2, c % 2
        m = dict(shared)
        m["xo"] = np.ascontiguousarray(xp[b, half * 1024:(half + 1) * 1024])
        m["xh"] = np.ascontiguousarray(xp[b, 0:1024]) if half == 1 else np.zeros((1024, D), np.float32)
        m["xs"] = np.ascontiguousarray(xs[c * NS:(c + 1) * NS])
        m["flag"] = np.ascontiguousarray(np.broadcast_to(np.array([[1.0, float(half)]], np.float32), (128, 2)))
        for nm, key in (("ca", "cache_a_kv"), ("cb1", "cache_b1_kv"), ("cb2", "cache_b2_kv"), ("cb3", "cache_b3_kv")):
            a = np.asarray(inp[key], np.float32)[0, c * NS:(c + 1) * NS]
            m[nm] = np.ascontiguousarray(a.reshape(NS, a.shape[1], -1))
        maps.append(m)
    return maps


_NC_CACHE = {}


def kernel(**inputs):
    if "nc" not in _NC_CACHE:
        _NC_CACHE["nc"] = build()
    nc = _NC_CACHE["nc"]
    maps = make_in_maps(inputs)
    res = run_bass_kernel_spmd(nc, maps, core_ids=list(range(8))).results
    y_p = np.zeros((4, 2048, D), np.float32)
    y_s = np.zeros((128, 1, D), np.float32)
    a_p = np.zeros((1, 4, 128, 2, 2, 128), np.float32)
    b1_p = np.zeros((1, 4, 128, 2, 4, 128), np.float32)
    b2_p = np.zeros((1, 4, 512, 2, 4, 128), np.float32)
    b3_p = np.zeros((1, 4, 2048, 2, 4, 128), np.float32)
    a_s = np.zeros((1, 128, 128, 2, 2, 128), np.float32)
    b1_s = np.zeros((1, 128, 128, 2, 4, 128), np.float32)
    b2_s = np.zeros((1, 128, 512, 2, 4, 128), np.float32)
    b3_s = np.zeros((1, 128, 2048, 2, 4, 128), np.float32)
    for c in range(8):
        b, half = c // 2, c % 2
        r = res[c]
        y_p[b, half * 1024:(half + 1) * 1024] = r["yp"]
        y_s[c * NS:(c + 1) * NS, 0] = r["ys"]
        b3_p[0, b, half * 1024:(half + 1) * 1024] = r["kvb3_p"].reshape(1024, 2, 4, 128)
        if half == 1:
            a_p[0, b] = r["kva_p"].reshape(128, 2, 2, 128)
            b1_p[0, b] = r["kvb1_p"].reshape(128, 2, 4, 128)
            b2_p[0, b] = r["kvb2_p"].reshape(512, 2, 4, 128)
        a_s[0, c * NS:(c + 1) * NS] = r["kva_s"].reshape(NS, 128, 2, 2, 128)
        b1_s[0, c * NS:(c + 1) * NS] = r["kvb1_s"].reshape(NS, 128, 2, 4, 128)
        b2_s[0, c * NS:(c + 1) * NS] = r["kvb2_s"].reshape(NS, 512, 2, 4, 128)
        b3_s[0, c * NS:(c + 1) * NS] = r["kvb3_s"].reshape(NS, 2048, 2, 4, 128)
    return (y_p, y_s, a_p, b1_p, b2_p, b3_p, a_s, b1_s, b2_s, b3_s)
```
